# Optimizing a Trainium2 kernel written in Bass

```python
import jax, jax.numpy as jnp
from jax import lax
import numpy as np

D_MODEL = 1024
BATCH = 32
SEQ = 2048
DEPTH = 2

CHUNK = 64

A_HEADS = 8
A_HEAD_DIM = 64
A_WIDTH = A_HEADS * A_HEAD_DIM
A_LEFT_CHUNKS = 8
A_BAND = (A_LEFT_CHUNKS + 1) * CHUNK
A_REL_CLIP = 256
A_N_REL = (CHUNK - 1) + A_REL_CLIP + 1

B_HEADS = 4
B_HEAD_DIM = 128
B_WIDTH = B_HEADS * B_HEAD_DIM
B_CONV = 4

C_WINDOWS = (2, 4, 8, 16)
C_GROUPS = 4
C_GROUP_DIM = 128
C_WIDTH = C_GROUPS * C_GROUP_DIM

N_BRANCH = 3
IN_SIZES = (A_WIDTH, A_WIDTH, A_WIDTH, 3 * B_WIDTH, B_HEADS, B_HEADS, B_WIDTH, C_WIDTH, N_BRANCH * D_MODEL)
D_IN = 3 * A_WIDTH + 4 * B_WIDTH + 2 * B_HEADS + C_WIDTH + N_BRANCH * D_MODEL

MOE_GROUPS = 4
MOE_PER_GROUP = 8
N_EXPERTS = MOE_GROUPS * MOE_PER_GROUP
MOE_TOPK = 2
MOE_FF = 512
MOE_BLOCK = 256

DN_ALPHA = (2 * DEPTH) ** 0.25
DN_BETA = (8 * DEPTH) ** -0.25
LN_EPS = 1e-5
RMS_EPS = 1e-6

kernel_name = 'hybrid_chunk_attn_gdn_pool_hmoe'


def layer_norm(x, g, b):
    xf = x.astype(jnp.float32)
    mu = jnp.mean(xf, axis=-1, keepdims=True)
    var = jnp.mean(jnp.square(xf - mu), axis=-1, keepdims=True)
    return ((xf - mu) * lax.rsqrt(var + LN_EPS) * g + b).astype(x.dtype)


def split_cols(t, sizes):
    outs, start = [], 0
    for s in sizes:
        outs.append(t[..., start:start + s])
        start += s
    return outs


def chunk_band_attention(q, k, v, rel_bias):
    b, l, h, dh = q.shape
    nc = l // CHUNK
    pad = A_LEFT_CHUNKS * CHUNK
    kp = jnp.pad(k, ((0, 0), (pad, 0), (0, 0), (0, 0)))
    vp = jnp.pad(v, ((0, 0), (pad, 0), (0, 0), (0, 0)))
    dist = jnp.arange(CHUNK)[:, None] + pad - jnp.arange(A_BAND)[None, :]
    idx = jnp.clip(dist, -(CHUNK - 1), A_REL_CLIP) + (CHUNK - 1)
    bias = rel_bias[:, idx].astype(jnp.float32)
    qc = jnp.moveaxis(q.reshape(b, nc, CHUNK, h, dh), 1, 0)
    scale = dh ** -0.5

    def one_chunk(args):
        qi, ci = args
        start = ci * CHUNK
        kb = lax.dynamic_slice_in_dim(kp, start, A_BAND, axis=1)
        vb = lax.dynamic_slice_in_dim(vp, start, A_BAND, axis=1)
        s = jnp.einsum('bqhd,bkhd->bhqk', qi, kb, preferred_element_type=jnp.float32) * scale + bias
        valid = (start - pad + jnp.arange(A_BAND)) >= 0
        s = jnp.where(valid, s, -jnp.inf)
        p = jax.nn.softmax(s, axis=-1).astype(vb.dtype)
        return jnp.einsum('bhqk,bkhd->bqhd', p, vb)

    o = lax.map(one_chunk, (qc, jnp.arange(nc)))
    return jnp.moveaxis(o, 0, 1).reshape(b, l, h * dh)


def causal_depthwise_conv(x, w):
    kw, c = w.shape
    return lax.conv_general_dilated(
        x, w[:, None, :].astype(x.dtype), window_strides=(1,), padding=[(kw - 1, 0)],
        dimension_numbers=('NWC', 'WIO', 'NWC'), feature_group_count=c)


def l2_normalize(t):
    return t * lax.rsqrt(jnp.sum(t * t, axis=-1, keepdims=True) + RMS_EPS)


def gated_delta_rule(q, k, v, beta, g):
    b, l, h, dk = q.shape
    dv = v.shape[-1]
    nc = l // CHUNK

    def to_chunks(t):
        t = t.reshape((b, nc, CHUNK, h) + t.shape[3:])
        return jnp.moveaxis(t, 3, 1)

    q = to_chunks(q) * (dk ** -0.5)
    k = to_chunks(k)
    v = to_chunks(v)
    beta = to_chunks(beta)
    gc = jnp.cumsum(to_chunks(g), axis=-1)
    tril = jnp.tril(jnp.ones((CHUNK, CHUNK), dtype=bool))
    strict = jnp.tril(jnp.ones((CHUNK, CHUNK), dtype=bool), -1)
    decay = jnp.exp(jnp.where(tril, gc[..., :, None] - gc[..., None, :], -jnp.inf))
    kbeta = k * beta[..., None]
    lower = jnp.where(strict, jnp.einsum('bhnid,bhnjd->bhnij', kbeta, k) * decay, 0.0)
    eye = jnp.eye(CHUNK, dtype=q.dtype)
    tmat = lax.linalg.triangular_solve(eye + lower, jnp.broadcast_to(eye, lower.shape),
                                       left_side=True, lower=True)
    u = tmat @ (v * beta[..., None])
    w = tmat @ (kbeta * jnp.exp(gc)[..., None])
    qk = jnp.einsum('bhnid,bhnjd->bhnij', q, k) * decay
    qg = q * jnp.exp(gc)[..., None]
    kd = k * jnp.exp(gc[..., -1:] - gc)[..., None]
    glast = jnp.exp(gc[..., -1])

    def step(state, inp):
        u_i, w_i, qk_i, qg_i, kd_i, gl_i = inp
        v_new = u_i - w_i @ state
        o_i = qg_i @ state + qk_i @ v_new
        state = state * gl_i[..., None, None] + jnp.einsum('bhck,bhcv->bhkv', kd_i, v_new)
        return state, o_i

    xs = tuple(jnp.moveaxis(t, 2, 0) for t in (u, w, qk, qg, kd, glast))
    s0 = jnp.zeros((b, h, dk, dv), q.dtype)
    _, o = lax.scan(step, s0, xs)
    return o.transpose(1, 0, 3, 2, 4).reshape(b, l, h, dv)


def multiscale_pool(u, pool_w, pool_scale):
    b, l, _ = u.shape
    uf = u.astype(jnp.float32)
    cs0 = jnp.pad(jnp.cumsum(uf, axis=1), ((0, 0), (1, 0), (0, 0)))
    pos = jnp.arange(1, l + 1, dtype=jnp.float32)
    groups = []
    for gi, win in enumerate(C_WINDOWS):
        lo_c, hi_c = gi * C_GROUP_DIM, (gi + 1) * C_GROUP_DIM
        hi = cs0[:, 1:, lo_c:hi_c]
        lo = jnp.pad(cs0[:, :, lo_c:hi_c], ((0, 0), (win - 1, 0), (0, 0)))[:, :l]
        cnt = jnp.minimum(pos, float(win))[None, :, None]
        groups.append((hi - lo) / cnt - uf[:, :, lo_c:hi_c])
    pooled = jnp.stack(groups, axis=2)
    mixed = jnp.einsum('blgc,gcd->blgd', pooled, pool_w.astype(jnp.float32))
    return (mixed.reshape(b, l, C_WIDTH) * pool_scale).astype(u.dtype)


def hybrid_mixer(x, w_in, rel_bias, conv_w, a_log, dt_bias, gdn_norm_g, pool_w, pool_scale,
                 w_br_a, w_br_b, w_br_c, gate_b, w_out):
    b, l, d = x.shape
    proj = x @ w_in
    qa, ka, va, qkv_b, beta_l, a_l, gate_bo, u_c, gate_l = split_cols(proj, IN_SIZES)
    shp_a = (b, l, A_HEADS, A_HEAD_DIM)
    o_a = chunk_band_attention(qa.reshape(shp_a), ka.reshape(shp_a), va.reshape(shp_a), rel_bias)
    qkv_b = jax.nn.silu(causal_depthwise_conv(qkv_b, conv_w)).astype(jnp.float32)
    shp_b = (b, l, B_HEADS, B_HEAD_DIM)
    qb, kb, vb = [t.reshape(shp_b) for t in split_cols(qkv_b, (B_WIDTH, B_WIDTH, B_WIDTH))]
    beta = jax.nn.sigmoid(beta_l.astype(jnp.float32))
    g = -jnp.exp(a_log.astype(jnp.float32)) * jax.nn.softplus(a_l.astype(jnp.float32) + dt_bias)
    o_b = gated_delta_rule(l2_normalize(qb), l2_normalize(kb), vb, beta, g)
    o_b = o_b * lax.rsqrt(jnp.mean(o_b * o_b, axis=-1, keepdims=True) + RMS_EPS) * gdn_norm_g
    o_b = o_b * jax.nn.silu(gate_bo.reshape(shp_b).astype(jnp.float32))
    o_b = o_b.reshape(b, l, B_WIDTH).astype(x.dtype)
    o_c = multiscale_pool(u_c, pool_w, pool_scale)
    gates = jax.nn.sigmoid(gate_l.reshape(b, l, N_BRANCH, d).astype(jnp.float32) + gate_b).astype(x.dtype)
    y = (gates[:, :, 0] * (o_a @ w_br_a) + gates[:, :, 1] * (o_b @ w_br_b)
         + gates[:, :, 2] * (o_c @ w_br_c))
    return y @ w_out


def hierarchical_moe(x, wg, bg, we, be, w1, w3, w2):
    b, l, d = x.shape
    xt = x.reshape(-1, d)
    t = xt.shape[0]
    lg = (xt @ wg).astype(jnp.float32) + bg
    pg = jax.nn.softmax(lg, axis=-1)
    gsel = jnp.argmax(lg, axis=-1)
    p_top = jnp.take_along_axis(pg, gsel[:, None], axis=-1)
    le = ((xt @ we).astype(jnp.float32) + be).reshape(t, MOE_GROUPS, MOE_PER_GROUP)
    le = jnp.take_along_axis(le, gsel[:, None, None], axis=1)[:, 0]
    top_l, top_i = lax.top_k(le, MOE_TOPK)
    wts = jax.nn.softmax(top_l, axis=-1) * p_top
    eid = gsel[:, None] * MOE_PER_GROUP + top_i
    m = t * MOE_TOPK
    e_flat = eid.reshape(-1).astype(jnp.int32)
    w_flat = wts.reshape(-1)
    tok_flat = jnp.repeat(jnp.arange(t, dtype=jnp.int32), MOE_TOPK)
    order = jnp.argsort(e_flat)
    e_sorted = e_flat[order]
    counts = jnp.bincount(e_flat, length=N_EXPERTS)
    padded = (counts + MOE_BLOCK - 1) // MOE_BLOCK * MOE_BLOCK
    pad_end = jnp.cumsum(padded)
    pad_start = pad_end - padded
    start = jnp.cumsum(counts) - counts
    dest = pad_start[e_sorted] + jnp.arange(m) - start[e_sorted]
    n_blocks = -(-m // MOE_BLOCK) + N_EXPERTS
    cap = n_blocks * MOE_BLOCK
    row_tok = jnp.zeros((cap,), jnp.int32).at[dest].set(tok_flat[order])
    row_w = jnp.zeros((cap,), jnp.float32).at[dest].set(w_flat[order])
    block_e = jnp.minimum(jnp.searchsorted(pad_end, jnp.arange(n_blocks) * MOE_BLOCK, side='right'),
                          N_EXPERTS - 1)
    xs = xt[row_tok].reshape(n_blocks, MOE_BLOCK, d)

    def expert_block(args):
        xb, e = args
        hdn = jax.nn.silu(xb @ w1[e]) * (xb @ w3[e])
        return hdn @ w2[e]

    yb = lax.map(expert_block, (xs, block_e)).reshape(cap, d)
    y = jnp.zeros((t, d), jnp.float32).at[row_tok].add(yb.astype(jnp.float32) * row_w[:, None])
    return y.astype(x.dtype).reshape(b, l, d)


def setup_inputs(seed: int = 0) -> dict:
    key = jax.random.key(seed)
    ks = iter(jax.random.split(key, 40))

    def nrm(shape, scale):
        return jax.random.normal(next(ks), shape, jnp.float32) * scale

    def unif(shape, lo, hi):
        return jax.random.uniform(next(ks), shape, jnp.float32, lo, hi)

    L = DEPTH
    d = D_MODEL
    dt = jnp.exp(unif((L, B_HEADS), float(np.log(1e-3)), float(np.log(1e-1))))
    return {
        'x': nrm((BATCH, SEQ, d), 1.0),
        'w_in': nrm((L, d, D_IN), d ** -0.5),
        'attn_rel_bias': nrm((L, A_HEADS, A_N_REL), 0.2),
        'gdn_conv_w': nrm((L, B_CONV, 3 * B_WIDTH), B_CONV ** -0.5),
        'gdn_a_log': jnp.log(unif((L, B_HEADS), 1.0, 16.0)),
        'gdn_dt_bias': dt + jnp.log(-jnp.expm1(-dt)),
        'gdn_norm_g': 1.0 + nrm((L, B_HEAD_DIM), 0.02),
        'pool_w': nrm((L, C_GROUPS, C_GROUP_DIM, C_GROUP_DIM), C_GROUP_DIM ** -0.5),
        'pool_scale': 1.0 + nrm((L, C_WIDTH), 0.02),
        'w_branch_a': nrm((L, A_WIDTH, d), A_WIDTH ** -0.5),
        'w_branch_b': nrm((L, B_WIDTH, d), B_WIDTH ** -0.5),
        'w_branch_c': nrm((L, C_WIDTH, d), C_WIDTH ** -0.5),
        'gate_b': nrm((L, N_BRANCH, d), 0.02),
        'w_out': nrm((L, d, d), d ** -0.5 * DN_BETA),
        'ln1_g': 1.0 + nrm((L, d), 0.02),
        'ln1_b': nrm((L, d), 0.02),
        'router_group_w': nrm((L, d, MOE_GROUPS), d ** -0.5),
        'router_group_b': nrm((L, MOE_GROUPS), 0.01),
        'router_expert_w': nrm((L, d, N_EXPERTS), d ** -0.5),
        'router_expert_b': nrm((L, N_EXPERTS), 0.01),
        'moe_w1': nrm((L, N_EXPERTS, d, MOE_FF), d ** -0.5),
        'moe_w3': nrm((L, N_EXPERTS, d, MOE_FF), d ** -0.5),
        'moe_w2': nrm((L, N_EXPERTS, MOE_FF, d), MOE_FF ** -0.5 * DN_BETA),
        'ln2_g': 1.0 + nrm((L, d), 0.02),
        'ln2_b': nrm((L, d), 0.02),
    }


def reference(x, w_in, attn_rel_bias, gdn_conv_w, gdn_a_log, gdn_dt_bias, gdn_norm_g, pool_w,
              pool_scale, w_branch_a, w_branch_b, w_branch_c, gate_b, w_out, ln1_g, ln1_b,
              router_group_w, router_group_b, router_expert_w, router_expert_b,
              moe_w1, moe_w3, moe_w2, ln2_g, ln2_b):
    for i in range(DEPTH):
        mix = hybrid_mixer(x, w_in[i], attn_rel_bias[i], gdn_conv_w[i], gdn_a_log[i], gdn_dt_bias[i],
                           gdn_norm_g[i], pool_w[i], pool_scale[i], w_branch_a[i], w_branch_b[i],
                           w_branch_c[i], gate_b[i], w_out[i])
        x = layer_norm(DN_ALPHA * x + mix, ln1_g[i], ln1_b[i])
        ffn = hierarchical_moe(x, router_group_w[i], router_group_b[i], router_expert_w[i],
                               router_expert_b[i], moe_w1[i], moe_w3[i], moe_w2[i])
        x = layer_norm(DN_ALPHA * x + ffn, ln2_g[i], ln2_b[i])
    return x
```

```python
import numpy as np
import concourse.bass as bass
import concourse.mybir as mybir
from concourse.bass_utils import run_bass_kernel_spmd

F32 = mybir.dt.float32
BF16 = mybir.dt.bfloat16
I32 = mybir.dt.int32
AF = mybir.ActivationFunctionType
ALU = mybir.AluOpType
AX = mybir.AxisListType

D = 1024
SEQ = 2048
NT = SEQ // 128
DEPTH = 2
D_IN = 7176
NEG = -30000.0
C_QA, C_KA, C_VA, C_QKVB, C_BETA, C_DEC, C_GBO, C_UC, C_GATE = 0, 512, 1024, 1536, 3072, 3076, 3080, 3592, 4104
DN_ALPHA = (2 * DEPTH) ** 0.25
LN_EPS = 1e-5
RMS_EPS = 1e-6


class Sem:
    def __init__(self, h):
        self.h = h
        self.val = 0


class T:
    def __init__(self, h, name=""):
        self.h = h
        self.name = name
        self.w = {}
        self.r = {}
        self.excl = False

    def __getitem__(self, k):
        return self.h[k]


class KB:
    NDS = 12

    def __init__(self, nc):
        self.nc = nc
        self.eng = {"pe": nc.tensor, "act": nc.scalar, "dve": nc.vector, "pool": nc.gpsimd, "sp": nc.sync}
        self.esem = {e: Sem(nc.alloc_semaphore("s_" + e)) for e in self.eng}
        self.seen = {e: {} for e in self.eng}
        self.dsem = {q: [Sem(nc.alloc_semaphore("d_%s%d" % (q, i))) for i in range(self.NDS)]
                     for q in ("sp", "pool")}
        self.dctr = {q: 0 for q in self.dsem}
        self.n_ins = 0
        self._ps = None
        self._psi = 0

    @staticmethod
    def _deps(reads, writes):
        deps = {}
        for t in reads:
            for s, v in t.w.items():
                if deps.get(s, 0) < v:
                    deps[s] = v
            if t.excl:
                for s, v in t.r.items():
                    if deps.get(s, 0) < v:
                        deps[s] = v
        for t in writes:
            for s, v in t.w.items():
                if deps.get(s, 0) < v:
                    deps[s] = v
            for s, v in t.r.items():
                if deps.get(s, 0) < v:
                    deps[s] = v
        return deps

    def _wait(self, e, deps):
        seen = self.seen[e]
        for s, v in deps.items():
            if seen.get(s, 0) < v:
                self.eng[e].wait_ge(s.h, v)
                seen[s] = v

    def op(self, e, fn, reads=(), writes=()):
        deps = self._deps(reads, writes)
        s = self.esem[e]
        if e == "pe":
            deps.pop(s, None)
        self._wait(e, deps)
        ins = fn(self.eng[e])
        s.val += 1
        ins.then_inc(s.h, 1)
        for t in reads:
            t.r[s] = s.val
        for t in writes:
            t.w[s] = s.val
        self.n_ins += 1
        return ins

    def dma(self, q, out, in_, reads=(), writes=(), **kw):
        pool = self.dsem[q]
        s = pool[self.dctr[q] % len(pool)]
        self.dctr[q] += 1
        deps = self._deps(reads, writes)
        if deps.get(s, 0) < s.val:
            deps[s] = s.val
        self._wait(q, deps)
        ins = self.eng[q].dma_start(out=out, in_=in_, **kw)
        s.val += 16
        ins.then_inc(s.h, 16)
        for t in reads:
            t.r[s] = s.val
        for t in writes:
            t.w[s] = s.val
        self.n_ins += 1
        return ins

    def barrier(self):
        allsems = list(self.esem.values()) + [s for p in self.dsem.values() for s in p]
        for e in self.eng:
            self._wait(e, {s: s.val for s in allsems if s.val > 0})

    def sb(self, name, shape, dtype):
        return T(self.nc.alloc_sbuf_tensor(name, list(shape), dtype), name)

    def init_arena(self, nbytes):
        self.arena = self.nc.alloc_sbuf_tensor("arena", [128, nbytes // 2], BF16)
        self.arena_n = nbytes // 2
        self.arena_p = 0

    def al(self, name, shape, dtype):
        n = 1
        for d in shape[1:]:
            n *= d
        n2 = n * (2 if dtype in (F32, I32) else 1)
        n2 = (n2 + 15) // 16 * 16
        assert self.arena_p + n2 <= self.arena_n, ("arena overflow", name, self.arena_p, n2, self.arena_n)
        v = self.arena[0:shape[0], self.arena_p:self.arena_p + n2]
        self.arena_p += n2
        if dtype != BF16:
            v = v.bitcast(dtype)
        v = v[:, 0:n]
        if len(shape) == 3:
            v = v.rearrange("p (a b) -> p a b", a=shape[1])
        elif len(shape) == 4:
            v = v.rearrange("p (a b c) -> p a b c", a=shape[1], b=shape[2])
        return T(v, name)

    def mark(self):
        return self.arena_p

    def release(self, m):
        self.barrier()
        self.arena_p = m

    def init_psum(self):
        self._ps = [T(self.nc.alloc_psum_tensor("ps%d" % i, [128, 512], F32), "ps%d" % i) for i in range(8)]
        for t in self._ps:
            t.excl = True

    def psum(self):
        t = self._ps[self._psi % 8]
        self._psi += 1
        return t

    def mm(self, out_t, out, lhsT_t, lhsT, rhs_t, rhs, start=True, stop=True):
        return self.op("pe", lambda e: e.matmul(out, lhsT, rhs, start=start, stop=stop),
                       reads=[lhsT_t, rhs_t], writes=[out_t])

    def tr(self, out_t, out, in_t, in_, id_t, ident):
        return self.op("pe", lambda e: e.transpose(out, in_, ident), reads=[in_t, id_t], writes=[out_t])


class Ctx:
    pass


def dram_in(nc, name, shape, dtype=F32):
    return nc.dram_tensor(name, list(shape), dtype, kind="ExternalInput").ap()


def load_w(kb, c, dst_t, dst, src, kc, cols):
    per = max(1, WST // cols)
    k0 = 0
    while k0 < kc:
        kk = min(per, kc - k0)
        st = c.wst[c.wst_i % len(c.wst)]
        c.wst_i += 1
        sv = st[:, 0:kk * cols].rearrange("p (k c) -> p k c", k=kk)
        kb.dma("sp", sv, src[k0 * 128:(k0 + kk) * 128, :].rearrange("(k p) c -> p k c", p=128), writes=[st])
        d = dst[:, k0:k0 + kk, :]
        kb.op("pool", lambda e: e.tensor_copy(d, sv), reads=[st], writes=[dst_t])
        k0 += kk


def bc(ap, shape):
    return ap.to_broadcast(list(shape))


def build_xT(kb, c, x_t, x_dram, xT):
    m0 = kb.mark()
    xin = [kb.al("xin%d" % i, [128, 1024], F32) for i in range(2)]
    for n in range(NT):
        xi = xin[n % 2]
        kb.dma("sp", xi[:, :], x_dram[n * 128:(n + 1) * 128, :], reads=[x_t], writes=[xi])
        for half in range(2):
            bank = kb.psum()
            for j in range(4):
                k = half * 4 + j
                kb.tr(bank, bank[:, j * 128:(j + 1) * 128], xi, xi[:, k * 128:(k + 1) * 128], c.identf, c.identf[:, :])
            dst = xT[:, half * 4:half * 4 + 4, n * 128:(n + 1) * 128]
            src = bank[:, :].rearrange("p (k t) -> p k t", k=4)
            if half == 0:
                kb.op("act", lambda e: e.copy(dst, src), reads=[bank], writes=[xT])
            else:
                kb.op("dve", lambda e: e.tensor_copy(dst, src), reads=[bank], writes=[xT])
    kb.release(m0)


def attention_stage(kb, c, L, dr, w_in_l, xT, oaT):
    m0 = kb.mark()
    qT = kb.al("qT", [128, 4, SEQ], BF16)
    kT = kb.al("kT", [128, 4, SEQ], BF16)
    vpad = kb.al("vpad", [128, NT, 8, 128], BF16)
    PT = [kb.al("PT%d" % i, [128, 10 * 128], BF16) for i in range(2)]
    rden = [kb.al("rden%d" % i, [128, 128], F32) for i in range(2)]
    btab = kb.al("btab", [128, 8, 5, 128], BF16)
    bst = [kb.al("bst%d" % i, [128, 640], F32) for i in range(2)]
    wb = c.wb
    for h in range(8):
        b_ = bst[h % 2]
        kb.dma("sp", b_[:, :], dr["a_bias"][L, :, h, :], writes=[b_])
        bt = btab[:, h, :, :].rearrange("p j q -> p (j q)")
        kb.op("dve", lambda e: e.tensor_tensor(bt, b_[:, :], c.amask[:, :], ALU.add), reads=[b_, c.amask], writes=[btab])
    kb.op("pool", lambda e: e.memset(vpad[:, :, :, :], 0.0), writes=[vpad])
    for wi, (c0, dstT, scale) in enumerate(((C_QA, qT, 0.125), (C_KA, kT, 1.0))):
        w = wb[wi % 2]
        load_w(kb, c, w, w[:, :, :], w_in_l[:, c0:c0 + 512], 8, 512)
        for cc in range(4):
            for tb in range(4):
                bank = kb.psum()
                for k in range(8):
                    kb.mm(bank, bank[:, :], w, w[:, k, cc * 128:(cc + 1) * 128], xT, xT[:, k, tb * 512:(tb + 1) * 512],
                          start=(k == 0), stop=(k == 7))
                dst = dstT[:, cc, tb * 512:(tb + 1) * 512]
                kb.op("act", lambda e: e.activation(dst, bank[:, :], AF.Copy, scale=scale), reads=[bank], writes=[dstT])
    w = wb[0]
    load_w(kb, c, w, w[:, :, :], w_in_l[:, C_VA:C_VA + 512], 8, 512)
    for n in range(NT):
        bank = kb.psum()
        for k in range(8):
            kb.mm(bank, bank[:, :], xT, xT[:, k, n * 128:(n + 1) * 128], w, w[:, k, :], start=(k == 0), stop=(k == 7))
        bv = bank[:, :].rearrange("p (hp two d) -> p hp two d", two=2, d=64)
        vv = vpad[:, n, :, :].rearrange("p (hp two) d -> p hp two d", two=2)
        kb.op("dve", lambda e: e.tensor_copy(vv[:, :, 0, 0:64], bv[:, :, 0, :]), reads=[bank], writes=[vpad])
        kb.op("act", lambda e: e.copy(vv[:, :, 1, 64:128], bv[:, :, 1, :]), reads=[bank], writes=[vpad])
    it = 0
    for i in range(NT):
        js = [j for j in range(i - 4, i + 1) if j >= 0]
        for p in range(4):
            tiles = [(hh, j) for hh in (0, 1) for j in js]
            pt = PT[it % 2]
            rd = rden[it % 2]
            it += 1
            for g in range(0, len(tiles), 4):
                grp = tiles[g:g + 4]
                bank = kb.psum()
                for idx, (hh, j) in enumerate(grp):
                    h = 2 * p + hh
                    jj = j - (i - 4)
                    o = bank[:, idx * 128:(idx + 1) * 128]
                    kb.mm(bank, o, kT, kT[hh * 64:(hh + 1) * 64, p, j * 128:(j + 1) * 128],
                          qT, qT[hh * 64:(hh + 1) * 64, p, i * 128:(i + 1) * 128], start=True, stop=False)
                    kb.mm(bank, o, c.identb, c.identb[:, :], btab, btab[:, h, jj, :], start=False, stop=True)
                n_ = len(grp) * 128
                dst = pt[:, g * 128:g * 128 + n_]
                src = bank[:, 0:n_]
                kb.op("act", lambda e: e.activation(dst, src, AF.Exp), reads=[bank], writes=[pt])
            bank = kb.psum()
            for idx, (hh, j) in enumerate(tiles):
                kb.mm(bank, bank[:, 0:128], vpad, vpad[:, j, 2 * p + hh, :], pt, pt[:, idx * 128:(idx + 1) * 128],
                      start=(idx == 0), stop=(idx == len(tiles) - 1))
            for idx, (hh, j) in enumerate(tiles):
                kb.mm(bank, bank[:, 128:256], c.onespad, c.onespad[:, hh, :], pt, pt[:, idx * 128:(idx + 1) * 128],
                      start=(idx == 0), stop=(idx == len(tiles) - 1))
            kb.op("dve", lambda e: e.reciprocal(rd[:, :], bank[:, 128:256]), reads=[bank], writes=[rd])
            dst = oaT[:, p, i * 128:(i + 1) * 128]
            kb.op("dve", lambda e: e.tensor_tensor(dst, bank[:, 0:128], rd[:, :], ALU.mult), reads=[bank, rd], writes=[oaT])
    kb.release(m0)


class _Stop(Exception):
    pass


def _chk(k):
    import os
    if float(os.environ.get("GDN_STOP", "0")) == k:
        raise _Stop()


def gdn_stage(kb, c, L, dr, w_in_l, xT, obT):
    m0 = kb.mark()
    try:
        _gdn_stage(kb, c, L, dr, w_in_l, xT, obT)
    except _Stop:
        pass
    kb.release(m0)


def _gdn_stage(kb, c, L, dr, w_in_l, xT, obT):
    al = kb.al
    pre = al("g_pre", [128, 3 + SEQ], F32)
    acc = al("g_acc", [128, 512], F32)
    tmp = al("g_tmp", [128, 512], F32)
    qb = al("g_qb", [128, SEQ], BF16)
    kbf = al("g_kb", [128, SEQ], BF16)
    vb = al("g_vb", [128, SEQ], BF16)
    gsl = al("g_gsl", [128, NT, 128], BF16)
    u = al("g_u", [128, NT, 128], BF16)
    wT = al("g_wT", [128, SEQ], BF16)
    kd = al("g_kd", [128, NT, 128], BF16)
    qkT = al("g_qkT", [128, NT, 128], BF16)
    obr = al("g_obr", [128, NT, 128], BF16)
    wsm = [al("g_wsm%d" % i, [128, 8, 128], BF16) for i in range(2)]
    w8 = al("g_w8", [128, 8, 8], BF16)
    cw = al("g_cw", [128, 12, 4], F32)
    vec4 = al("g_vec4", [128, 8], F32)
    normg = al("g_normg", [128, 128], F32)
    bg = al("g_bg", [128, 128], F32)
    sm = {n_: al("g_" + n_, [128, 64], F32) for n_ in
          ("beta", "g", "gcum", "gtot", "egc", "ekd", "egl", "beg", "nbeta", "z", "az", "sp")}
    aexp = al("g_aexp", [128, 4], F32)
    ssum = al("g_ssum", [128, NT], F32)
    rstd = al("g_rstd", [128, NT], F32)
    rg = [al("g_rg%d" % i, [128, 4, 128], F32) for i in range(2)]
    Dm3 = [al("g_Dm%d" % i, [128, 4, 128], F32) for i in range(2)]
    Dm = [T(d_[:, :, :].rearrange("p j d -> p (j d)"), d_.name) for d_ in Dm3]
    for a_, b__ in zip(Dm, Dm3):
        a_.w, a_.r = b__.w, b__.r
    DT = [al("g_DT%d" % i, [128, 512], F32) for i in range(2)]
    P = [al("g_P%d" % i, [128, 4, 128], BF16) for i in range(2)]
    PTt = [al("g_PT%d" % i, [128, 4, 128], BF16) for i in range(2)]
    X = [al("g_X%d" % i, [128, 4, 256], BF16) for i in range(2)]
    wtok = rg
    MoT = [al("g_MoT%d" % i, [128, 4, 128], BF16) for i in range(2)]
    TdT = [al("g_TdT%d" % i, [128, 4, 128], BF16) for i in range(2)]
    Bx = [al("g_Bx%d" % i, [128, 4, 256], BF16) for i in range(2)]
    S = al("g_S", [128, 128], F32)
    Sb = al("g_Sb", [128, 128], BF16)
    vn = [al("g_vn%d" % i, [128, 128], BF16) for i in range(2)]
    t1 = [al("g_t1%d" % i, [128, 128], F32) for i in range(2)]
    onf = Dm3

    def v3(t_):
        return t_[:, :].rearrange("p (n h) -> p n h", h=4)

    kb.dma("sp", cw[:, :, :], dr["p_cw"][L], writes=[cw])
    kb.dma("sp", vec4[:, :], dr["p_vec4"][L], writes=[vec4])
    kb.dma("sp", normg[:, :], dr["p_normg"][L], writes=[normg])
    kb.op("pool", lambda e: e.memset(pre[:, 0:3], 0.0), writes=[pre])

    _chk(0.1)
    load_w(kb, c, w8, w8[:, :, :], w_in_l[:, C_BETA:C_BETA + 8], 8, 8)
    _chk(0.2)
    bank = kb.psum()
    for n in range(NT):
        for k in range(8):
            kb.mm(bank, bank[:, n * 8:(n + 1) * 8], xT, xT[:, k, n * 128:(n + 1) * 128], w8, w8[:, k, :],
                  start=(k == 0), stop=(k == 7))
    kb.op("act", lambda e: e.copy(bg[:, :], bank[:, 0:128]), reads=[bank], writes=[bg])
    _chk(0.3)
    bg3 = bg[:, :].rearrange("p (n c) -> p n c", c=8)
    beta, g, gcum, gtot, egc, ekd, egl, beg, nbeta = (sm[k_] for k_ in
                                                      ("beta", "g", "gcum", "gtot", "egc", "ekd", "egl", "beg", "nbeta"))
    z, az, sp = sm["z"], sm["az"], sm["sp"]
    kb.op("act", lambda e: e.activation(v3(beta), bg3[:, :, 0:4], AF.Sigmoid), reads=[bg], writes=[beta])
    kb.op("dve", lambda e: e.tensor_tensor(v3(z), bg3[:, :, 4:8], bc(vec4[:, 4:8].unsqueeze(1), [128, NT, 4]), ALU.add),
          reads=[bg, vec4], writes=[z])
    kb.op("dve", lambda e: e.scalar_tensor_tensor(az[:, :], z[:, :], -1.0, z[:, :], ALU.mult, ALU.max), reads=[z], writes=[az])
    _chk(0.4)
    kb.op("act", lambda e: e.activation(az[:, :], az[:, :], AF.Exp, scale=-1.0), reads=[az], writes=[az])
    kb.op("act", lambda e: e.activation(az[:, :], az[:, :], AF.Ln, bias=c.one1[:, 0:1]), reads=[az, c.one1], writes=[az])
    kb.op("dve", lambda e: e.scalar_tensor_tensor(sp[:, :], z[:, :], 0.0, az[:, :], ALU.max, ALU.add),
          reads=[z, az], writes=[sp])
    kb.op("act", lambda e: e.activation(aexp[:, :], vec4[:, 0:4], AF.Exp), reads=[vec4], writes=[aexp])
    kb.op("dve", lambda e: e.scalar_tensor_tensor(v3(g), v3(sp), -1.0, bc(aexp[:, :].unsqueeze(1), [128, NT, 4]),
                                                  ALU.mult, ALU.mult), reads=[sp, aexp], writes=[g])
    _chk(0.5)
    bank = kb.psum()
    kb.mm(bank, bank[:, 0:64], c.U, c.U[:, :], g, g[:, :])
    _chk(0.52)
    kb.mm(bank, bank[:, 64:128], c.onesf, c.onesf[:, :], g, g[:, :])
    _chk(0.54)
    kb.op("act", lambda e: e.copy(gcum[:, :], bank[:, 0:64]), reads=[bank], writes=[gcum])
    _chk(0.56)
    kb.op("dve", lambda e: e.tensor_copy(gtot[:, :], bank[:, 64:128]), reads=[bank], writes=[gtot])
    _chk(0.6)
    kb.op("act", lambda e: e.activation(egc[:, :], gcum[:, :], AF.Exp), reads=[gcum], writes=[egc])
    kb.op("dve", lambda e: e.tensor_tensor(ekd[:, :], gtot[:, :], gcum[:, :], ALU.subtract), reads=[gtot, gcum], writes=[ekd])
    kb.op("act", lambda e: e.activation(ekd[:, :], ekd[:, :], AF.Exp), reads=[ekd], writes=[ekd])
    kb.op("act", lambda e: e.activation(egl[:, :], gtot[:, :], AF.Exp), reads=[gtot], writes=[egl])
    kb.op("dve", lambda e: e.tensor_tensor(beg[:, :], beta[:, :], egc[:, :], ALU.mult), reads=[beta, egc], writes=[beg])
    kb.op("dve", lambda e: e.tensor_scalar(nbeta[:, :], beta[:, :], -1.0, None, ALU.mult), reads=[beta], writes=[nbeta])

    _chk(1)
    wi = 0
    for h in range(4):
        w = wsm[wi % 2]
        wi += 1
        load_w(kb, c, w, w[:, :, :], w_in_l[:, C_GBO + h * 128:C_GBO + (h + 1) * 128], 8, 128)
        for n0 in range(0, NT, 4):
            bank = kb.psum()
            for j in range(4):
                n = n0 + j
                for k in range(8):
                    kb.mm(bank, bank[:, j * 128:(j + 1) * 128], xT, xT[:, k, n * 128:(n + 1) * 128], w, w[:, k, :],
                          start=(k == 0), stop=(k == 7))
            dst = gsl[:, n0:n0 + 4, :]
            src = bank[:, :].rearrange("p (j d) -> p j d", j=4)
            kb.op("act", lambda e: e.activation(dst, src, AF.Silu), reads=[bank], writes=[gsl])
        _chk(2)
        for t, dstb in ((0, qb), (1, kbf), (2, vb)):
            w = wsm[wi % 2]
            wi += 1
            col = C_QKVB + t * 512 + h * 128
            load_w(kb, c, w, w[:, :, :], w_in_l[:, col:col + 128], 8, 128)
            for tb in range(4):
                bank = kb.psum()
                for k in range(8):
                    kb.mm(bank, bank[:, :], w, w[:, k, :], xT, xT[:, k, tb * 512:(tb + 1) * 512],
                          start=(k == 0), stop=(k == 7))
                dst = pre[:, 3 + tb * 512:3 + (tb + 1) * 512]
                kb.op("act", lambda e: e.copy(dst, bank[:, :]), reads=[bank], writes=[pre])
            cwv = cw[:, t * 4 + h, :]
            for hb in range(4):
                o0 = hb * 512
                kb.op("dve", lambda e: e.tensor_scalar(acc[:, :], pre[:, o0:o0 + 512], cwv[:, 0:1], None, ALU.mult),
                      reads=[pre, cw], writes=[acc])
                for j in range(1, 4):
                    kb.op("dve", lambda e: e.scalar_tensor_tensor(acc[:, :], pre[:, o0 + j:o0 + j + 512], cwv[:, j:j + 1],
                                                                  acc[:, :], ALU.mult, ALU.add),
                          reads=[pre, cw, acc], writes=[acc])
                kb.op("act", lambda e: e.activation(tmp[:, :], acc[:, :], AF.Silu), reads=[acc], writes=[tmp])
                dst = dstb[:, o0:o0 + 512]
                if t < 2:
                    kb.op("pool", lambda e: e.tensor_tensor(acc[:, :], tmp[:, :], tmp[:, :], ALU.mult), reads=[tmp], writes=[acc])
                    sc = (128.0 ** -0.5) if t == 0 else 1.0
                    bank = kb.psum()
                    kb.mm(bank, bank[:, :], c.onesf, c.onesf[:, :], acc, acc[:, :])
                    kb.op("act", lambda e: e.activation(acc[:, :], bank[:, :], AF.Sqrt, bias=c.eps6[:, 0:1]),
                          reads=[bank, c.eps6], writes=[acc])
                    kb.op("dve", lambda e: e.reciprocal(acc[:, :], acc[:, :]), reads=[acc], writes=[acc])
                    kb.op("dve", lambda e: e.scalar_tensor_tensor(dst, tmp[:, :], sc, acc[:, :], ALU.mult, ALU.mult),
                          reads=[tmp, acc], writes=[dstb])
                else:
                    kb.op("pool", lambda e: e.tensor_copy(dst, tmp[:, :]), reads=[tmp], writes=[dstb])
        _chk(3)
        for pair in range(2):
            tgs = [2 * pair, 2 * pair + 1]
            for s, tg in enumerate(tgs):
                n0 = tg * 4
                kb.op("dve", lambda e: e.tensor_tensor(rg[s][:, :, :], bc(c.V[:, :].unsqueeze(1), [128, 4, 128]),
                                                       bc(v3(g)[:, n0:n0 + 4, h:h + 1], [128, 4, 128]), ALU.mult),
                      reads=[c.V, g], writes=[rg[s]])
                bG = kb.psum()
                bGT = kb.psum()
                for j in range(4):
                    o = bG[:, j * 128:(j + 1) * 128]
                    kb.mm(bG, o, c.U, c.U[:, :], rg[s], rg[s][:, j, :], start=True, stop=False)
                    kb.mm(bG, o, c.identf, c.identf[:, :], c.mSL, c.mSL[:, :], start=False, stop=True)
                for j in range(4):
                    o = bGT[:, j * 128:(j + 1) * 128]
                    kb.mm(bGT, o, rg[s], rg[s][:, j, :], c.U, c.U[:, :], start=True, stop=False)
                    kb.mm(bGT, o, c.identf, c.identf[:, :], c.mTU, c.mTU[:, :], start=False, stop=True)
                kb.op("act", lambda e: e.activation(Dm[s][:, :], bG[:, :], AF.Exp), reads=[bG], writes=[Dm[s]])
                kb.op("act", lambda e: e.activation(DT[s][:, :], bGT[:, :], AF.Exp), reads=[bGT], writes=[DT[s]])
                bKK = kb.psum()
                bQK = kb.psum()
                for j in range(4):
                    ts_ = slice((n0 + j) * 128, (n0 + j + 1) * 128)
                    kb.mm(bKK, bKK[:, j * 128:(j + 1) * 128], kbf, kbf[:, ts_], kbf, kbf[:, ts_])
                    kb.mm(bQK, bQK[:, j * 128:(j + 1) * 128], kbf, kbf[:, ts_], qb, qb[:, ts_])
                kb.op("dve", lambda e: e.tensor_tensor(Dm[s][:, :], bKK[:, :], Dm[s][:, :], ALU.mult),
                      reads=[bKK, Dm[s]], writes=[Dm[s]])
                kb.op("dve", lambda e: e.tensor_tensor(P[s][:, :, :], Dm[s][:, :].rearrange("p (j d) -> p j d", j=4),
                                                        bc(v3(nbeta)[:, n0:n0 + 4, h:h + 1], [128, 4, 128]), ALU.mult),
                      reads=[Dm[s], nbeta], writes=[P[s]])
                kb.op("dve", lambda e: e.tensor_tensor(qkT[:, n0:n0 + 4, :], bQK[:, :].rearrange("p (j d) -> p j d", j=4),
                                                       DT[s][:, :].rearrange("p (j d) -> p j d", j=4), ALU.mult),
                      reads=[bQK, DT[s]], writes=[qkT])
                bT = kb.psum()
                bTv = bT[:, :].bitcast(BF16)
                for j in range(4):
                    kb.tr(bT, bTv[:, j * 128:(j + 1) * 128], P[s], P[s][:, j, :], c.identb, c.identb[:, :])
                mtv = bTv[:, 0:512].rearrange("p (j d) -> p j d", j=4)
                bd_b = bc(c.bd[:, :].unsqueeze(1), [128, 4, 128])
                obd_b = bc(c.obd[:, :].unsqueeze(1), [128, 4, 128])
                kb.op("dve", lambda e: e.tensor_tensor(PTt[s][:, :, :], mtv, bd_b, ALU.mult), reads=[bT, c.bd], writes=[PTt[s]])
                kb.op("dve", lambda e: e.tensor_tensor(MoT[s][:, :, :], mtv, obd_b, ALU.mult), reads=[bT, c.obd], writes=[MoT[s]])
                kb.op("dve", lambda e: e.tensor_tensor(P[s][:, :, :], P[s][:, :, :], bd_b, ALU.mult), reads=[P[s], c.bd], writes=[P[s]])
                kb.op("dve", lambda e: e.tensor_tensor(TdT[s][:, :, :], PTt[s][:, :, :], bc(c.identf[:, :].unsqueeze(1), [128, 4, 128]), ALU.add),
                      reads=[PTt[s], c.identf], writes=[TdT[s]])
                bX = kb.psum()
                bXv = bX[:, :].bitcast(BF16)
                for j in range(4):
                    ts_ = slice((n0 + j) * 128, (n0 + j + 1) * 128)
                    kb.tr(bX, bXv[:, j * 128:(j + 1) * 128], vb, vb[:, ts_], c.identb, c.identb[:, :])
                    kb.tr(bX, bXv[:, 512 + j * 128:512 + (j + 1) * 128], kbf, kbf[:, ts_], c.identb, c.identb[:, :])
                vps = bXv[:, 0:512].rearrange("p (j d) -> p j d", j=4)
                kps = bXv[:, 512:1024].rearrange("p (j d) -> p j d", j=4)
                kb.op("dve", lambda e: e.tensor_tensor(Bx[s][:, :, 0:128], vps, bc(v3(beta)[:, n0:n0 + 4, h:h + 1], [128, 4, 128]),
                                                       ALU.mult), reads=[bX, beta], writes=[Bx[s]])
                kb.op("dve", lambda e: e.tensor_tensor(Bx[s][:, :, 128:256], kps, bc(v3(beg)[:, n0:n0 + 4, h:h + 1], [128, 4, 128]),
                                                       ALU.mult), reads=[bX, beg], writes=[Bx[s]])
                kb.op("dve", lambda e: e.tensor_tensor(kd[:, n0:n0 + 4, :], kps, bc(v3(ekd)[:, n0:n0 + 4, h:h + 1], [128, 4, 128]),
                                                       ALU.mult), reads=[bX, ekd], writes=[kd])
            _chk(4)
            for lev in range(4):
                for s, tg in enumerate(tgs):
                    bP = kb.psum()
                    bPT = kb.psum()
                    for j in range(4):
                        kb.mm(bP, bP[:, j * 128:(j + 1) * 128], PTt[s], PTt[s][:, j, :], P[s], P[s][:, j, :])
                    for j in range(4):
                        kb.mm(bPT, bPT[:, j * 128:(j + 1) * 128], P[s], P[s][:, j, :], PTt[s], PTt[s][:, j, :])
                    kb.op("act", lambda e: e.copy(P[s][:, :, :], bP[:, :].rearrange("p (j d) -> p j d", j=4)),
                          reads=[bP], writes=[P[s]])
                    kb.op("act", lambda e: e.copy(PTt[s][:, :, :], bPT[:, :].rearrange("p (j d) -> p j d", j=4)),
                          reads=[bPT], writes=[PTt[s]])
                    bU = kb.psum()
                    for j in range(4):
                        kb.mm(bU, bU[:, j * 128:(j + 1) * 128], P[s], P[s][:, j, :], TdT[s], TdT[s][:, j, :])
                    kb.op("dve", lambda e: e.tensor_tensor(TdT[s][:, :, :], TdT[s][:, :, :], bU[:, :].rearrange("p (j d) -> p j d", j=4), ALU.add),
                          reads=[TdT[s], bU], writes=[TdT[s]])
            for it in range(4):
                for s, tg in enumerate(tgs):
                    n0 = tg * 4
                    if it > 0:
                        bY = [kb.psum(), kb.psum()]
                        for j in range(4):
                            kb.mm(bY[j // 2], bY[j // 2][:, (j % 2) * 256:(j % 2 + 1) * 256], MoT[s], MoT[s][:, j, :], X[s], X[s][:, j, :])
                        for b_ in range(2):
                            yv = bY[b_][:, :].rearrange("p (j d) -> p j d", j=2)
                            kb.op("dve", lambda e: e.tensor_tensor(X[s][:, 2 * b_:2 * b_ + 2, :], Bx[s][:, 2 * b_:2 * b_ + 2, :], yv, ALU.add),
                                  reads=[Bx[s], bY[b_]], writes=[X[s]])
                    src_t = Bx[s] if it == 0 else X[s]
                    bA = [kb.psum(), kb.psum()]
                    for j in range(4):
                        kb.mm(bA[j // 2], bA[j // 2][:, (j % 2) * 256:(j % 2 + 1) * 256], TdT[s], TdT[s][:, j, :], src_t, src_t[:, j, :])
                    for b_ in range(2):
                        av = bA[b_][:, :].rearrange("p (j d) -> p j d", j=2)
                        if it < 3:
                            kb.op("act", lambda e: e.copy(X[s][:, 2 * b_:2 * b_ + 2, :], av), reads=[bA[b_]], writes=[X[s]])
                        else:
                            kb.op("act", lambda e: e.copy(u[:, n0 + 2 * b_:n0 + 2 * b_ + 2, :], av[:, :, 0:128]), reads=[bA[b_]], writes=[u])
                            kb.op("dve", lambda e: e.tensor_copy(wtok[s][:, 2 * b_:2 * b_ + 2, :], av[:, :, 128:256]), reads=[bA[b_]], writes=[wtok[s]])
            _chk(5)
            for s, tg in enumerate(tgs):
                n0 = tg * 4
                bW = kb.psum()
                for j in range(4):
                    kb.tr(bW, bW[:, j * 128:(j + 1) * 128], wtok[s], wtok[s][:, j, :], c.identf, c.identf[:, :])
                kb.op("act", lambda e: e.copy(wT[:, n0 * 128:(n0 + 4) * 128], bW[:, :]), reads=[bW], writes=[wT])
        _chk(6)
        kb.op("pool", lambda e: e.memset(S[:, :], 0.0), writes=[S])
        kb.op("pool", lambda e: e.memset(Sb[:, :], 0.0), writes=[Sb])
        for n in range(NT):
            ts_ = slice(n * 128, (n + 1) * 128)
            col = n * 4 + h
            b1 = kb.psum()
            kb.mm(b1, b1[:, 0:128], wT, wT[:, ts_], Sb, Sb[:, :])
            v_ = vn[n % 2]
            kb.op("dve", lambda e: e.tensor_tensor(v_[:, :], u[:, n, :], b1[:, 0:128], ALU.subtract), reads=[u, b1], writes=[v_])
            b2 = kb.psum()
            kb.mm(b2, b2[:, 0:128], qb, qb[:, ts_], Sb, Sb[:, :])
            kb.mm(b2, b2[:, 128:256], qkT, qkT[:, n, :], v_, v_[:, :])
            t_ = t1[n % 2]
            kb.op("dve", lambda e: e.tensor_scalar(t_[:, :], b2[:, 0:128], egc[:, col:col + 1], None, ALU.mult),
                  reads=[b2, egc], writes=[t_])
            kb.op("dve", lambda e: e.tensor_tensor(obr[:, n, :], t_[:, :], b2[:, 128:256], ALU.add), reads=[t_, b2], writes=[obr])
            if n < NT - 1:
                b3 = kb.psum()
                kb.mm(b3, b3[:, 0:128], kd, kd[:, n, :], v_, v_[:, :])
                kb.op("dve", lambda e: e.scalar_tensor_tensor(S[:, :], S[:, :], egl[:, col:col + 1], b3[:, 0:128], ALU.mult, ALU.add),
                      reads=[S, egl, b3], writes=[S])
                kb.op("act", lambda e: e.copy(Sb[:, :], S[:, :]), reads=[S], writes=[Sb])
        _chk(7)
        for g4 in range(4):
            n0 = g4 * 4
            o_ = onf[g4 % 2]
            kb.op("dve", lambda e: e.tensor_tensor(o_[:, :, :], obr[:, n0:n0 + 4, :], obr[:, n0:n0 + 4, :], ALU.mult),
                  reads=[obr], writes=[o_])
            kb.op("dve", lambda e: e.tensor_reduce(ssum[:, n0:n0 + 4], o_[:, :, :], AX.X, ALU.add), reads=[o_], writes=[ssum])
        kb.op("act", lambda e: e.activation(rstd[:, :], ssum[:, :], AF.Sqrt, bias=c.eps6[:, 0:1], scale=1.0 / 128.0),
              reads=[ssum, c.eps6], writes=[rstd])
        kb.op("dve", lambda e: e.reciprocal(rstd[:, :], rstd[:, :]), reads=[rstd], writes=[rstd])
        for g4 in range(4):
            n0 = g4 * 4
            o_ = onf[g4 % 2]
            kb.op("dve", lambda e: e.tensor_tensor(o_[:, :, :], obr[:, n0:n0 + 4, :], bc(rstd[:, n0:n0 + 4].unsqueeze(2), [128, 4, 128]),
                                                   ALU.mult), reads=[obr, rstd], writes=[o_])
            kb.op("dve", lambda e: e.tensor_tensor(o_[:, :, :], o_[:, :, :], bc(normg[:, :].unsqueeze(1), [128, 4, 128]), ALU.mult),
                  reads=[o_, normg], writes=[o_])
            kb.op("dve", lambda e: e.tensor_tensor(o_[:, :, :], o_[:, :, :], gsl[:, n0:n0 + 4, :], ALU.mult),
                  reads=[o_, gsl], writes=[o_])
            bank = kb.psum()
            for j in range(4):
                kb.tr(bank, bank[:, j * 128:(j + 1) * 128], o_, o_[:, j, :], c.identf, c.identf[:, :])
            kb.op("act", lambda e: e.copy(obT[:, h, n0 * 128:(n0 + 4) * 128], bank[:, :]), reads=[bank], writes=[obT])


def pool_stage(kb, c, L, dr, w_in_l, xT, ocT):
    m0 = kb.mark()
    al = kb.al
    PAD = 16
    up = al("p_up", [128, PAD + SEQ], F32)
    sa = al("p_sa", [128, PAD + SEQ], F32)
    sbb = al("p_sb", [128, PAD + SEQ], F32)
    wu = [al("p_wu%d" % i, [128, 8, 128], BF16) for i in range(2)]
    pw = [al("p_pw%d" % i, [128, 1, 128], BF16) for i in range(2)]
    pl = al("p_pl", [128, SEQ], BF16)
    psc = al("p_psc", [128, 4], F32)
    invc = al("p_invc", [128, 4, 16], F32)
    t16 = al("p_t16", [128, 16], F32)
    kb.dma("sp", psc[:, :], dr["p_pscale"][L], writes=[psc])
    kb.dma("sp", invc[:, :, :], dr["c_invc"], writes=[invc])
    for b_ in (up, sa, sbb):
        kb.op("pool", lambda e: e.memset(b_[:, 0:PAD], 0.0), writes=[b_])
    for gi in range(4):
        win = 2 ** (gi + 1)
        w = wu[gi % 2]
        load_w(kb, c, w, w[:, :, :], w_in_l[:, C_UC + gi * 128:C_UC + (gi + 1) * 128], 8, 128)
        for tb in range(4):
            bank = kb.psum()
            for k in range(8):
                kb.mm(bank, bank[:, :], w, w[:, k, :], xT, xT[:, k, tb * 512:(tb + 1) * 512], start=(k == 0), stop=(k == 7))
            dst = up[:, PAD + tb * 512:PAD + (tb + 1) * 512]
            kb.op("act", lambda e: e.copy(dst, bank[:, :]), reads=[bank], writes=[up])
        src = up
        shift = 1
        bufs = [sa, sbb]
        for st in range(gi + 1):
            dst_t = bufs[st % 2]
            eng = "dve" if st % 2 == 0 else "pool"
            a_ = src[:, PAD:PAD + SEQ]
            b_ = src[:, PAD - shift:PAD - shift + SEQ]
            d_ = dst_t[:, PAD:PAD + SEQ]
            kb.op(eng, lambda e: e.tensor_tensor(d_, a_, b_, ALU.add), reads=[src], writes=[dst_t])
            src = dst_t
            shift *= 2
        kb.op("dve", lambda e: e.scalar_tensor_tensor(pl[:, :], src[:, PAD:PAD + SEQ], 1.0 / win, up[:, PAD:PAD + SEQ],
                                                      ALU.mult, ALU.subtract), reads=[src, up], writes=[pl])
        kb.op("dve", lambda e: e.tensor_tensor(t16[:, :], src[:, PAD:PAD + 16], invc[:, gi, :], ALU.mult),
              reads=[src, invc], writes=[t16])
        kb.op("dve", lambda e: e.tensor_tensor(pl[:, 0:16], t16[:, :], up[:, PAD:PAD + 16], ALU.subtract),
              reads=[t16, up, pl], writes=[pl])
        w2 = pw[gi % 2]
        load_w(kb, c, w2, w2[:, :, :], dr["pool_w"][L, gi], 1, 128)
        for tb in range(4):
            bank = kb.psum()
            kb.mm(bank, bank[:, :], w2, w2[:, 0, :], pl, pl[:, tb * 512:(tb + 1) * 512])
            dst = ocT[:, gi, tb * 512:(tb + 1) * 512]
            kb.op("act", lambda e: e.activation(dst, bank[:, :], AF.Identity, scale=psc[:, gi:gi + 1]), reads=[bank, psc], writes=[ocT])
    kb.release(m0)


def layer_norm(kb, c, r_t, r_, g_t, b_t, ws):
    kb.op("dve", lambda e: e.bn_stats(ws.stats[:, 0, :], r_[:, 0:512]), reads=[r_t], writes=[ws.stats])
    kb.op("dve", lambda e: e.bn_stats(ws.stats[:, 1, :], r_[:, 512:1024]), reads=[r_t], writes=[ws.stats])
    kb.op("dve", lambda e: e.bn_aggr(ws.mv[:, :], ws.stats[:, :, :].rearrange("p a b -> p (a b)")), reads=[ws.stats], writes=[ws.mv])
    kb.op("act", lambda e: e.activation(ws.sd[:, :], ws.mv[:, 1:2], AF.Sqrt, bias=c.eps5[:, 0:1]), reads=[ws.mv, c.eps5], writes=[ws.sd])
    kb.op("dve", lambda e: e.reciprocal(ws.sd[:, :], ws.sd[:, :]), reads=[ws.sd], writes=[ws.sd])
    kb.op("dve", lambda e: e.tensor_scalar(r_, r_, ws.mv[:, 0:1], ws.sd[:, 0:1], ALU.subtract, ALU.mult),
          reads=[r_t, ws.mv, ws.sd], writes=[r_t])
    kb.op("pool", lambda e: e.tensor_tensor(r_, r_, g_t[:, :], ALU.mult), reads=[r_t, g_t], writes=[r_t])
    kb.op("pool", lambda e: e.tensor_tensor(r_, r_, b_t[:, :], ALU.add), reads=[r_t, b_t], writes=[r_t])


def ln_ws(kb, pfx):
    ws = Ctx()
    ws.stats = kb.al(pfx + "_st", [128, 2, 6], F32)
    ws.mv = kb.al(pfx + "_mv", [128, 2], F32)
    ws.sd = kb.al(pfx + "_sd", [128, 1], F32)
    return ws


def routing(kb, c, bank, rb, wout_t, wout, ws):
    lgle, mx, ohg, eg, se, tmp48, lsel, top8, sel, ex, den, w8 = (
        ws.lgle, ws.mx, ws.ohg, ws.eg, ws.se, ws.tmp48, ws.lsel, ws.top8, ws.sel, ws.ex, ws.den, ws.w8)
    op = kb.op
    op("dve", lambda e: e.tensor_tensor(lgle[:, :], bank[:, 0:36], rb[:, :], ALU.add), reads=[bank, rb], writes=[lgle])
    op("dve", lambda e: e.tensor_reduce(mx[:, 0:1], lgle[:, 0:4], AX.X, ALU.max), reads=[lgle], writes=[mx])
    op("dve", lambda e: e.tensor_scalar(ohg[:, :], lgle[:, 0:4], mx[:, 0:1], None, ALU.is_equal), reads=[lgle, mx], writes=[ohg])
    op("dve", lambda e: e.tensor_scalar(mx[:, 1:2], mx[:, 0:1], -1.0, None, ALU.mult), reads=[mx], writes=[mx])
    op("act", lambda e: e.activation(eg[:, :], lgle[:, 0:4], AF.Exp, bias=mx[:, 1:2]), reads=[lgle, mx], writes=[eg])
    op("dve", lambda e: e.tensor_reduce(se[:, 0:1], eg[:, :], AX.X, ALU.add), reads=[eg], writes=[se])
    op("dve", lambda e: e.reciprocal(se[:, 1:2], se[:, 0:1]), reads=[se], writes=[se])
    le3 = lgle[:, 4:36].rearrange("p (g e) -> p g e", g=4)
    op("dve", lambda e: e.tensor_tensor(tmp48[:, :, :], le3, bc(ohg[:, :].unsqueeze(2), [128, 4, 8]), ALU.mult),
       reads=[lgle, ohg], writes=[tmp48])
    op("dve", lambda e: e.tensor_reduce(lsel[:, :], tmp48[:, :, :].rearrange("p g e -> p e g"), AX.X, ALU.add),
       reads=[tmp48], writes=[lsel])
    op("dve", lambda e: e.max(top8[:, :], lsel[:, :]), reads=[lsel], writes=[top8])
    op("dve", lambda e: e.tensor_scalar(sel[:, :], lsel[:, :], top8[:, 1:2], None, ALU.is_ge), reads=[lsel, top8], writes=[sel])
    op("dve", lambda e: e.tensor_scalar(mx[:, 2:3], top8[:, 0:1], -1.0, None, ALU.mult), reads=[top8], writes=[mx])
    op("act", lambda e: e.activation(ex[:, :], lsel[:, :], AF.Exp, bias=mx[:, 2:3]), reads=[lsel, mx], writes=[ex])
    op("dve", lambda e: e.tensor_tensor(ex[:, :], ex[:, :], sel[:, :], ALU.mult), reads=[ex, sel], writes=[ex])
    op("dve", lambda e: e.tensor_reduce(den[:, 0:1], ex[:, :], AX.X, ALU.add), reads=[ex], writes=[den])
    op("dve", lambda e: e.reciprocal(den[:, 1:2], den[:, 0:1]), reads=[den], writes=[den])
    op("dve", lambda e: e.tensor_tensor(den[:, 2:3], den[:, 1:2], se[:, 1:2], ALU.mult), reads=[den, se], writes=[den])
    op("dve", lambda e: e.tensor_scalar(w8[:, :], ex[:, :], den[:, 2:3], None, ALU.mult), reads=[ex, den], writes=[w8])
    op("dve", lambda e: e.tensor_tensor(wout, bc(ohg[:, :].unsqueeze(2), [128, 4, 8]), bc(w8[:, :].unsqueeze(1), [128, 4, 8]), ALU.mult),
       reads=[ohg, w8], writes=[wout_t])


def merge_stage(kb, c, L, dr, w_in_l, xT, oT, x_t, x_dram, x1_t, x1_dram, wts):
    m0 = kb.mark()
    al = kb.al
    yT = al("m_yT", [128, 8, SEQ], BF16)
    wg = [al("m_wg%d" % i, [128, 8, 128], BF16) for i in range(3)]
    wbr = [al("m_wbr%d" % i, [128, 4, 128], BF16) for i in range(3)]
    gs = [al("m_gs%d" % i, [128, 512], F32) for i in range(2)]
    ya = [al("m_ya%d" % i, [128, 512], F32) for i in range(2)]
    tm = [al("m_tm%d" % i, [128, 512], F32) for i in range(2)]
    gateb = al("m_gateb", [128, 24], F32)
    kb.dma("sp", gateb[:, :], dr["p_gateb"][L], writes=[gateb])
    brw = (dr["w_branch_a"][L], dr["w_branch_b"][L], dr["w_branch_c"][L])
    it = 0
    for dc in range(8):
        for br in range(3):
            col = C_GATE + br * 1024 + dc * 128
            load_w(kb, c, wg[br], wg[br][:, :, :], w_in_l[:, col:col + 128], 8, 128)
            load_w(kb, c, wbr[br], wbr[br][:, :, :], brw[br][:, dc * 128:(dc + 1) * 128], 4, 128)
        for tb in range(4):
            tsl = slice(tb * 512, (tb + 1) * 512)
            y_ = ya[tb % 2]
            for br in range(3):
                bg_ = kb.psum()
                for k in range(8):
                    kb.mm(bg_, bg_[:, :], wg[br], wg[br][:, k, :], xT, xT[:, k, tsl], start=(k == 0), stop=(k == 7))
                g_ = gs[it % 2]
                t_ = tm[it % 2]
                it += 1
                kb.op("act", lambda e: e.activation(g_[:, :], bg_[:, :], AF.Sigmoid, bias=gateb[:, br * 8 + dc:br * 8 + dc + 1]),
                      reads=[bg_, gateb], writes=[g_])
                bb_ = kb.psum()
                for k in range(4):
                    kb.mm(bb_, bb_[:, :], wbr[br], wbr[br][:, k, :], oT[br], oT[br][:, k, tsl], start=(k == 0), stop=(k == 3))
                if br == 0:
                    kb.op("dve", lambda e: e.tensor_tensor(y_[:, :], g_[:, :], bb_[:, :], ALU.mult), reads=[g_, bb_], writes=[y_])
                elif br == 1:
                    kb.op("dve", lambda e: e.tensor_tensor(t_[:, :], g_[:, :], bb_[:, :], ALU.mult), reads=[g_, bb_], writes=[t_])
                    kb.op("pool", lambda e: e.tensor_tensor(y_[:, :], y_[:, :], t_[:, :], ALU.add), reads=[y_, t_], writes=[y_])
                else:
                    kb.op("dve", lambda e: e.tensor_tensor(t_[:, :], g_[:, :], bb_[:, :], ALU.mult), reads=[g_, bb_], writes=[t_])
                    kb.op("pool", lambda e: e.tensor_tensor(yT[:, dc, tsl], y_[:, :], t_[:, :], ALU.add), reads=[y_, t_], writes=[yT])
    wo = c.wb
    load_w(kb, c, wo[0], wo[0][:, :, :], dr["w_out"][L][:, 0:512], 8, 512)
    load_w(kb, c, wo[1], wo[1][:, :, :], dr["w_out"][L][:, 512:1024], 8, 512)
    lng = al("m_lng", [128, 1024], F32)
    lnb = al("m_lnb", [128, 1024], F32)
    kb.dma("sp", lng[:, :], dr["p_ln"][L, 0], writes=[lng])
    kb.dma("sp", lnb[:, :], dr["p_ln"][L, 1], writes=[lnb])
    rw = al("m_rw", [128, 8, 36], F32)
    rb = al("m_rb", [128, 36], F32)
    kb.dma("sp", rw[:, :, :], dr["p_rw"][L], writes=[rw])
    kb.dma("sp", rb[:, :], dr["p_rb"][L], writes=[rb])
    xin = [al("m_xin%d" % i, [128, 1024], F32) for i in range(2)]
    rr = [al("m_r%d" % i, [128, 1024], F32) for i in range(2)]
    x1Tf = al("m_x1Tf", [128, 8, 128], F32)
    lws = ln_ws(kb, "m_ln")
    rws = Ctx()
    rws.lgle = al("r_lgle", [128, 36], F32)
    rws.mx = al("r_mx", [128, 4], F32)
    rws.ohg = al("r_ohg", [128, 4], F32)
    rws.eg = al("r_eg", [128, 4], F32)
    rws.se = al("r_se", [128, 2], F32)
    rws.tmp48 = al("r_tmp48", [128, 4, 8], F32)
    rws.lsel = al("r_lsel", [128, 8], F32)
    rws.top8 = al("r_top8", [128, 8], F32)
    rws.sel = al("r_sel", [128, 8], F32)
    rws.ex = al("r_ex", [128, 8], F32)
    rws.den = al("r_den", [128, 4], F32)
    rws.w8 = al("r_w8", [128, 8], F32)
    for n in range(NT):
        xi = xin[n % 2]
        r_ = rr[n % 2]
        kb.dma("sp", xi[:, :], x_dram[n * 128:(n + 1) * 128, :], reads=[x_t], writes=[xi])
        for half in range(2):
            bank = kb.psum()
            for k in range(8):
                kb.mm(bank, bank[:, :], yT, yT[:, k, n * 128:(n + 1) * 128], wo[half], wo[half][:, k, :], start=(k == 0), stop=(k == 7))
            hs = slice(half * 512, (half + 1) * 512)
            kb.op("dve", lambda e: e.scalar_tensor_tensor(r_[:, hs], xi[:, hs], DN_ALPHA, bank[:, :], ALU.mult, ALU.add),
                  reads=[xi, bank], writes=[r_])
        layer_norm(kb, c, r_, r_[:, :], lng, lnb, lws)
        kb.dma("sp", x1_dram[n * 128:(n + 1) * 128, :], r_[:, :], reads=[r_], writes=[x1_t])
        for half in range(2):
            bank = kb.psum()
            for j in range(4):
                k = half * 4 + j
                kb.tr(bank, bank[:, j * 128:(j + 1) * 128], r_, r_[:, k * 128:(k + 1) * 128], c.identf, c.identf[:, :])
            src = bank[:, :].rearrange("p (k t) -> p k t", k=4)
            kb.op("act", lambda e: e.copy(xT[:, half * 4:half * 4 + 4, n * 128:(n + 1) * 128], src), reads=[bank], writes=[xT])
            kb.op("dve", lambda e: e.tensor_copy(x1Tf[:, half * 4:half * 4 + 4, :], src), reads=[bank], writes=[x1Tf])
        bank = kb.psum()
        for k in range(8):
            kb.mm(bank, bank[:, 0:36], x1Tf, x1Tf[:, k, :], rw, rw[:, k, :], start=(k == 0), stop=(k == 7))
        routing(kb, c, bank, rb, wts, wts[:, n, :].rearrange("p (g e) -> p g e", g=4), rws)
    kb.release(m0)


def moe_stage(kb, c, L, dr, xT, wts, x1_t, x1_dram, o_t, out_dram, n_exp=32):
    m0 = kb.mark()
    al = kb.al
    acc = al("e_acc", [128, 8, 1024], F32)
    w1b = [al("e_w1b%d" % i, [128, 8, 512], BF16) for i in range(2)]
    w3b = [al("e_w3b%d" % i, [128, 8, 512], BF16) for i in range(2)]
    w2b = [al("e_w2b%d" % i, [128, 4, 1024], BF16) for i in range(2)]
    hT = [al("e_hT%d" % i, [128, 4, 512], BF16) for i in range(2)]
    sl = [al("e_sl%d" % i, [128, 512], F32) for i in range(2)]
    lng = al("e_lng", [128, 1024], F32)
    lnb = al("e_lnb", [128, 1024], F32)
    kb.dma("sp", lng[:, :], dr["p_ln"][L, 2], writes=[lng])
    kb.dma("sp", lnb[:, :], dr["p_ln"][L, 3], writes=[lnb])
    xin = [al("e_xin%d" % i, [128, 1024], F32) for i in range(2)]
    lws = ln_ws(kb, "e_ln")
    it = 0
    for hf in range(2):
        for ex in range(n_exp):
            wi = ex % 2
            load_w(kb, c, w1b[wi], w1b[wi][:, :, :], dr["moe_w1"][L, ex], 8, 512)
            load_w(kb, c, w3b[wi], w3b[wi][:, :, :], dr["moe_w3"][L, ex], 8, 512)
            load_w(kb, c, w2b[wi], w2b[wi][:, :, :], dr["moe_w2"][L, ex], 4, 1024)
            for tb2 in range(2):
                tok0 = hf * 1024 + tb2 * 512
                h_ = hT[it % 2]
                it += 1
                for fc in range(4):
                    b1 = kb.psum()
                    for k in range(8):
                        kb.mm(b1, b1[:, :], w1b[wi], w1b[wi][:, k, fc * 128:(fc + 1) * 128], xT, xT[:, k, tok0:tok0 + 512],
                              start=(k == 0), stop=(k == 7))
                    b3 = kb.psum()
                    for k in range(8):
                        kb.mm(b3, b3[:, :], w3b[wi], w3b[wi][:, k, fc * 128:(fc + 1) * 128], xT, xT[:, k, tok0:tok0 + 512],
                              start=(k == 0), stop=(k == 7))
                    s_ = sl[fc % 2]
                    kb.op("act", lambda e: e.activation(s_[:, :], b1[:, :], AF.Silu), reads=[b1], writes=[s_])
                    kb.op("dve", lambda e: e.tensor_tensor(h_[:, fc, :], s_[:, :], b3[:, :], ALU.mult), reads=[s_, b3], writes=[h_])
                for sub in range(4):
                    nl = tb2 * 4 + sub
                    n = hf * 8 + nl
                    for half in range(2):
                        b_ = kb.psum()
                        for fc in range(4):
                            kb.mm(b_, b_[:, :], h_, h_[:, fc, sub * 128:(sub + 1) * 128], w2b[wi], w2b[wi][:, fc, half * 512:(half + 1) * 512],
                                  start=(fc == 0), stop=(fc == 3))
                        a_ = acc[:, nl, half * 512:(half + 1) * 512]
                        wcol = wts[:, n, ex:ex + 1]
                        if ex == 0:
                            kb.op("dve", lambda e: e.tensor_scalar(a_, b_[:, :], wcol, None, ALU.mult), reads=[b_, wts], writes=[acc])
                        else:
                            kb.op("dve", lambda e: e.scalar_tensor_tensor(a_, b_[:, :], wcol, a_, ALU.mult, ALU.add),
                                  reads=[b_, wts, acc], writes=[acc])
        for nl in range(8):
            n = hf * 8 + nl
            xi = xin[nl % 2]
            kb.dma("sp", xi[:, :], x1_dram[n * 128:(n + 1) * 128, :], reads=[x1_t], writes=[xi])
            kb.op("dve", lambda e: e.scalar_tensor_tensor(xi[:, :], xi[:, :], DN_ALPHA, acc[:, nl, :], ALU.mult, ALU.add),
                  reads=[xi, acc], writes=[xi])
            layer_norm(kb, c, xi, xi[:, :], lng, lnb, lws)
            kb.dma("sp", out_dram[n * 128:(n + 1) * 128, :], xi[:, :], reads=[xi], writes=[o_t])
    kb.release(m0)


def setup_consts(kb, c, dr):
    def ld(name, shape, src):
        t_ = kb.sb(name, shape, F32)
        kb.dma("sp", t_[:, :], src, writes=[t_])
        return t_
    c.identf = ld("identf", [128, 128], dr["c_ident"][:, :])
    c.U = ld("cU", [128, 128], dr["c_U"][:, :])
    c.V = ld("cV", [128, 128], dr["c_V"][:, :])
    c.onesf = ld("cones", [128, 128], dr["c_ones"][:, :])
    c.mSL = ld("cmSL", [128, 128], dr["c_mSL"][:, :])
    c.mTU = ld("cmTU", [128, 128], dr["c_mTU"][:, :])
    c.bd = ld("cbd", [128, 128], dr["c_bd"][:, :])
    c.obd = ld("cobd", [128, 128], dr["c_obd"][:, :])
    c.amask = ld("amask", [128, 640], dr["c_amask"][:, :])
    c.identb = kb.sb("identb", [128, 128], BF16)
    kb.op("dve", lambda e: e.tensor_copy(c.identb[:, :], c.identf[:, :]), reads=[c.identf], writes=[c.identb])
    c.onespad = kb.sb("onespad", [128, 2, 128], BF16)
    kb.op("pool", lambda e: e.memset(c.onespad[:, :, :], 0.0), writes=[c.onespad])
    kb.op("pool", lambda e: e.memset(c.onespad[:, 0, 0:64], 1.0), writes=[c.onespad])
    kb.op("pool", lambda e: e.memset(c.onespad[:, 1, 64:128], 1.0), writes=[c.onespad])
    c.eps6 = kb.sb("eps6", [128, 1], F32)
    c.eps5 = kb.sb("eps5", [128, 1], F32)
    kb.op("pool", lambda e: e.memset(c.eps6[:, :], RMS_EPS), writes=[c.eps6])
    kb.op("pool", lambda e: e.memset(c.eps5[:, :], LN_EPS), writes=[c.eps5])
    c.one1 = kb.sb("one1", [128, 1], F32)
    kb.op("pool", lambda e: e.memset(c.one1[:, :], 1.0), writes=[c.one1])
    c.wst = [kb.sb("wst%d" % i, [128, WST], F32) for i in range(2)]
    c.wst_i = 0
    c.wb = [kb.sb("wb%d" % i, [128, 8, 512], BF16) for i in range(2)]


IN_SPECS = [
    ("x", None), ("w_in", [DEPTH, D, D_IN]), ("pool_w", [DEPTH, 4, 128, 128]),
    ("w_branch_a", [DEPTH, 512, D]), ("w_branch_b", [DEPTH, 512, D]), ("w_branch_c", [DEPTH, 512, D]),
    ("w_out", [DEPTH, D, D]), ("moe_w1", [DEPTH, 32, D, 512]), ("moe_w3", [DEPTH, 32, D, 512]),
    ("moe_w2", [DEPTH, 32, 512, D]),
    ("a_bias", [DEPTH, 128, 8, 640]), ("c_ident", [128, 128]), ("c_amask", [128, 640]), ("c_U", [128, 128]),
    ("c_V", [128, 128]), ("c_ones", [128, 128]), ("c_mSL", [128, 128]), ("c_mTU", [128, 128]), ("c_bd", [128, 128]), ("c_obd", [128, 128]), ("c_invc", [128, 4, 16]),
    ("p_cw", [DEPTH, 128, 12, 4]), ("p_vec4", [DEPTH, 128, 8]), ("p_normg", [DEPTH, 128, 128]),
    ("p_pscale", [DEPTH, 128, 4]), ("p_gateb", [DEPTH, 128, 24]), ("p_ln", [DEPTH, 4, 128, D]),
    ("p_rw", [DEPTH, 128, 8, 36]), ("p_rb", [DEPTH, 128, 36]),
]


def host_consts(inputs):
    f = lambda k_: np.asarray(inputs[k_], dtype=np.float32)
    out = {}
    out["c_ident"] = np.eye(128, dtype=np.float32)
    t = np.arange(128)
    out["c_U"] = (t[:, None] <= t[None, :]).astype(np.float32)
    out["c_V"] = (t[:, None] > t[None, :]).astype(np.float32)
    out["c_ones"] = np.ones((128, 128), np.float32)
    out["c_mSL"] = np.where(t[:, None] > t[None, :], 0.0, NEG).astype(np.float32)
    out["c_mTU"] = np.where(t[:, None] <= t[None, :], 0.0, NEG).astype(np.float32)
    out["c_bd"] = ((t[:, None] // 32) == (t[None, :] // 32)).astype(np.float32)
    out["c_obd"] = 1.0 - out["c_bd"]
    invc = np.zeros((128, 4, 16), np.float32)
    for gi in range(4):
        invc[:, gi, :] = 1.0 / np.minimum(np.arange(16) + 1.0, float(2 ** (gi + 1)))
    out["c_invc"] = invc
    k = np.arange(128)[:, None, None]
    jj = np.arange(5)[None, :, None]
    q = np.arange(128)[None, None, :]
    dist = q - ((jj - 4) * 128 + k)
    idx = np.clip(dist, -63, 256) + 63
    qc = q // 64
    kch = (jj - 4) * 2 + k // 64
    valid = (kch <= qc) & (kch >= qc - 8)
    out["c_amask"] = np.where(valid, 0.0, NEG).astype(np.float32).reshape(128, 640)
    rb = f("attn_rel_bias")
    g = rb[:, :, idx]
    out["a_bias"] = np.ascontiguousarray(g.transpose(0, 2, 1, 3, 4)).reshape(DEPTH, 128, 8, 640)
    cw = f("gdn_conv_w")
    out["p_cw"] = np.ascontiguousarray(cw.reshape(DEPTH, 4, 12, 128).transpose(0, 3, 2, 1))
    v4 = np.concatenate([f("gdn_a_log"), f("gdn_dt_bias")], axis=1)
    out["p_vec4"] = np.ascontiguousarray(np.broadcast_to(v4[:, None, :], (DEPTH, 128, 8)))
    out["p_normg"] = np.ascontiguousarray(np.broadcast_to(f("gdn_norm_g")[:, None, :], (DEPTH, 128, 128)))
    out["p_pscale"] = np.ascontiguousarray(f("pool_scale").reshape(DEPTH, 4, 128).transpose(0, 2, 1))
    out["p_gateb"] = np.ascontiguousarray(f("gate_b").reshape(DEPTH, 3, 8, 128).transpose(0, 3, 1, 2)).reshape(DEPTH, 128, 24)
    ln = np.stack([f("ln1_g"), f("ln1_b"), f("ln2_g"), f("ln2_b")], axis=1)
    out["p_ln"] = np.ascontiguousarray(np.broadcast_to(ln[:, :, None, :], (DEPTH, 4, 128, D)))
    rw = np.concatenate([f("router_group_w"), f("router_expert_w")], axis=2)
    out["p_rw"] = np.ascontiguousarray(rw.reshape(DEPTH, 8, 128, 36).transpose(0, 2, 1, 3))
    rbias = np.concatenate([f("router_group_b"), f("router_expert_b")], axis=1)
    out["p_rb"] = np.ascontiguousarray(np.broadcast_to(rbias[:, None, :], (DEPTH, 128, 36)))
    return out


ARENA_BYTES = 141 * 1024
WST = 1024


def build_program(n_seq=4, n_layers=DEPTH, stages=("attn", "gdn", "pool", "merge", "moe"), debug=False, n_exp=32, layer0=0):
    nc = bass.Bass("TRN2", target_bir_lowering=False)
    dr = {}
    for name, shape in IN_SPECS:
        shape = shape if shape is not None else [n_seq * SEQ, D]
        if name.startswith("moe_w"):
            shape = [shape[0], n_exp] + list(shape[2:])
        dr[name] = dram_in(nc, name, shape)
    out = nc.dram_tensor("out", [n_seq * SEQ, D], F32, kind="ExternalOutput").ap()
    kind = "ExternalOutput" if debug else "Internal"
    x1s = nc.dram_tensor("x1s", [n_seq * SEQ, D], F32, kind=kind).ap()
    x2s = nc.dram_tensor("x2s", [n_seq * SEQ, D], F32, kind=kind).ap()
    kb = KB(nc)
    kb.init_psum()
    c = Ctx()
    setup_consts(kb, c, dr)
    xT = kb.sb("xT", [128, 8, SEQ], BF16)
    wts = kb.sb("wts", [128, NT, 32], F32)
    kb.init_arena(ARENA_BYTES)
    dbg = {}
    if debug:
        for nm in ("oaT", "obT", "ocT"):
            dbg[nm] = nc.dram_tensor("dbg_" + nm, [4, 128, SEQ], BF16, kind="ExternalOutput").ap()
        dbg["wts"] = nc.dram_tensor("dbg_wts", [128, NT, 32], F32, kind="ExternalOutput").ap()
    dbg_t = T(None, "dbg")
    x_t, x1_t, x2_t, o_t = T(None, "x"), T(None, "x1s"), T(None, "x2s"), T(None, "out")
    for L in range(layer0, n_layers):
        src, src_t = (dr["x"], x_t) if L == layer0 else (x2s, x2_t)
        dst, dst_t = (out, o_t) if L == n_layers - 1 else (x2s, x2_t)
        for s in range(n_seq):
            rows = slice(s * SEQ, (s + 1) * SEQ)
            build_xT(kb, c, src_t, src[rows, :], xT)
            mo = kb.mark()
            oT = [kb.al(nm, [128, 4, SEQ], BF16) for nm in ("oaT", "obT", "ocT")]
            if "attn" in stages:
                attention_stage(kb, c, L, dr, dr["w_in"][L], xT, oT[0])
            if "gdn" in stages:
                gdn_stage(kb, c, L, dr, dr["w_in"][L], xT, oT[1])
            if "pool" in stages:
                pool_stage(kb, c, L, dr, dr["w_in"][L], xT, oT[2])
            if debug and L == layer0 and s == 0:
                for i, nm in enumerate(("oaT", "obT", "ocT")):
                    kb.dma("sp", dbg[nm].rearrange("c p t -> p c t"), oT[i][:, :, :], reads=[oT[i]], writes=[dbg_t])
            if "merge" in stages:
                merge_stage(kb, c, L, dr, dr["w_in"][L], xT, oT, src_t, src[rows, :], x1_t, x1s[rows, :], wts)
                if debug and L == layer0 and s == 0:
                    kb.dma("sp", dbg["wts"], wts[:, :, :], reads=[wts], writes=[dbg_t])
            kb.release(mo)
            if "moe" in stages:
                moe_stage(kb, c, L, dr, xT, wts, x1_t, x1s[rows, :], dst_t, dst[rows, :], n_exp=n_exp)
    kb.barrier()
    return nc, kb


_PROG = {}


def kernel(**inputs):
    n_cores = 8
    n_seq = 32 // n_cores
    if "p" not in _PROG:
        _PROG["p"] = build_program(n_seq=n_seq)
    nc, _ = _PROG["p"]
    hc = host_consts(inputs)
    shared = {}
    for name, shape in IN_SPECS:
        if name == "x":
            continue
        shared[name] = hc[name] if name in hc else np.ascontiguousarray(np.asarray(inputs[name], dtype=np.float32))
    x = np.ascontiguousarray(np.asarray(inputs["x"], dtype=np.float32)).reshape(n_cores, n_seq * SEQ, D)
    in_maps = [dict(shared, x=x[i]) for i in range(n_cores)]
    res = run_bass_kernel_spmd(nc, in_maps, core_ids=list(range(n_cores)))
    outs = [np.asarray(r["out"], dtype=np.float32).reshape(n_seq, SEQ, D) for r in res.results]
    return np.concatenate(outs, axis=0)
```

```python
import numpy as np
import concourse.bass as bass
import concourse.mybir as mybir
from concourse.bass_utils import run_bass_kernel_spmd

F32 = mybir.dt.float32
BF16 = mybir.dt.bfloat16
I32 = mybir.dt.int32
AF = mybir.ActivationFunctionType
ALU = mybir.AluOpType
AX = mybir.AxisListType

D = 1024
SEQ = 2048
NT = SEQ // 128
DEPTH = 2
D_IN = 7176
NEG = -30000.0
C_QA, C_KA, C_VA, C_QKVB, C_BETA, C_DEC, C_GBO, C_UC, C_GATE = 0, 512, 1024, 1536, 3072, 3076, 3080, 3592, 4104
DN_ALPHA = (2 * DEPTH) ** 0.25
LN_EPS = 1e-5
RMS_EPS = 1e-6


class Sem:
    def __init__(self, h):
        self.h = h
        self.val = 0


class T:
    def __init__(self, h, name=""):
        self.h = h
        self.name = name
        self.w = {}
        self.r = {}
        self.excl = False

    def __getitem__(self, k):
        return self.h[k]


class KB:
    NDS = 12

    def __init__(self, nc):
        self.nc = nc
        self.eng = {"pe": nc.tensor, "act": nc.scalar, "dve": nc.vector, "pool": nc.gpsimd, "sp": nc.sync}
        self.esem = {e: Sem(nc.alloc_semaphore("s_" + e)) for e in self.eng}
        self.seen = {e: {} for e in self.eng}
        self.dsem = {q: [Sem(nc.alloc_semaphore("d_%s%d" % (q, i))) for i in range(self.NDS)]
                     for q in ("sp", "pool")}
        self.dctr = {q: 0 for q in self.dsem}
        self.n_ins = 0
        self._ps = None
        self._psi = 0

    @staticmethod
    def _deps(reads, writes):
        deps = {}
        for t in reads:
            for s, v in t.w.items():
                if deps.get(s, 0) < v:
                    deps[s] = v
            if t.excl:
                for s, v in t.r.items():
                    if deps.get(s, 0) < v:
                        deps[s] = v
        for t in writes:
            for s, v in t.w.items():
                if deps.get(s, 0) < v:
                    deps[s] = v
            for s, v in t.r.items():
                if deps.get(s, 0) < v:
                    deps[s] = v
        return deps

    def _wait(self, e, deps):
        seen = self.seen[e]
        for s, v in deps.items():
            if seen.get(s, 0) < v:
                self.eng[e].wait_ge(s.h, v)
                seen[s] = v

    def op(self, e, fn, reads=(), writes=()):
        deps = self._deps(reads, writes)
        s = self.esem[e]
        if e == "pe":
            deps.pop(s, None)
        self._wait(e, deps)
        ins = fn(self.eng[e])
        s.val += 1
        ins.then_inc(s.h, 1)
        for t in reads:
            t.r[s] = s.val
        for t in writes:
            t.w[s] = s.val
        self.n_ins += 1
        return ins

    def dma(self, q, out, in_, reads=(), writes=(), **kw):
        pool = self.dsem[q]
        s = pool[self.dctr[q] % len(pool)]
        self.dctr[q] += 1
        deps = self._deps(reads, writes)
        if deps.get(s, 0) < s.val:
            deps[s] = s.val
        self._wait(q, deps)
        ins = self.eng[q].dma_start(out=out, in_=in_, **kw)
        s.val += 16
        ins.then_inc(s.h, 16)
        for t in reads:
            t.r[s] = s.val
        for t in writes:
            t.w[s] = s.val
        self.n_ins += 1
        return ins

    def idma(self, out, out_off, in_, in_off, reads=(), writes=()):
        q = "pool"
        pool = self.dsem[q]
        s = pool[self.dctr[q] % len(pool)]
        self.dctr[q] += 1
        deps = self._deps(reads, writes)
        if deps.get(s, 0) < s.val:
            deps[s] = s.val
        self._wait(q, deps)
        ins = self.nc.gpsimd.indirect_dma_start(out=out, out_offset=out_off, in_=in_, in_offset=in_off)
        s.val += 16
        ins.then_inc(s.h, 16)
        for t in reads:
            t.r[s] = s.val
        for t in writes:
            t.w[s] = s.val
        self.n_ins += 1
        return ins

    def barrier(self):
        allsems = list(self.esem.values()) + [s for p in self.dsem.values() for s in p]
        for e in self.eng:
            self._wait(e, {s: s.val for s in allsems if s.val > 0})

    def sb(self, name, shape, dtype):
        return T(self.nc.alloc_sbuf_tensor(name, list(shape), dtype), name)

    def init_arena(self, nbytes):
        self.arena = self.nc.alloc_sbuf_tensor("arena", [128, nbytes // 2], BF16)
        self.arena_n = nbytes // 2
        self.arena_p = 0

    def al(self, name, shape, dtype):
        n = 1
        for d in shape[1:]:
            n *= d
        n2 = n * (2 if dtype in (F32, I32) else 1)
        n2 = (n2 + 15) // 16 * 16
        assert self.arena_p + n2 <= self.arena_n, ("arena overflow", name, self.arena_p, n2, self.arena_n)
        v = self.arena[0:shape[0], self.arena_p:self.arena_p + n2]
        self.arena_p += n2
        if dtype != BF16:
            v = v.bitcast(dtype)
        v = v[:, 0:n]
        if len(shape) == 3:
            v = v.rearrange("p (a b) -> p a b", a=shape[1])
        elif len(shape) == 4:
            v = v.rearrange("p (a b c) -> p a b c", a=shape[1], b=shape[2])
        return T(v, name)

    def mark(self):
        return self.arena_p

    def release(self, m):
        self.barrier()
        self.arena_p = m

    def init_psum(self):
        self._ps = [T(self.nc.alloc_psum_tensor("ps%d" % i, [128, 512], F32), "ps%d" % i) for i in range(8)]
        for t in self._ps:
            t.excl = True

    def psum(self):
        t = self._ps[self._psi % 8]
        self._psi += 1
        return t

    def mm(self, out_t, out, lhsT_t, lhsT, rhs_t, rhs, start=True, stop=True):
        return self.op("pe", lambda e: e.matmul(out, lhsT, rhs, start=start, stop=stop),
                       reads=[lhsT_t, rhs_t], writes=[out_t])

    def tr(self, out_t, out, in_t, in_, id_t, ident):
        return self.op("pe", lambda e: e.transpose(out, in_, ident), reads=[in_t, id_t], writes=[out_t])


class Ctx:
    pass


def dram_in(nc, name, shape, dtype=F32):
    return nc.dram_tensor(name, list(shape), dtype, kind="ExternalInput").ap()


def load_w(kb, c, dst_t, dst, src, kc, cols):
    per = max(1, WST // cols)
    k0 = 0
    while k0 < kc:
        kk = min(per, kc - k0)
        st = c.wst[c.wst_i % len(c.wst)]
        c.wst_i += 1
        sv = st[:, 0:kk * cols].rearrange("p (k c) -> p k c", k=kk)
        kb.dma("sp", sv, src[k0 * 128:(k0 + kk) * 128, :].rearrange("(k p) c -> p k c", p=128), writes=[st])
        d = dst[:, k0:k0 + kk, :]
        kb.op("pool", lambda e: e.tensor_copy(d, sv), reads=[st], writes=[dst_t])
        k0 += kk


def bc(ap, shape):
    return ap.to_broadcast(list(shape))


def build_xT(kb, c, x_t, x_dram, xT):
    m0 = kb.mark()
    xin = [kb.al("xin%d" % i, [128, 1024], F32) for i in range(2)]
    for n in range(NT):
        xi = xin[n % 2]
        kb.dma("sp", xi[:, :], x_dram[n * 128:(n + 1) * 128, :], reads=[x_t], writes=[xi])
        for half in range(2):
            bank = kb.psum()
            for j in range(4):
                k = half * 4 + j
                kb.tr(bank, bank[:, j * 128:(j + 1) * 128], xi, xi[:, k * 128:(k + 1) * 128], c.identf, c.identf[:, :])
            dst = xT[:, half * 4:half * 4 + 4, n * 128:(n + 1) * 128]
            src = bank[:, :].rearrange("p (k t) -> p k t", k=4)
            if half == 0:
                kb.op("act", lambda e: e.copy(dst, src), reads=[bank], writes=[xT])
            else:
                kb.op("dve", lambda e: e.tensor_copy(dst, src), reads=[bank], writes=[xT])
    kb.release(m0)


def attention_stage(kb, c, L, dr, w_in_l, xT, oaT):
    m0 = kb.mark()
    qT = kb.al("qT", [128, 4, SEQ], BF16)
    kT = kb.al("kT", [128, 4, SEQ], BF16)
    vpad = kb.al("vpad", [128, NT, 8, 128], BF16)
    PT = [kb.al("PT%d" % i, [128, 10 * 128], BF16) for i in range(2)]
    rden = [kb.al("rden%d" % i, [128, 128], F32) for i in range(2)]
    btab = kb.al("btab", [128, 8, 5, 128], BF16)
    bst = [kb.al("bst%d" % i, [128, 640], F32) for i in range(2)]
    wb = c.wb
    for h in range(8):
        b_ = bst[h % 2]
        kb.dma("sp", b_[:, :], dr["a_bias"][L, :, h, :], writes=[b_])
        bt = btab[:, h, :, :].rearrange("p j q -> p (j q)")
        kb.op("dve", lambda e: e.tensor_tensor(bt, b_[:, :], c.amask[:, :], ALU.add), reads=[b_, c.amask], writes=[btab])
    kb.op("pool", lambda e: e.memset(vpad[:, :, :, :], 0.0), writes=[vpad])
    for wi, (c0, dstT, scale) in enumerate(((C_QA, qT, 0.125), (C_KA, kT, 1.0))):
        w = wb[wi % 2]
        load_w(kb, c, w, w[:, :, :], w_in_l[:, c0:c0 + 512], 8, 512)
        for cc in range(4):
            for tb in range(4):
                bank = kb.psum()
                for k in range(8):
                    kb.mm(bank, bank[:, :], w, w[:, k, cc * 128:(cc + 1) * 128], xT, xT[:, k, tb * 512:(tb + 1) * 512],
                          start=(k == 0), stop=(k == 7))
                dst = dstT[:, cc, tb * 512:(tb + 1) * 512]
                kb.op("act", lambda e: e.activation(dst, bank[:, :], AF.Copy, scale=scale), reads=[bank], writes=[dstT])
    w = wb[0]
    load_w(kb, c, w, w[:, :, :], w_in_l[:, C_VA:C_VA + 512], 8, 512)
    for n in range(NT):
        bank = kb.psum()
        for k in range(8):
            kb.mm(bank, bank[:, :], xT, xT[:, k, n * 128:(n + 1) * 128], w, w[:, k, :], start=(k == 0), stop=(k == 7))
        bv = bank[:, :].rearrange("p (hp two d) -> p hp two d", two=2, d=64)
        vv = vpad[:, n, :, :].rearrange("p (hp two) d -> p hp two d", two=2)
        kb.op("dve", lambda e: e.tensor_copy(vv[:, :, 0, 0:64], bv[:, :, 0, :]), reads=[bank], writes=[vpad])
        kb.op("act", lambda e: e.copy(vv[:, :, 1, 64:128], bv[:, :, 1, :]), reads=[bank], writes=[vpad])
    it = 0
    for i in range(NT):
        js = [j for j in range(i - 4, i + 1) if j >= 0]
        for p in range(4):
            tiles = [(hh, j) for hh in (0, 1) for j in js]
            pt = PT[it % 2]
            rd = rden[it % 2]
            it += 1
            for g in range(0, len(tiles), 4):
                grp = tiles[g:g + 4]
                bank = kb.psum()
                for idx, (hh, j) in enumerate(grp):
                    h = 2 * p + hh
                    jj = j - (i - 4)
                    o = bank[:, idx * 128:(idx + 1) * 128]
                    kb.mm(bank, o, kT, kT[hh * 64:(hh + 1) * 64, p, j * 128:(j + 1) * 128],
                          qT, qT[hh * 64:(hh + 1) * 64, p, i * 128:(i + 1) * 128], start=True, stop=False)
                    kb.mm(bank, o, c.identb, c.identb[:, :], btab, btab[:, h, jj, :], start=False, stop=True)
                n_ = len(grp) * 128
                dst = pt[:, g * 128:g * 128 + n_]
                src = bank[:, 0:n_]
                kb.op("act", lambda e: e.activation(dst, src, AF.Exp), reads=[bank], writes=[pt])
            bank = kb.psum()
            for idx, (hh, j) in enumerate(tiles):
                kb.mm(bank, bank[:, 0:128], vpad, vpad[:, j, 2 * p + hh, :], pt, pt[:, idx * 128:(idx + 1) * 128],
                      start=(idx == 0), stop=(idx == len(tiles) - 1))
            for idx, (hh, j) in enumerate(tiles):
                kb.mm(bank, bank[:, 128:256], c.onespad, c.onespad[:, hh, :], pt, pt[:, idx * 128:(idx + 1) * 128],
                      start=(idx == 0), stop=(idx == len(tiles) - 1))
            kb.op("dve", lambda e: e.reciprocal(rd[:, :], bank[:, 128:256]), reads=[bank], writes=[rd])
            dst = oaT[:, p, i * 128:(i + 1) * 128]
            kb.op("dve", lambda e: e.tensor_tensor(dst, bank[:, 0:128], rd[:, :], ALU.mult), reads=[bank, rd], writes=[oaT])
    kb.release(m0)


class _Stop(Exception):
    pass


def _chk(k):
    import os
    if float(os.environ.get("GDN_STOP", "0")) == k:
        raise _Stop()


def gdn_stage(kb, c, L, dr, w_in_l, xT, obT):
    m0 = kb.mark()
    try:
        _gdn_stage(kb, c, L, dr, w_in_l, xT, obT)
    except _Stop:
        pass
    kb.release(m0)


def _gdn_stage(kb, c, L, dr, w_in_l, xT, obT):
    al = kb.al
    pre = al("g_pre", [128, 3 + SEQ], F32)
    acc = al("g_acc", [128, 512], F32)
    tmp = al("g_tmp", [128, 512], F32)
    qb = al("g_qb", [128, SEQ], BF16)
    kbf = al("g_kb", [128, SEQ], BF16)
    vb = al("g_vb", [128, SEQ], BF16)
    gsl = al("g_gsl", [128, NT, 128], BF16)
    u = al("g_u", [128, NT, 128], BF16)
    wT = al("g_wT", [128, SEQ], BF16)
    kd = al("g_kd", [128, NT, 128], BF16)
    qkT = al("g_qkT", [128, NT, 128], BF16)
    obr = al("g_obr", [128, NT, 128], BF16)
    wsm = [al("g_wsm%d" % i, [128, 8, 128], BF16) for i in range(2)]
    w8 = al("g_w8", [128, 8, 8], BF16)
    cw = al("g_cw", [128, 12, 4], F32)
    vec4 = al("g_vec4", [128, 8], F32)
    normg = al("g_normg", [128, 128], F32)
    bg = al("g_bg", [128, 128], F32)
    sm = {n_: al("g_" + n_, [128, 64], F32) for n_ in
          ("beta", "g", "gcum", "gtot", "egc", "ekd", "egl", "beg", "nbeta", "z", "az", "sp")}
    aexp = al("g_aexp", [128, 4], F32)
    ssum = al("g_ssum", [128, NT], F32)
    rstd = al("g_rstd", [128, NT], F32)
    rg = [al("g_rg%d" % i, [128, 4, 128], F32) for i in range(2)]
    Dm3 = [al("g_Dm%d" % i, [128, 4, 128], F32) for i in range(2)]
    Dm = [T(d_[:, :, :].rearrange("p j d -> p (j d)"), d_.name) for d_ in Dm3]
    for a_, b__ in zip(Dm, Dm3):
        a_.w, a_.r = b__.w, b__.r
    DT = [al("g_DT%d" % i, [128, 512], F32) for i in range(2)]
    P = [al("g_P%d" % i, [128, 4, 128], BF16) for i in range(2)]
    PTt = [al("g_PT%d" % i, [128, 4, 128], BF16) for i in range(2)]
    X = [al("g_X%d" % i, [128, 4, 256], BF16) for i in range(2)]
    wtok = rg
    MoT = [al("g_MoT%d" % i, [128, 4, 128], BF16) for i in range(2)]
    TdT = [al("g_TdT%d" % i, [128, 4, 128], BF16) for i in range(2)]
    Bx = [al("g_Bx%d" % i, [128, 4, 256], BF16) for i in range(2)]
    S = al("g_S", [128, 128], F32)
    Sb = al("g_Sb", [128, 128], BF16)
    vn = [al("g_vn%d" % i, [128, 128], BF16) for i in range(2)]
    t1 = [al("g_t1%d" % i, [128, 128], F32) for i in range(2)]
    onf = Dm3

    def v3(t_):
        return t_[:, :].rearrange("p (n h) -> p n h", h=4)

    kb.dma("sp", cw[:, :, :], dr["p_cw"][L], writes=[cw])
    kb.dma("sp", vec4[:, :], dr["p_vec4"][L], writes=[vec4])
    kb.dma("sp", normg[:, :], dr["p_normg"][L], writes=[normg])
    kb.op("pool", lambda e: e.memset(pre[:, 0:3], 0.0), writes=[pre])

    _chk(0.1)
    load_w(kb, c, w8, w8[:, :, :], w_in_l[:, C_BETA:C_BETA + 8], 8, 8)
    _chk(0.2)
    bank = kb.psum()
    for n in range(NT):
        for k in range(8):
            kb.mm(bank, bank[:, n * 8:(n + 1) * 8], xT, xT[:, k, n * 128:(n + 1) * 128], w8, w8[:, k, :],
                  start=(k == 0), stop=(k == 7))
    kb.op("act", lambda e: e.copy(bg[:, :], bank[:, 0:128]), reads=[bank], writes=[bg])
    _chk(0.3)
    bg3 = bg[:, :].rearrange("p (n c) -> p n c", c=8)
    beta, g, gcum, gtot, egc, ekd, egl, beg, nbeta = (sm[k_] for k_ in
                                                      ("beta", "g", "gcum", "gtot", "egc", "ekd", "egl", "beg", "nbeta"))
    z, az, sp = sm["z"], sm["az"], sm["sp"]
    kb.op("act", lambda e: e.activation(v3(beta), bg3[:, :, 0:4], AF.Sigmoid), reads=[bg], writes=[beta])
    kb.op("dve", lambda e: e.tensor_tensor(v3(z), bg3[:, :, 4:8], bc(vec4[:, 4:8].unsqueeze(1), [128, NT, 4]), ALU.add),
          reads=[bg, vec4], writes=[z])
    kb.op("dve", lambda e: e.scalar_tensor_tensor(az[:, :], z[:, :], -1.0, z[:, :], ALU.mult, ALU.max), reads=[z], writes=[az])
    _chk(0.4)
    kb.op("act", lambda e: e.activation(az[:, :], az[:, :], AF.Exp, scale=-1.0), reads=[az], writes=[az])
    kb.op("act", lambda e: e.activation(az[:, :], az[:, :], AF.Ln, bias=c.one1[:, 0:1]), reads=[az, c.one1], writes=[az])
    kb.op("dve", lambda e: e.scalar_tensor_tensor(sp[:, :], z[:, :], 0.0, az[:, :], ALU.max, ALU.add),
          reads=[z, az], writes=[sp])
    kb.op("act", lambda e: e.activation(aexp[:, :], vec4[:, 0:4], AF.Exp), reads=[vec4], writes=[aexp])
    kb.op("dve", lambda e: e.scalar_tensor_tensor(v3(g), v3(sp), -1.0, bc(aexp[:, :].unsqueeze(1), [128, NT, 4]),
                                                  ALU.mult, ALU.mult), reads=[sp, aexp], writes=[g])
    _chk(0.5)
    bank = kb.psum()
    kb.mm(bank, bank[:, 0:64], c.U, c.U[:, :], g, g[:, :])
    _chk(0.52)
    kb.mm(bank, bank[:, 64:128], c.onesf, c.onesf[:, :], g, g[:, :])
    _chk(0.54)
    kb.op("act", lambda e: e.copy(gcum[:, :], bank[:, 0:64]), reads=[bank], writes=[gcum])
    _chk(0.56)
    kb.op("dve", lambda e: e.tensor_copy(gtot[:, :], bank[:, 64:128]), reads=[bank], writes=[gtot])
    _chk(0.6)
    kb.op("act", lambda e: e.activation(egc[:, :], gcum[:, :], AF.Exp), reads=[gcum], writes=[egc])
    kb.op("dve", lambda e: e.tensor_tensor(ekd[:, :], gtot[:, :], gcum[:, :], ALU.subtract), reads=[gtot, gcum], writes=[ekd])
    kb.op("act", lambda e: e.activation(ekd[:, :], ekd[:, :], AF.Exp), reads=[ekd], writes=[ekd])
    kb.op("act", lambda e: e.activation(egl[:, :], gtot[:, :], AF.Exp), reads=[gtot], writes=[egl])
    kb.op("dve", lambda e: e.tensor_tensor(beg[:, :], beta[:, :], egc[:, :], ALU.mult), reads=[beta, egc], writes=[beg])
    kb.op("dve", lambda e: e.tensor_scalar(nbeta[:, :], beta[:, :], -1.0, None, ALU.mult), reads=[beta], writes=[nbeta])

    _chk(1)
    wi = 0
    for h in range(4):
        w = wsm[wi % 2]
        wi += 1
        load_w(kb, c, w, w[:, :, :], w_in_l[:, C_GBO + h * 128:C_GBO + (h + 1) * 128], 8, 128)
        for n0 in range(0, NT, 4):
            bank = kb.psum()
            for j in range(4):
                n = n0 + j
                for k in range(8):
                    kb.mm(bank, bank[:, j * 128:(j + 1) * 128], xT, xT[:, k, n * 128:(n + 1) * 128], w, w[:, k, :],
                          start=(k == 0), stop=(k == 7))
            dst = gsl[:, n0:n0 + 4, :]
            src = bank[:, :].rearrange("p (j d) -> p j d", j=4)
            kb.op("act", lambda e: e.activation(dst, src, AF.Silu), reads=[bank], writes=[gsl])
        _chk(2)
        for t, dstb in ((0, qb), (1, kbf), (2, vb)):
            w = wsm[wi % 2]
            wi += 1
            col = C_QKVB + t * 512 + h * 128
            load_w(kb, c, w, w[:, :, :], w_in_l[:, col:col + 128], 8, 128)
            for tb in range(4):
                bank = kb.psum()
                for k in range(8):
                    kb.mm(bank, bank[:, :], w, w[:, k, :], xT, xT[:, k, tb * 512:(tb + 1) * 512],
                          start=(k == 0), stop=(k == 7))
                dst = pre[:, 3 + tb * 512:3 + (tb + 1) * 512]
                kb.op("act", lambda e: e.copy(dst, bank[:, :]), reads=[bank], writes=[pre])
            cwv = cw[:, t * 4 + h, :]
            for hb in range(4):
                o0 = hb * 512
                kb.op("dve", lambda e: e.tensor_scalar(acc[:, :], pre[:, o0:o0 + 512], cwv[:, 0:1], None, ALU.mult),
                      reads=[pre, cw], writes=[acc])
                for j in range(1, 4):
                    kb.op("dve", lambda e: e.scalar_tensor_tensor(acc[:, :], pre[:, o0 + j:o0 + j + 512], cwv[:, j:j + 1],
                                                                  acc[:, :], ALU.mult, ALU.add),
                          reads=[pre, cw, acc], writes=[acc])
                kb.op("act", lambda e: e.activation(tmp[:, :], acc[:, :], AF.Silu), reads=[acc], writes=[tmp])
                dst = dstb[:, o0:o0 + 512]
                if t < 2:
                    kb.op("pool", lambda e: e.tensor_tensor(acc[:, :], tmp[:, :], tmp[:, :], ALU.mult), reads=[tmp], writes=[acc])
                    sc = (128.0 ** -0.5) if t == 0 else 1.0
                    bank = kb.psum()
                    kb.mm(bank, bank[:, :], c.onesf, c.onesf[:, :], acc, acc[:, :])
                    kb.op("act", lambda e: e.activation(acc[:, :], bank[:, :], AF.Sqrt, bias=c.eps6[:, 0:1]),
                          reads=[bank, c.eps6], writes=[acc])
                    kb.op("dve", lambda e: e.reciprocal(acc[:, :], acc[:, :]), reads=[acc], writes=[acc])
                    kb.op("dve", lambda e: e.scalar_tensor_tensor(dst, tmp[:, :], sc, acc[:, :], ALU.mult, ALU.mult),
                          reads=[tmp, acc], writes=[dstb])
                else:
                    kb.op("pool", lambda e: e.tensor_copy(dst, tmp[:, :]), reads=[tmp], writes=[dstb])
        _chk(3)
        for pair in range(2):
            tgs = [2 * pair, 2 * pair + 1]
            for s, tg in enumerate(tgs):
                n0 = tg * 4
                kb.op("dve", lambda e: e.tensor_tensor(rg[s][:, :, :], bc(c.V[:, :].unsqueeze(1), [128, 4, 128]),
                                                       bc(v3(g)[:, n0:n0 + 4, h:h + 1], [128, 4, 128]), ALU.mult),
                      reads=[c.V, g], writes=[rg[s]])
                bG = kb.psum()
                bGT = kb.psum()
                for j in range(4):
                    o = bG[:, j * 128:(j + 1) * 128]
                    kb.mm(bG, o, c.U, c.U[:, :], rg[s], rg[s][:, j, :], start=True, stop=False)
                    kb.mm(bG, o, c.identf, c.identf[:, :], c.mSL, c.mSL[:, :], start=False, stop=True)
                for j in range(4):
                    o = bGT[:, j * 128:(j + 1) * 128]
                    kb.mm(bGT, o, rg[s], rg[s][:, j, :], c.U, c.U[:, :], start=True, stop=False)
                    kb.mm(bGT, o, c.identf, c.identf[:, :], c.mTU, c.mTU[:, :], start=False, stop=True)
                kb.op("act", lambda e: e.activation(Dm[s][:, :], bG[:, :], AF.Exp), reads=[bG], writes=[Dm[s]])
                kb.op("act", lambda e: e.activation(DT[s][:, :], bGT[:, :], AF.Exp), reads=[bGT], writes=[DT[s]])
                bKK = kb.psum()
                bQK = kb.psum()
                for j in range(4):
                    ts_ = slice((n0 + j) * 128, (n0 + j + 1) * 128)
                    kb.mm(bKK, bKK[:, j * 128:(j + 1) * 128], kbf, kbf[:, ts_], kbf, kbf[:, ts_])
                    kb.mm(bQK, bQK[:, j * 128:(j + 1) * 128], kbf, kbf[:, ts_], qb, qb[:, ts_])
                kb.op("dve", lambda e: e.tensor_tensor(Dm[s][:, :], bKK[:, :], Dm[s][:, :], ALU.mult),
                      reads=[bKK, Dm[s]], writes=[Dm[s]])
                kb.op("dve", lambda e: e.tensor_tensor(P[s][:, :, :], Dm[s][:, :].rearrange("p (j d) -> p j d", j=4),
                                                        bc(v3(nbeta)[:, n0:n0 + 4, h:h + 1], [128, 4, 128]), ALU.mult),
                      reads=[Dm[s], nbeta], writes=[P[s]])
                kb.op("dve", lambda e: e.tensor_tensor(qkT[:, n0:n0 + 4, :], bQK[:, :].rearrange("p (j d) -> p j d", j=4),
                                                       DT[s][:, :].rearrange("p (j d) -> p j d", j=4), ALU.mult),
                      reads=[bQK, DT[s]], writes=[qkT])
                bT = kb.psum()
                bTv = bT[:, :].bitcast(BF16)
                for j in range(4):
                    kb.tr(bT, bTv[:, j * 128:(j + 1) * 128], P[s], P[s][:, j, :], c.identb, c.identb[:, :])
                mtv = bTv[:, 0:512].rearrange("p (j d) -> p j d", j=4)
                bd_b = bc(c.bd[:, :].unsqueeze(1), [128, 4, 128])
                obd_b = bc(c.obd[:, :].unsqueeze(1), [128, 4, 128])
                kb.op("dve", lambda e: e.tensor_tensor(PTt[s][:, :, :], mtv, bd_b, ALU.mult), reads=[bT, c.bd], writes=[PTt[s]])
                kb.op("dve", lambda e: e.tensor_tensor(MoT[s][:, :, :], mtv, obd_b, ALU.mult), reads=[bT, c.obd], writes=[MoT[s]])
                kb.op("dve", lambda e: e.tensor_tensor(P[s][:, :, :], P[s][:, :, :], bd_b, ALU.mult), reads=[P[s], c.bd], writes=[P[s]])
                kb.op("dve", lambda e: e.tensor_tensor(TdT[s][:, :, :], PTt[s][:, :, :], bc(c.identf[:, :].unsqueeze(1), [128, 4, 128]), ALU.add),
                      reads=[PTt[s], c.identf], writes=[TdT[s]])
                bX = kb.psum()
                bXv = bX[:, :].bitcast(BF16)
                for j in range(4):
                    ts_ = slice((n0 + j) * 128, (n0 + j + 1) * 128)
                    kb.tr(bX, bXv[:, j * 128:(j + 1) * 128], vb, vb[:, ts_], c.identb, c.identb[:, :])
                    kb.tr(bX, bXv[:, 512 + j * 128:512 + (j + 1) * 128], kbf, kbf[:, ts_], c.identb, c.identb[:, :])
                vps = bXv[:, 0:512].rearrange("p (j d) -> p j d", j=4)
                kps = bXv[:, 512:1024].rearrange("p (j d) -> p j d", j=4)
                kb.op("dve", lambda e: e.tensor_tensor(Bx[s][:, :, 0:128], vps, bc(v3(beta)[:, n0:n0 + 4, h:h + 1], [128, 4, 128]),
                                                       ALU.mult), reads=[bX, beta], writes=[Bx[s]])
                kb.op("dve", lambda e: e.tensor_tensor(Bx[s][:, :, 128:256], kps, bc(v3(beg)[:, n0:n0 + 4, h:h + 1], [128, 4, 128]),
                                                       ALU.mult), reads=[bX, beg], writes=[Bx[s]])
                kb.op("dve", lambda e: e.tensor_tensor(kd[:, n0:n0 + 4, :], kps, bc(v3(ekd)[:, n0:n0 + 4, h:h + 1], [128, 4, 128]),
                                                       ALU.mult), reads=[bX, ekd], writes=[kd])
            _chk(4)
            for lev in range(4):
                for s, tg in enumerate(tgs):
                    bP = kb.psum()
                    bPT = kb.psum()
                    for j in range(4):
                        kb.mm(bP, bP[:, j * 128:(j + 1) * 128], PTt[s], PTt[s][:, j, :], P[s], P[s][:, j, :])
                    for j in range(4):
                        kb.mm(bPT, bPT[:, j * 128:(j + 1) * 128], P[s], P[s][:, j, :], PTt[s], PTt[s][:, j, :])
                    kb.op("act", lambda e: e.copy(P[s][:, :, :], bP[:, :].rearrange("p (j d) -> p j d", j=4)),
                          reads=[bP], writes=[P[s]])
                    kb.op("act", lambda e: e.copy(PTt[s][:, :, :], bPT[:, :].rearrange("p (j d) -> p j d", j=4)),
                          reads=[bPT], writes=[PTt[s]])
                    bU = kb.psum()
                    for j in range(4):
                        kb.mm(bU, bU[:, j * 128:(j + 1) * 128], P[s], P[s][:, j, :], TdT[s], TdT[s][:, j, :])
                    kb.op("dve", lambda e: e.tensor_tensor(TdT[s][:, :, :], TdT[s][:, :, :], bU[:, :].rearrange("p (j d) -> p j d", j=4), ALU.add),
                          reads=[TdT[s], bU], writes=[TdT[s]])
            for it in range(4):
                for s, tg in enumerate(tgs):
                    n0 = tg * 4
                    if it > 0:
                        bY = [kb.psum(), kb.psum()]
                        for j in range(4):
                            kb.mm(bY[j // 2], bY[j // 2][:, (j % 2) * 256:(j % 2 + 1) * 256], MoT[s], MoT[s][:, j, :], X[s], X[s][:, j, :])
                        for b_ in range(2):
                            yv = bY[b_][:, :].rearrange("p (j d) -> p j d", j=2)
                            kb.op("dve", lambda e: e.tensor_tensor(X[s][:, 2 * b_:2 * b_ + 2, :], Bx[s][:, 2 * b_:2 * b_ + 2, :], yv, ALU.add),
                                  reads=[Bx[s], bY[b_]], writes=[X[s]])
                    src_t = Bx[s] if it == 0 else X[s]
                    bA = [kb.psum(), kb.psum()]
                    for j in range(4):
                        kb.mm(bA[j // 2], bA[j // 2][:, (j % 2) * 256:(j % 2 + 1) * 256], TdT[s], TdT[s][:, j, :], src_t, src_t[:, j, :])
                    for b_ in range(2):
                        av = bA[b_][:, :].rearrange("p (j d) -> p j d", j=2)
                        if it < 3:
                            kb.op("act", lambda e: e.copy(X[s][:, 2 * b_:2 * b_ + 2, :], av), reads=[bA[b_]], writes=[X[s]])
                        else:
                            kb.op("act", lambda e: e.copy(u[:, n0 + 2 * b_:n0 + 2 * b_ + 2, :], av[:, :, 0:128]), reads=[bA[b_]], writes=[u])
                            kb.op("dve", lambda e: e.tensor_copy(wtok[s][:, 2 * b_:2 * b_ + 2, :], av[:, :, 128:256]), reads=[bA[b_]], writes=[wtok[s]])
            _chk(5)
            for s, tg in enumerate(tgs):
                n0 = tg * 4
                bW = kb.psum()
                for j in range(4):
                    kb.tr(bW, bW[:, j * 128:(j + 1) * 128], wtok[s], wtok[s][:, j, :], c.identf, c.identf[:, :])
                kb.op("act", lambda e: e.copy(wT[:, n0 * 128:(n0 + 4) * 128], bW[:, :]), reads=[bW], writes=[wT])
        _chk(6)
        kb.op("pool", lambda e: e.memset(S[:, :], 0.0), writes=[S])
        kb.op("pool", lambda e: e.memset(Sb[:, :], 0.0), writes=[Sb])
        for n in range(NT):
            ts_ = slice(n * 128, (n + 1) * 128)
            col = n * 4 + h
            b1 = kb.psum()
            kb.mm(b1, b1[:, 0:128], wT, wT[:, ts_], Sb, Sb[:, :])
            v_ = vn[n % 2]
            kb.op("dve", lambda e: e.tensor_tensor(v_[:, :], u[:, n, :], b1[:, 0:128], ALU.subtract), reads=[u, b1], writes=[v_])
            b2 = kb.psum()
            kb.mm(b2, b2[:, 0:128], qb, qb[:, ts_], Sb, Sb[:, :])
            kb.mm(b2, b2[:, 128:256], qkT, qkT[:, n, :], v_, v_[:, :])
            t_ = t1[n % 2]
            kb.op("dve", lambda e: e.tensor_scalar(t_[:, :], b2[:, 0:128], egc[:, col:col + 1], None, ALU.mult),
                  reads=[b2, egc], writes=[t_])
            kb.op("dve", lambda e: e.tensor_tensor(obr[:, n, :], t_[:, :], b2[:, 128:256], ALU.add), reads=[t_, b2], writes=[obr])
            if n < NT - 1:
                b3 = kb.psum()
                kb.mm(b3, b3[:, 0:128], kd, kd[:, n, :], v_, v_[:, :])
                kb.op("dve", lambda e: e.scalar_tensor_tensor(S[:, :], S[:, :], egl[:, col:col + 1], b3[:, 0:128], ALU.mult, ALU.add),
                      reads=[S, egl, b3], writes=[S])
                kb.op("act", lambda e: e.copy(Sb[:, :], S[:, :]), reads=[S], writes=[Sb])
        _chk(7)
        for g4 in range(4):
            n0 = g4 * 4
            o_ = onf[g4 % 2]
            kb.op("dve", lambda e: e.tensor_tensor(o_[:, :, :], obr[:, n0:n0 + 4, :], obr[:, n0:n0 + 4, :], ALU.mult),
                  reads=[obr], writes=[o_])
            kb.op("dve", lambda e: e.tensor_reduce(ssum[:, n0:n0 + 4], o_[:, :, :], AX.X, ALU.add), reads=[o_], writes=[ssum])
        kb.op("act", lambda e: e.activation(rstd[:, :], ssum[:, :], AF.Sqrt, bias=c.eps6[:, 0:1], scale=1.0 / 128.0),
              reads=[ssum, c.eps6], writes=[rstd])
        kb.op("dve", lambda e: e.reciprocal(rstd[:, :], rstd[:, :]), reads=[rstd], writes=[rstd])
        for g4 in range(4):
            n0 = g4 * 4
            o_ = onf[g4 % 2]
            kb.op("dve", lambda e: e.tensor_tensor(o_[:, :, :], obr[:, n0:n0 + 4, :], bc(rstd[:, n0:n0 + 4].unsqueeze(2), [128, 4, 128]),
                                                   ALU.mult), reads=[obr, rstd], writes=[o_])
            kb.op("dve", lambda e: e.tensor_tensor(o_[:, :, :], o_[:, :, :], bc(normg[:, :].unsqueeze(1), [128, 4, 128]), ALU.mult),
                  reads=[o_, normg], writes=[o_])
            kb.op("dve", lambda e: e.tensor_tensor(o_[:, :, :], o_[:, :, :], gsl[:, n0:n0 + 4, :], ALU.mult),
                  reads=[o_, gsl], writes=[o_])
            bank = kb.psum()
            for j in range(4):
                kb.tr(bank, bank[:, j * 128:(j + 1) * 128], o_, o_[:, j, :], c.identf, c.identf[:, :])
            kb.op("act", lambda e: e.copy(obT[:, h, n0 * 128:(n0 + 4) * 128], bank[:, :]), reads=[bank], writes=[obT])


def pool_stage(kb, c, L, dr, w_in_l, xT, ocT):
    m0 = kb.mark()
    al = kb.al
    PAD = 16
    up = al("p_up", [128, PAD + SEQ], F32)
    sa = al("p_sa", [128, PAD + SEQ], F32)
    sbb = al("p_sb", [128, PAD + SEQ], F32)
    wu = [al("p_wu%d" % i, [128, 8, 128], BF16) for i in range(2)]
    pw = [al("p_pw%d" % i, [128, 1, 128], BF16) for i in range(2)]
    pl = al("p_pl", [128, SEQ], BF16)
    psc = al("p_psc", [128, 4], F32)
    invc = al("p_invc", [128, 4, 16], F32)
    t16 = al("p_t16", [128, 16], F32)
    kb.dma("sp", psc[:, :], dr["p_pscale"][L], writes=[psc])
    kb.dma("sp", invc[:, :, :], dr["c_invc"], writes=[invc])
    for b_ in (up, sa, sbb):
        kb.op("pool", lambda e: e.memset(b_[:, 0:PAD], 0.0), writes=[b_])
    for gi in range(4):
        win = 2 ** (gi + 1)
        w = wu[gi % 2]
        load_w(kb, c, w, w[:, :, :], w_in_l[:, C_UC + gi * 128:C_UC + (gi + 1) * 128], 8, 128)
        for tb in range(4):
            bank = kb.psum()
            for k in range(8):
                kb.mm(bank, bank[:, :], w, w[:, k, :], xT, xT[:, k, tb * 512:(tb + 1) * 512], start=(k == 0), stop=(k == 7))
            dst = up[:, PAD + tb * 512:PAD + (tb + 1) * 512]
            kb.op("act", lambda e: e.copy(dst, bank[:, :]), reads=[bank], writes=[up])
        src = up
        shift = 1
        bufs = [sa, sbb]
        for st in range(gi + 1):
            dst_t = bufs[st % 2]
            eng = "dve" if st % 2 == 0 else "pool"
            a_ = src[:, PAD:PAD + SEQ]
            b_ = src[:, PAD - shift:PAD - shift + SEQ]
            d_ = dst_t[:, PAD:PAD + SEQ]
            kb.op(eng, lambda e: e.tensor_tensor(d_, a_, b_, ALU.add), reads=[src], writes=[dst_t])
            src = dst_t
            shift *= 2
        kb.op("dve", lambda e: e.scalar_tensor_tensor(pl[:, :], src[:, PAD:PAD + SEQ], 1.0 / win, up[:, PAD:PAD + SEQ],
                                                      ALU.mult, ALU.subtract), reads=[src, up], writes=[pl])
        kb.op("dve", lambda e: e.tensor_tensor(t16[:, :], src[:, PAD:PAD + 16], invc[:, gi, :], ALU.mult),
              reads=[src, invc], writes=[t16])
        kb.op("dve", lambda e: e.tensor_tensor(pl[:, 0:16], t16[:, :], up[:, PAD:PAD + 16], ALU.subtract),
              reads=[t16, up, pl], writes=[pl])
        w2 = pw[gi % 2]
        load_w(kb, c, w2, w2[:, :, :], dr["pool_w"][L, gi], 1, 128)
        for tb in range(4):
            bank = kb.psum()
            kb.mm(bank, bank[:, :], w2, w2[:, 0, :], pl, pl[:, tb * 512:(tb + 1) * 512])
            dst = ocT[:, gi, tb * 512:(tb + 1) * 512]
            kb.op("act", lambda e: e.activation(dst, bank[:, :], AF.Identity, scale=psc[:, gi:gi + 1]), reads=[bank, psc], writes=[ocT])
    kb.release(m0)


def layer_norm(kb, c, r_t, r_, g_t, b_t, ws):
    kb.op("dve", lambda e: e.bn_stats(ws.stats[:, 0, :], r_[:, 0:512]), reads=[r_t], writes=[ws.stats])
    kb.op("dve", lambda e: e.bn_stats(ws.stats[:, 1, :], r_[:, 512:1024]), reads=[r_t], writes=[ws.stats])
    kb.op("dve", lambda e: e.bn_aggr(ws.mv[:, :], ws.stats[:, :, :].rearrange("p a b -> p (a b)")), reads=[ws.stats], writes=[ws.mv])
    kb.op("act", lambda e: e.activation(ws.sd[:, :], ws.mv[:, 1:2], AF.Sqrt, bias=c.eps5[:, 0:1]), reads=[ws.mv, c.eps5], writes=[ws.sd])
    kb.op("dve", lambda e: e.reciprocal(ws.sd[:, :], ws.sd[:, :]), reads=[ws.sd], writes=[ws.sd])
    kb.op("dve", lambda e: e.tensor_scalar(r_, r_, ws.mv[:, 0:1], ws.sd[:, 0:1], ALU.subtract, ALU.mult),
          reads=[r_t, ws.mv, ws.sd], writes=[r_t])
    kb.op("pool", lambda e: e.tensor_tensor(r_, r_, g_t[:, :], ALU.mult), reads=[r_t, g_t], writes=[r_t])
    kb.op("pool", lambda e: e.tensor_tensor(r_, r_, b_t[:, :], ALU.add), reads=[r_t, b_t], writes=[r_t])


def ln_ws(kb, pfx):
    ws = Ctx()
    ws.stats = kb.al(pfx + "_st", [128, 2, 6], F32)
    ws.mv = kb.al(pfx + "_mv", [128, 2], F32)
    ws.sd = kb.al(pfx + "_sd", [128, 1], F32)
    return ws


def routing(kb, c, bank, rb, wout_t, wout, ws, sp=None):
    lgle, mx, ohg, eg, se, tmp48, lsel, top8, sel, ex, den, w8 = (
        ws.lgle, ws.mx, ws.ohg, ws.eg, ws.se, ws.tmp48, ws.lsel, ws.top8, ws.sel, ws.ex, ws.den, ws.w8)
    op = kb.op
    op("dve", lambda e: e.tensor_tensor(lgle[:, :], bank[:, 0:36], rb[:, :], ALU.add), reads=[bank, rb], writes=[lgle])
    op("dve", lambda e: e.tensor_reduce(mx[:, 0:1], lgle[:, 0:4], AX.X, ALU.max), reads=[lgle], writes=[mx])
    op("dve", lambda e: e.tensor_scalar(ohg[:, :], lgle[:, 0:4], mx[:, 0:1], None, ALU.is_equal), reads=[lgle, mx], writes=[ohg])
    op("dve", lambda e: e.tensor_scalar(mx[:, 1:2], mx[:, 0:1], -1.0, None, ALU.mult), reads=[mx], writes=[mx])
    op("act", lambda e: e.activation(eg[:, :], lgle[:, 0:4], AF.Exp, bias=mx[:, 1:2]), reads=[lgle, mx], writes=[eg])
    op("dve", lambda e: e.tensor_reduce(se[:, 0:1], eg[:, :], AX.X, ALU.add), reads=[eg], writes=[se])
    op("dve", lambda e: e.reciprocal(se[:, 1:2], se[:, 0:1]), reads=[se], writes=[se])
    le3 = lgle[:, 4:36].rearrange("p (g e) -> p g e", g=4)
    op("dve", lambda e: e.tensor_tensor(tmp48[:, :, :], le3, bc(ohg[:, :].unsqueeze(2), [128, 4, 8]), ALU.mult),
       reads=[lgle, ohg], writes=[tmp48])
    op("dve", lambda e: e.tensor_reduce(lsel[:, :], tmp48[:, :, :].rearrange("p g e -> p e g"), AX.X, ALU.add),
       reads=[tmp48], writes=[lsel])
    op("dve", lambda e: e.max(top8[:, :], lsel[:, :]), reads=[lsel], writes=[top8])
    op("dve", lambda e: e.tensor_scalar(sel[:, :], lsel[:, :], top8[:, 1:2], None, ALU.is_ge), reads=[lsel, top8], writes=[sel])
    op("dve", lambda e: e.tensor_scalar(mx[:, 2:3], top8[:, 0:1], -1.0, None, ALU.mult), reads=[top8], writes=[mx])
    op("act", lambda e: e.activation(ex[:, :], lsel[:, :], AF.Exp, bias=mx[:, 2:3]), reads=[lsel, mx], writes=[ex])
    op("dve", lambda e: e.tensor_tensor(ex[:, :], ex[:, :], sel[:, :], ALU.mult), reads=[ex, sel], writes=[ex])
    op("dve", lambda e: e.tensor_reduce(den[:, 0:1], ex[:, :], AX.X, ALU.add), reads=[ex], writes=[den])
    op("dve", lambda e: e.reciprocal(den[:, 1:2], den[:, 0:1]), reads=[den], writes=[den])
    op("dve", lambda e: e.tensor_tensor(den[:, 2:3], den[:, 1:2], se[:, 1:2], ALU.mult), reads=[den, se], writes=[den])
    op("dve", lambda e: e.tensor_scalar(w8[:, :], ex[:, :], den[:, 2:3], None, ALU.mult), reads=[ex, den], writes=[w8])
    op("dve", lambda e: e.tensor_tensor(wout, bc(ohg[:, :].unsqueeze(2), [128, 4, 8]), bc(w8[:, :].unsqueeze(1), [128, 4, 8]), ALU.mult),
       reads=[ohg, w8], writes=[wout_t])
    if sp is not None:
        ohs_t, ohs, oha_t, oha, wab_t, wab = sp
        selA = ws.selA
        op("dve", lambda e: e.tensor_scalar(selA[:, :], lsel[:, :], top8[:, 0:1], None, ALU.is_equal), reads=[lsel, top8], writes=[selA])
        op("dve", lambda e: e.tensor_tensor(ohs, bc(ohg[:, :].unsqueeze(2), [128, 4, 8]), bc(sel[:, :].unsqueeze(1), [128, 4, 8]), ALU.mult),
           reads=[ohg, sel], writes=[ohs_t])
        op("dve", lambda e: e.tensor_tensor(oha, bc(ohg[:, :].unsqueeze(2), [128, 4, 8]), bc(selA[:, :].unsqueeze(1), [128, 4, 8]), ALU.mult),
           reads=[ohg, selA], writes=[oha_t])
        op("dve", lambda e: e.tensor_tensor(selA[:, :], selA[:, :], w8[:, :], ALU.mult), reads=[selA, w8], writes=[selA])
        op("dve", lambda e: e.tensor_reduce(wab[:, 0:1], selA[:, :], AX.X, ALU.add), reads=[selA], writes=[wab_t])
        op("dve", lambda e: e.tensor_reduce(den[:, 3:4], w8[:, :], AX.X, ALU.add), reads=[w8], writes=[den])
        op("dve", lambda e: e.tensor_tensor(wab[:, 1:2], den[:, 3:4], wab[:, 0:1], ALU.subtract), reads=[den, wab_t], writes=[wab_t])


def merge_stage(kb, c, L, dr, w_in_l, xT, oT, x_t, x_dram, x1_t, x1_dram, wts, spr=None):
    m0 = kb.mark()
    al = kb.al
    yT = al("m_yT", [128, 8, SEQ], BF16)
    if wts is None:
        wts = al("m_wts", [128, NT, 32], F32)
    wg = [al("m_wg%d" % i, [128, 8, 128], BF16) for i in range(3)]
    wbr = [al("m_wbr%d" % i, [128, 4, 128], BF16) for i in range(3)]
    gs = [al("m_gs%d" % i, [128, 512], F32) for i in range(2)]
    ya = [al("m_ya%d" % i, [128, 512], F32) for i in range(2)]
    tm = [al("m_tm%d" % i, [128, 512], F32) for i in range(2)]
    gateb = al("m_gateb", [128, 24], F32)
    kb.dma("sp", gateb[:, :], dr["p_gateb"][L], writes=[gateb])
    brw = (dr["w_branch_a"][L], dr["w_branch_b"][L], dr["w_branch_c"][L])
    it = 0
    for dc in range(8):
        for br in range(3):
            col = C_GATE + br * 1024 + dc * 128
            load_w(kb, c, wg[br], wg[br][:, :, :], w_in_l[:, col:col + 128], 8, 128)
            load_w(kb, c, wbr[br], wbr[br][:, :, :], brw[br][:, dc * 128:(dc + 1) * 128], 4, 128)
        for tb in range(4):
            tsl = slice(tb * 512, (tb + 1) * 512)
            y_ = ya[tb % 2]
            for br in range(3):
                bg_ = kb.psum()
                for k in range(8):
                    kb.mm(bg_, bg_[:, :], wg[br], wg[br][:, k, :], xT, xT[:, k, tsl], start=(k == 0), stop=(k == 7))
                g_ = gs[it % 2]
                t_ = tm[it % 2]
                it += 1
                kb.op("act", lambda e: e.activation(g_[:, :], bg_[:, :], AF.Sigmoid, bias=gateb[:, br * 8 + dc:br * 8 + dc + 1]),
                      reads=[bg_, gateb], writes=[g_])
                bb_ = kb.psum()
                for k in range(4):
                    kb.mm(bb_, bb_[:, :], wbr[br], wbr[br][:, k, :], oT[br], oT[br][:, k, tsl], start=(k == 0), stop=(k == 3))
                if br == 0:
                    kb.op("dve", lambda e: e.tensor_tensor(y_[:, :], g_[:, :], bb_[:, :], ALU.mult), reads=[g_, bb_], writes=[y_])
                elif br == 1:
                    kb.op("dve", lambda e: e.tensor_tensor(t_[:, :], g_[:, :], bb_[:, :], ALU.mult), reads=[g_, bb_], writes=[t_])
                    kb.op("pool", lambda e: e.tensor_tensor(y_[:, :], y_[:, :], t_[:, :], ALU.add), reads=[y_, t_], writes=[y_])
                else:
                    kb.op("dve", lambda e: e.tensor_tensor(t_[:, :], g_[:, :], bb_[:, :], ALU.mult), reads=[g_, bb_], writes=[t_])
                    kb.op("pool", lambda e: e.tensor_tensor(yT[:, dc, tsl], y_[:, :], t_[:, :], ALU.add), reads=[y_, t_], writes=[yT])
    wo = c.wb
    load_w(kb, c, wo[0], wo[0][:, :, :], dr["w_out"][L][:, 0:512], 8, 512)
    load_w(kb, c, wo[1], wo[1][:, :, :], dr["w_out"][L][:, 512:1024], 8, 512)
    lng = al("m_lng", [128, 1024], F32)
    lnb = al("m_lnb", [128, 1024], F32)
    kb.dma("sp", lng[:, :], dr["p_ln"][L, 0], writes=[lng])
    kb.dma("sp", lnb[:, :], dr["p_ln"][L, 1], writes=[lnb])
    rw = al("m_rw", [128, 8, 36], F32)
    rb = al("m_rb", [128, 36], F32)
    kb.dma("sp", rw[:, :, :], dr["p_rw"][L], writes=[rw])
    kb.dma("sp", rb[:, :], dr["p_rb"][L], writes=[rb])
    xin = [al("m_xin%d" % i, [128, 1024], F32) for i in range(2)]
    rr = [al("m_r%d" % i, [128, 1024], F32) for i in range(2)]
    x1Tf = al("m_x1Tf", [128, 8, 128], F32)
    lws = ln_ws(kb, "m_ln")
    rws = Ctx()
    rws.lgle = al("r_lgle", [128, 36], F32)
    rws.mx = al("r_mx", [128, 4], F32)
    rws.ohg = al("r_ohg", [128, 4], F32)
    rws.eg = al("r_eg", [128, 4], F32)
    rws.se = al("r_se", [128, 2], F32)
    rws.tmp48 = al("r_tmp48", [128, 4, 8], F32)
    rws.lsel = al("r_lsel", [128, 8], F32)
    rws.top8 = al("r_top8", [128, 8], F32)
    rws.sel = al("r_sel", [128, 8], F32)
    rws.ex = al("r_ex", [128, 8], F32)
    rws.den = al("r_den", [128, 4], F32)
    rws.w8 = al("r_w8", [128, 8], F32)
    rws.selA = al("r_selA", [128, 8], F32)
    for n in range(NT):
        xi = xin[n % 2]
        r_ = rr[n % 2]
        kb.dma("sp", xi[:, :], x_dram[n * 128:(n + 1) * 128, :], reads=[x_t], writes=[xi])
        for half in range(2):
            bank = kb.psum()
            for k in range(8):
                kb.mm(bank, bank[:, :], yT, yT[:, k, n * 128:(n + 1) * 128], wo[half], wo[half][:, k, :], start=(k == 0), stop=(k == 7))
            hs = slice(half * 512, (half + 1) * 512)
            kb.op("dve", lambda e: e.scalar_tensor_tensor(r_[:, hs], xi[:, hs], DN_ALPHA, bank[:, :], ALU.mult, ALU.add),
                  reads=[xi, bank], writes=[r_])
        layer_norm(kb, c, r_, r_[:, :], lng, lnb, lws)
        kb.dma("sp", x1_dram[n * 128:(n + 1) * 128, :], r_[:, :], reads=[r_], writes=[x1_t])
        for half in range(2):
            bank = kb.psum()
            for j in range(4):
                k = half * 4 + j
                kb.tr(bank, bank[:, j * 128:(j + 1) * 128], r_, r_[:, k * 128:(k + 1) * 128], c.identf, c.identf[:, :])
            src = bank[:, :].rearrange("p (k t) -> p k t", k=4)
            kb.op("act", lambda e: e.copy(xT[:, half * 4:half * 4 + 4, n * 128:(n + 1) * 128], src), reads=[bank], writes=[xT])
            kb.op("dve", lambda e: e.tensor_copy(x1Tf[:, half * 4:half * 4 + 4, :], src), reads=[bank], writes=[x1Tf])
        bank = kb.psum()
        for k in range(8):
            kb.mm(bank, bank[:, 0:36], x1Tf, x1Tf[:, k, :], rw, rw[:, k, :], start=(k == 0), stop=(k == 7))
        sp = None
        if spr is not None:
            ohs, oha, wab = spr
            sp = (ohs, ohs[:, n, :].rearrange("p (g e) -> p g e", g=4), oha, oha[:, n, :].rearrange("p (g e) -> p g e", g=4),
                  wab, wab[:, n, :])
        routing(kb, c, bank, rb, wts, wts[:, n, :].rearrange("p (g e) -> p g e", g=4), rws, sp)
    kb.release(m0)


def moe_stage(kb, c, L, dr, xT, wts, x1_t, x1_dram, o_t, out_dram, n_exp=32):
    m0 = kb.mark()
    al = kb.al
    acc = al("e_acc", [128, 8, 1024], F32)
    w1b = [al("e_w1b%d" % i, [128, 8, 512], BF16) for i in range(2)]
    w3b = [al("e_w3b%d" % i, [128, 8, 512], BF16) for i in range(2)]
    w2b = [al("e_w2b%d" % i, [128, 4, 1024], BF16) for i in range(2)]
    hT = [al("e_hT%d" % i, [128, 4, 512], BF16) for i in range(2)]
    sl = [al("e_sl%d" % i, [128, 512], F32) for i in range(2)]
    lng = al("e_lng", [128, 1024], F32)
    lnb = al("e_lnb", [128, 1024], F32)
    kb.dma("sp", lng[:, :], dr["p_ln"][L, 2], writes=[lng])
    kb.dma("sp", lnb[:, :], dr["p_ln"][L, 3], writes=[lnb])
    xin = [al("e_xin%d" % i, [128, 1024], F32) for i in range(2)]
    lws = ln_ws(kb, "e_ln")
    it = 0
    for hf in range(2):
        for ex in range(n_exp):
            wi = ex % 2
            load_w(kb, c, w1b[wi], w1b[wi][:, :, :], dr["moe_w1"][L, ex], 8, 512)
            load_w(kb, c, w3b[wi], w3b[wi][:, :, :], dr["moe_w3"][L, ex], 8, 512)
            load_w(kb, c, w2b[wi], w2b[wi][:, :, :], dr["moe_w2"][L, ex], 4, 1024)
            for tb2 in range(2):
                tok0 = hf * 1024 + tb2 * 512
                h_ = hT[it % 2]
                it += 1
                for fc in range(4):
                    b1 = kb.psum()
                    for k in range(8):
                        kb.mm(b1, b1[:, :], w1b[wi], w1b[wi][:, k, fc * 128:(fc + 1) * 128], xT, xT[:, k, tok0:tok0 + 512],
                              start=(k == 0), stop=(k == 7))
                    b3 = kb.psum()
                    for k in range(8):
                        kb.mm(b3, b3[:, :], w3b[wi], w3b[wi][:, k, fc * 128:(fc + 1) * 128], xT, xT[:, k, tok0:tok0 + 512],
                              start=(k == 0), stop=(k == 7))
                    s_ = sl[fc % 2]
                    kb.op("act", lambda e: e.activation(s_[:, :], b1[:, :], AF.Silu), reads=[b1], writes=[s_])
                    kb.op("dve", lambda e: e.tensor_tensor(h_[:, fc, :], s_[:, :], b3[:, :], ALU.mult), reads=[s_, b3], writes=[h_])
                for sub in range(4):
                    nl = tb2 * 4 + sub
                    n = hf * 8 + nl
                    for half in range(2):
                        b_ = kb.psum()
                        for fc in range(4):
                            kb.mm(b_, b_[:, :], h_, h_[:, fc, sub * 128:(sub + 1) * 128], w2b[wi], w2b[wi][:, fc, half * 512:(half + 1) * 512],
                                  start=(fc == 0), stop=(fc == 3))
                        a_ = acc[:, nl, half * 512:(half + 1) * 512]
                        wcol = wts[:, n, ex:ex + 1]
                        if ex == 0:
                            kb.op("dve", lambda e: e.tensor_scalar(a_, b_[:, :], wcol, None, ALU.mult), reads=[b_, wts], writes=[acc])
                        else:
                            kb.op("dve", lambda e: e.scalar_tensor_tensor(a_, b_[:, :], wcol, a_, ALU.mult, ALU.add),
                                  reads=[b_, wts, acc], writes=[acc])
        for nl in range(8):
            n = hf * 8 + nl
            xi = xin[nl % 2]
            kb.dma("sp", xi[:, :], x1_dram[n * 128:(n + 1) * 128, :], reads=[x1_t], writes=[xi])
            kb.op("dve", lambda e: e.scalar_tensor_tensor(xi[:, :], xi[:, :], DN_ALPHA, acc[:, nl, :], ALU.mult, ALU.add),
                  reads=[xi, acc], writes=[xi])
            layer_norm(kb, c, xi, xi[:, :], lng, lnb, lws)
            kb.dma("sp", out_dram[n * 128:(n + 1) * 128, :], xi[:, :], reads=[xi], writes=[o_t])
    kb.release(m0)


NBLK = 64


def moe_sparse_stage(kb, c, L, dr, spr, x1_t, x1_dram, o_t, out_dram, scr):
    IOA = bass.IndirectOffsetOnAxis
    ohs, oha, wab = spr
    xs_t, xs, yb_t, ybuf = scr
    m0 = kb.mark()
    al = kb.al
    rk = al("s_rk", [128, NT, 32], F32)
    cnt = al("s_cnt", [128, NT, 32], F32)
    off = al("s_off", [128, NT, 32], F32)
    tot = al("s_tot", [128, 32], F32)
    cmp1 = al("s_cmp1", [128, 32, 16], F32)
    nb = al("s_nb", [128, 32], F32)
    pe_ = al("s_pe", [128, 32], F32)
    ps_ = al("s_ps", [128, 32], F32)
    one32 = al("s_one32", [128, 32], F32)
    pos = al("s_pos", [128, NT, 32], F32)
    tmpp = al("s_tmpp", [128, NT, 32], F32)
    dA = al("s_dA", [128, NT], F32)
    dB = al("s_dB", [128, NT], F32)
    dAi = al("s_dAi", [128, NT], I32)
    dBi = al("s_dBi", [128, NT], I32)
    cmp2 = al("s_cmp2", [128, NBLK, 32], F32)
    be = al("s_be", [128, NBLK], F32)
    idxw = al("s_idxw", [128, NBLK], I32)
    thr = al("s_thr", [128, 16 + NBLK + 1], F32)
    kb.dma("sp", thr[:, :], dr["c_thr"][:, :], writes=[thr])
    kb.op("pool", lambda e: e.memset(one32[:, :], 1.0), writes=[one32])
    ohf = ohs[:, :, :].rearrange("p n e -> p (n e)")
    bank = kb.psum()
    kb.mm(bank, bank[:, :], c.SU, c.SU[:, :], ohs, ohf)
    kb.op("act", lambda e: e.copy(rk[:, :, :].rearrange("p n e -> p (n e)"), bank[:, :]), reads=[bank], writes=[rk])
    bank = kb.psum()
    kb.mm(bank, bank[:, :], c.onesf, c.onesf[:, :], ohs, ohf)
    kb.op("dve", lambda e: e.tensor_copy(cnt[:, :, :].rearrange("p n e -> p (n e)"), bank[:, :]), reads=[bank], writes=[cnt])
    kb.op("pool", lambda e: e.memset(off[:, 0, :], 0.0), writes=[off])
    for n in range(1, NT):
        kb.op("dve", lambda e: e.tensor_tensor(off[:, n, :], off[:, n - 1, :], cnt[:, n - 1, :], ALU.add), reads=[off, cnt], writes=[off])
    kb.op("dve", lambda e: e.tensor_tensor(tot[:, :], off[:, NT - 1, :], cnt[:, NT - 1, :], ALU.add), reads=[off, cnt], writes=[tot])
    kb.op("dve", lambda e: e.tensor_tensor(cmp1[:, :, :], bc(tot[:, :].unsqueeze(2), [128, 32, 16]), bc(thr[:, 0:16].unsqueeze(1), [128, 32, 16]),
                                           ALU.is_gt), reads=[tot, thr], writes=[cmp1])
    kb.op("dve", lambda e: e.tensor_reduce(nb[:, :], cmp1[:, :, :], AX.X, ALU.add), reads=[cmp1], writes=[nb])
    kb.op("dve", lambda e: e.tensor_tensor_scan(pe_[:, :], one32[:, :], nb[:, :], 0.0, ALU.mult, ALU.add), reads=[one32, nb], writes=[pe_])
    kb.op("dve", lambda e: e.tensor_tensor(ps_[:, :], pe_[:, :], nb[:, :], ALU.subtract), reads=[pe_, nb], writes=[ps_])
    kb.op("dve", lambda e: e.tensor_scalar(ps_[:, :], ps_[:, :], 128.0, None, ALU.mult), reads=[ps_], writes=[ps_])
    kb.op("dve", lambda e: e.tensor_scalar(pe_[:, :], pe_[:, :], 128.0, None, ALU.mult), reads=[pe_], writes=[pe_])
    kb.op("dve", lambda e: e.tensor_tensor(pos[:, :, :], off[:, :, :], bc(ps_[:, :].unsqueeze(1), [128, NT, 32]), ALU.add),
          reads=[off, ps_], writes=[pos])
    kb.op("dve", lambda e: e.tensor_tensor(pos[:, :, :], pos[:, :, :], rk[:, :, :], ALU.add), reads=[pos, rk], writes=[pos])
    kb.op("dve", lambda e: e.tensor_tensor(tmpp[:, :, :], pos[:, :, :], oha[:, :, :], ALU.mult), reads=[pos, oha], writes=[tmpp])
    kb.op("dve", lambda e: e.tensor_reduce(dA[:, :], tmpp[:, :, :], AX.X, ALU.add), reads=[tmpp], writes=[dA])
    kb.op("dve", lambda e: e.tensor_tensor(tmpp[:, :, :], pos[:, :, :], ohs[:, :, :], ALU.mult), reads=[pos, ohs], writes=[tmpp])
    kb.op("dve", lambda e: e.tensor_reduce(dB[:, :], tmpp[:, :, :], AX.X, ALU.add), reads=[tmpp], writes=[dB])
    kb.op("dve", lambda e: e.tensor_tensor(dB[:, :], dB[:, :], dA[:, :], ALU.subtract), reads=[dB, dA], writes=[dB])
    kb.op("dve", lambda e: e.tensor_copy(dAi[:, :], dA[:, :]), reads=[dA], writes=[dAi])
    kb.op("dve", lambda e: e.tensor_copy(dBi[:, :], dB[:, :]), reads=[dB], writes=[dBi])
    kb.op("dve", lambda e: e.tensor_tensor(cmp2[:, :, :], bc(pe_[:, :].unsqueeze(1), [128, NBLK, 32]),
                                           bc(thr[:, 16:16 + NBLK].unsqueeze(2), [128, NBLK, 32]), ALU.is_le), reads=[pe_, thr], writes=[cmp2])
    kb.op("dve", lambda e: e.tensor_reduce(be[:, :], cmp2[:, :, :], AX.X, ALU.add), reads=[cmp2], writes=[be])
    kb.op("dve", lambda e: e.tensor_scalar(be[:, :], be[:, :], 31.0, 128.0, ALU.min, ALU.mult), reads=[be], writes=[be])
    kb.op("dve", lambda e: e.tensor_scalar(be[:, :], be[:, :], thr[:, 16 + NBLK:16 + NBLK + 1], None, ALU.add), reads=[be, thr], writes=[be])
    idxw2 = al("s_idxw2", [128, NBLK], I32)
    kb.op("dve", lambda e: e.tensor_scalar(be[:, :], be[:, :], float(L * 8192), None, ALU.add), reads=[be], writes=[be])
    kb.op("dve", lambda e: e.tensor_copy(idxw[:, :], be[:, :]), reads=[be], writes=[idxw])
    kb.op("dve", lambda e: e.tensor_scalar(be[:, :], be[:, :], 4096.0, None, ALU.add), reads=[be], writes=[be])
    kb.op("dve", lambda e: e.tensor_copy(idxw2[:, :], be[:, :]), reads=[be], writes=[idxw2])
    idxs = (idxw, idxw2)
    xin = [al("s_xin%d" % i, [128, 1024], F32) for i in range(2)]
    x1b = [al("s_x1b%d" % i, [128, 1024], BF16) for i in range(2)]
    for n in range(NT):
        xi, xb_ = xin[n % 2], x1b[n % 2]
        kb.dma("sp", xi[:, :], x1_dram[n * 128:(n + 1) * 128, :], reads=[x1_t], writes=[xi])
        kb.op("act", lambda e: e.copy(xb_[:, :], xi[:, :]), reads=[xi], writes=[xb_])
        kb.idma(xs[:, :], IOA(ap=dAi[:, n:n + 1], axis=0), xb_[:, :], None, reads=[xb_, dAi], writes=[xs_t])
        kb.idma(xs[:, :], IOA(ap=dBi[:, n:n + 1], axis=0), xb_[:, :], None, reads=[xb_, dBi], writes=[xs_t])
    w1b = [al("s_w1b%d" % i, [128, 8, 512], BF16) for i in range(2)]
    w3b = [al("s_w3b%d" % i, [128, 8, 512], BF16) for i in range(2)]
    w2b = [al("s_w2b%d" % i, [128, 4, 1024], BF16) for i in range(2)]
    xbk = [al("s_xbk%d" % i, [128, 1024], BF16) for i in range(2)]
    xsT = [al("s_xsT%d" % i, [128, 8, 128], BF16) for i in range(2)]
    sl = [al("s_sl%d" % i, [128, 512], F32) for i in range(2)]
    hT = [al("s_hT%d" % i, [128, 4, 128], BF16) for i in range(2)]
    ybk = [al("s_ybk%d" % i, [128, 1024], F32) for i in range(2)]
    for b in range(NBLK):
        i2 = b % 2
        for (wt_, name) in ((w1b[i2], "moe_w1h"), (w3b[i2], "moe_w3h"), (w2b[i2], "moe_w2h")):
            flat = wt_[:, :, :].rearrange("p a b -> p (a b)")
            for hf in range(2):
                kb.idma(flat[:, hf * 2048:(hf + 1) * 2048], None, dr[name].rearrange("l h r c -> (l h r) c"),
                        IOA(ap=idxs[hf][:, b:b + 1], axis=0), reads=[idxs[hf]], writes=[wt_])
        xb_ = xbk[i2]
        kb.dma("sp", xb_[:, :], xs[b * 128:(b + 1) * 128, :], reads=[xs_t], writes=[xb_])
        bT = kb.psum()
        bTv = bT[:, :].bitcast(BF16)
        for k in range(8):
            kb.tr(bT, bTv[:, k * 128:(k + 1) * 128], xb_, xb_[:, k * 128:(k + 1) * 128], c.identb, c.identb[:, :])
        xt_ = xsT[i2]
        kb.op("act", lambda e: e.copy(xt_[:, :, :], bTv[:, :].rearrange("p (k t) -> p k t", k=8)), reads=[bT], writes=[xt_])
        b1 = kb.psum()
        b3 = kb.psum()
        for fc in range(4):
            for k in range(8):
                kb.mm(b1, b1[:, fc * 128:(fc + 1) * 128], w1b[i2], w1b[i2][:, k, fc * 128:(fc + 1) * 128], xt_, xt_[:, k, :],
                      start=(k == 0), stop=(k == 7))
        for fc in range(4):
            for k in range(8):
                kb.mm(b3, b3[:, fc * 128:(fc + 1) * 128], w3b[i2], w3b[i2][:, k, fc * 128:(fc + 1) * 128], xt_, xt_[:, k, :],
                      start=(k == 0), stop=(k == 7))
        s_ = sl[i2]
        h_ = hT[i2]
        kb.op("act", lambda e: e.activation(s_[:, :], b1[:, :], AF.Silu), reads=[b1], writes=[s_])
        kb.op("dve", lambda e: e.tensor_tensor(h_[:, :, :].rearrange("p a b -> p (a b)"), s_[:, :], b3[:, :], ALU.mult), reads=[s_, b3], writes=[h_])
        y_ = ybk[i2]
        for half in range(2):
            b_ = kb.psum()
            for fc in range(4):
                kb.mm(b_, b_[:, :], h_, h_[:, fc, :], w2b[i2], w2b[i2][:, fc, half * 512:(half + 1) * 512], start=(fc == 0), stop=(fc == 3))
            if half == 0:
                kb.op("act", lambda e: e.copy(y_[:, 0:512], b_[:, :]), reads=[b_], writes=[y_])
            else:
                kb.op("dve", lambda e: e.tensor_copy(y_[:, 512:1024], b_[:, :]), reads=[b_], writes=[y_])
        kb.dma("sp", ybuf[b * 128:(b + 1) * 128, :], y_[:, :], reads=[y_], writes=[yb_t])
    lng = al("s_lng", [128, 1024], F32)
    lnb = al("s_lnb", [128, 1024], F32)
    kb.dma("sp", lng[:, :], dr["p_ln"][L, 2], writes=[lng])
    kb.dma("sp", lnb[:, :], dr["p_ln"][L, 3], writes=[lnb])
    yA = [al("s_yA%d" % i, [128, 1024], F32) for i in range(2)]
    yB = [al("s_yB%d" % i, [128, 1024], F32) for i in range(2)]
    lws = ln_ws(kb, "s_ln")
    for n in range(NT):
        xi, ya_, yb_ = xin[n % 2], yA[n % 2], yB[n % 2]
        kb.dma("sp", xi[:, :], x1_dram[n * 128:(n + 1) * 128, :], reads=[x1_t], writes=[xi])
        kb.idma(ya_[:, :], None, ybuf[:, :], IOA(ap=dAi[:, n:n + 1], axis=0), reads=[yb_t, dAi], writes=[ya_])
        kb.idma(yb_[:, :], None, ybuf[:, :], IOA(ap=dBi[:, n:n + 1], axis=0), reads=[yb_t, dBi], writes=[yb_])
        kb.op("dve", lambda e: e.tensor_scalar(ya_[:, :], ya_[:, :], wab[:, n, 0:1], None, ALU.mult), reads=[ya_, wab], writes=[ya_])
        kb.op("dve", lambda e: e.scalar_tensor_tensor(ya_[:, :], yb_[:, :], wab[:, n, 1:2], ya_[:, :], ALU.mult, ALU.add),
              reads=[yb_, wab, ya_], writes=[ya_])
        kb.op("dve", lambda e: e.scalar_tensor_tensor(xi[:, :], xi[:, :], DN_ALPHA, ya_[:, :], ALU.mult, ALU.add),
              reads=[xi, ya_], writes=[xi])
        layer_norm(kb, c, xi, xi[:, :], lng, lnb, lws)
        kb.dma("sp", out_dram[n * 128:(n + 1) * 128, :], xi[:, :], reads=[xi], writes=[o_t])
    kb.release(m0)


def setup_consts(kb, c, dr):
    def ld(name, shape, src):
        t_ = kb.sb(name, shape, F32)
        kb.dma("sp", t_[:, :], src, writes=[t_])
        return t_
    c.identf = ld("identf", [128, 128], dr["c_ident"][:, :])
    c.U = ld("cU", [128, 128], dr["c_U"][:, :])
    c.V = ld("cV", [128, 128], dr["c_V"][:, :])
    c.onesf = ld("cones", [128, 128], dr["c_ones"][:, :])
    c.mSL = ld("cmSL", [128, 128], dr["c_mSL"][:, :])
    c.mTU = ld("cmTU", [128, 128], dr["c_mTU"][:, :])
    c.SU = ld("cSU", [128, 128], dr["c_SU"][:, :])
    c.bd = ld("cbd", [128, 128], dr["c_bd"][:, :])
    c.obd = ld("cobd", [128, 128], dr["c_obd"][:, :])
    c.amask = ld("amask", [128, 640], dr["c_amask"][:, :])
    c.identb = kb.sb("identb", [128, 128], BF16)
    kb.op("dve", lambda e: e.tensor_copy(c.identb[:, :], c.identf[:, :]), reads=[c.identf], writes=[c.identb])
    c.onespad = kb.sb("onespad", [128, 2, 128], BF16)
    kb.op("pool", lambda e: e.memset(c.onespad[:, :, :], 0.0), writes=[c.onespad])
    kb.op("pool", lambda e: e.memset(c.onespad[:, 0, 0:64], 1.0), writes=[c.onespad])
    kb.op("pool", lambda e: e.memset(c.onespad[:, 1, 64:128], 1.0), writes=[c.onespad])
    c.eps6 = kb.sb("eps6", [128, 1], F32)
    c.eps5 = kb.sb("eps5", [128, 1], F32)
    kb.op("pool", lambda e: e.memset(c.eps6[:, :], RMS_EPS), writes=[c.eps6])
    kb.op("pool", lambda e: e.memset(c.eps5[:, :], LN_EPS), writes=[c.eps5])
    c.one1 = kb.sb("one1", [128, 1], F32)
    kb.op("pool", lambda e: e.memset(c.one1[:, :], 1.0), writes=[c.one1])
    c.wst = [kb.sb("wst%d" % i, [128, WST], F32) for i in range(2)]
    c.wst_i = 0
    c.wb = [kb.sb("wb%d" % i, [128, 8, 512], BF16) for i in range(2)]


IN_SPECS = [
    ("x", None), ("w_in", [DEPTH, D, D_IN]), ("pool_w", [DEPTH, 4, 128, 128]),
    ("w_branch_a", [DEPTH, 512, D]), ("w_branch_b", [DEPTH, 512, D]), ("w_branch_c", [DEPTH, 512, D]),
    ("w_out", [DEPTH, D, D]), ("moe_w1h", [DEPTH, 2, 4096, 2048]), ("moe_w3h", [DEPTH, 2, 4096, 2048]),
    ("moe_w2h", [DEPTH, 2, 4096, 2048]), ("c_SU", [128, 128]), ("c_thr", [128, 16 + 64 + 1]),
    ("a_bias", [DEPTH, 128, 8, 640]), ("c_ident", [128, 128]), ("c_amask", [128, 640]), ("c_U", [128, 128]),
    ("c_V", [128, 128]), ("c_ones", [128, 128]), ("c_mSL", [128, 128]), ("c_mTU", [128, 128]), ("c_bd", [128, 128]), ("c_obd", [128, 128]), ("c_invc", [128, 4, 16]),
    ("p_cw", [DEPTH, 128, 12, 4]), ("p_vec4", [DEPTH, 128, 8]), ("p_normg", [DEPTH, 128, 128]),
    ("p_pscale", [DEPTH, 128, 4]), ("p_gateb", [DEPTH, 128, 24]), ("p_ln", [DEPTH, 4, 128, D]),
    ("p_rw", [DEPTH, 128, 8, 36]), ("p_rb", [DEPTH, 128, 36]),
]


def host_consts(inputs):
    f = lambda k_: np.asarray(inputs[k_], dtype=np.float32)
    out = {}
    out["c_ident"] = np.eye(128, dtype=np.float32)
    t = np.arange(128)
    out["c_U"] = (t[:, None] <= t[None, :]).astype(np.float32)
    out["c_V"] = (t[:, None] > t[None, :]).astype(np.float32)
    out["c_ones"] = np.ones((128, 128), np.float32)
    out["c_mSL"] = np.where(t[:, None] > t[None, :], 0.0, NEG).astype(np.float32)
    out["c_mTU"] = np.where(t[:, None] <= t[None, :], 0.0, NEG).astype(np.float32)
    out["c_SU"] = (t[:, None] < t[None, :]).astype(np.float32)
    thr = np.zeros((128, 16 + 64 + 1), np.float32)
    thr[:, 0:16] = 128.0 * np.arange(16)
    thr[:, 16:80] = 128.0 * np.arange(64)
    thr[:, 80] = np.arange(128)
    out["c_thr"] = thr
    for nm in ("moe_w1", "moe_w3"):
        w = f(nm).reshape(DEPTH, 32, 2, 4, 128, 512)
        out[nm + "h"] = np.ascontiguousarray(w.transpose(0, 2, 1, 4, 3, 5)).reshape(DEPTH, 2, 4096, 2048)
    w = f("moe_w2").reshape(DEPTH, 32, 2, 2, 128, 1024)
    out["moe_w2h"] = np.ascontiguousarray(w.transpose(0, 2, 1, 4, 3, 5)).reshape(DEPTH, 2, 4096, 2048)
    out["c_bd"] = ((t[:, None] // 32) == (t[None, :] // 32)).astype(np.float32)
    out["c_obd"] = 1.0 - out["c_bd"]
    invc = np.zeros((128, 4, 16), np.float32)
    for gi in range(4):
        invc[:, gi, :] = 1.0 / np.minimum(np.arange(16) + 1.0, float(2 ** (gi + 1)))
    out["c_invc"] = invc
    k = np.arange(128)[:, None, None]
    jj = np.arange(5)[None, :, None]
    q = np.arange(128)[None, None, :]
    dist = q - ((jj - 4) * 128 + k)
    idx = np.clip(dist, -63, 256) + 63
    qc = q // 64
    kch = (jj - 4) * 2 + k // 64
    valid = (kch <= qc) & (kch >= qc - 8)
    out["c_amask"] = np.where(valid, 0.0, NEG).astype(np.float32).reshape(128, 640)
    rb = f("attn_rel_bias")
    g = rb[:, :, idx]
    out["a_bias"] = np.ascontiguousarray(g.transpose(0, 2, 1, 3, 4)).reshape(DEPTH, 128, 8, 640)
    cw = f("gdn_conv_w")
    out["p_cw"] = np.ascontiguousarray(cw.reshape(DEPTH, 4, 12, 128).transpose(0, 3, 2, 1))
    v4 = np.concatenate([f("gdn_a_log"), f("gdn_dt_bias")], axis=1)
    out["p_vec4"] = np.ascontiguousarray(np.broadcast_to(v4[:, None, :], (DEPTH, 128, 8)))
    out["p_normg"] = np.ascontiguousarray(np.broadcast_to(f("gdn_norm_g")[:, None, :], (DEPTH, 128, 128)))
    out["p_pscale"] = np.ascontiguousarray(f("pool_scale").reshape(DEPTH, 4, 128).transpose(0, 2, 1))
    out["p_gateb"] = np.ascontiguousarray(f("gate_b").reshape(DEPTH, 3, 8, 128).transpose(0, 3, 1, 2)).reshape(DEPTH, 128, 24)
    ln = np.stack([f("ln1_g"), f("ln1_b"), f("ln2_g"), f("ln2_b")], axis=1)
    out["p_ln"] = np.ascontiguousarray(np.broadcast_to(ln[:, :, None, :], (DEPTH, 4, 128, D)))
    rw = np.concatenate([f("router_group_w"), f("router_expert_w")], axis=2)
    out["p_rw"] = np.ascontiguousarray(rw.reshape(DEPTH, 8, 128, 36).transpose(0, 2, 1, 3))
    rbias = np.concatenate([f("router_group_b"), f("router_expert_b")], axis=1)
    out["p_rb"] = np.ascontiguousarray(np.broadcast_to(rbias[:, None, :], (DEPTH, 128, 36)))
    return out


ARENA_BYTES = 139 * 1024
WST = 1024


def build_program(n_seq=4, n_layers=DEPTH, stages=("attn", "gdn", "pool", "merge", "moe"), debug=False, n_exp=32, layer0=0):
    nc = bass.Bass("TRN2", target_bir_lowering=False)
    dr = {}
    for name, shape in IN_SPECS:
        shape = shape if shape is not None else [n_seq * SEQ, D]
        dr[name] = dram_in(nc, name, shape)
    out = nc.dram_tensor("out", [n_seq * SEQ, D], F32, kind="ExternalOutput").ap()
    kind = "ExternalOutput" if debug else "Internal"
    x1s = nc.dram_tensor("x1s", [n_seq * SEQ, D], F32, kind=kind).ap()
    x2s = nc.dram_tensor("x2s", [n_seq * SEQ, D], F32, kind=kind).ap()
    kb = KB(nc)
    kb.init_psum()
    c = Ctx()
    setup_consts(kb, c, dr)
    xT = kb.sb("xT", [128, 8, SEQ], BF16)
    ohs = kb.sb("ohs", [128, NT, 32], F32)
    oha = kb.sb("oha", [128, NT, 32], F32)
    wab = kb.sb("wab", [128, NT, 2], F32)
    spr = (ohs, oha, wab)
    xs = nc.dram_tensor("xs_scr", [NBLK * 128, D], BF16, kind="Internal").ap()
    ybuf = nc.dram_tensor("yb_scr", [NBLK * 128, D], F32, kind="Internal").ap()
    scr = (T(None, "xs"), xs, T(None, "yb"), ybuf)
    kb.init_arena(ARENA_BYTES)
    dbg = {}
    if debug:
        for nm in ("oaT", "obT", "ocT"):
            dbg[nm] = nc.dram_tensor("dbg_" + nm, [4, 128, SEQ], BF16, kind="ExternalOutput").ap()
        dbg["wts"] = nc.dram_tensor("dbg_wts", [128, NT, 32], F32, kind="ExternalOutput").ap()
    dbg_t = T(None, "dbg")
    x_t, x1_t, x2_t, o_t = T(None, "x"), T(None, "x1s"), T(None, "x2s"), T(None, "out")
    for L in range(layer0, n_layers):
        src, src_t = (dr["x"], x_t) if L == layer0 else (x2s, x2_t)
        dst, dst_t = (out, o_t) if L == n_layers - 1 else (x2s, x2_t)
        for s in range(n_seq):
            rows = slice(s * SEQ, (s + 1) * SEQ)
            build_xT(kb, c, src_t, src[rows, :], xT)
            mo = kb.mark()
            oT = [kb.al(nm, [128, 4, SEQ], BF16) for nm in ("oaT", "obT", "ocT")]
            if "attn" in stages:
                attention_stage(kb, c, L, dr, dr["w_in"][L], xT, oT[0])
            if "gdn" in stages:
                gdn_stage(kb, c, L, dr, dr["w_in"][L], xT, oT[1])
            if "pool" in stages:
                pool_stage(kb, c, L, dr, dr["w_in"][L], xT, oT[2])
            if debug and L == layer0 and s == 0:
                for i, nm in enumerate(("oaT", "obT", "ocT")):
                    kb.dma("sp", dbg[nm].rearrange("c p t -> p c t"), oT[i][:, :, :], reads=[oT[i]], writes=[dbg_t])
            if "merge" in stages:
                merge_stage(kb, c, L, dr, dr["w_in"][L], xT, oT, src_t, src[rows, :], x1_t, x1s[rows, :], None, spr)
            kb.release(mo)
            if "moe" in stages:
                moe_sparse_stage(kb, c, L, dr, spr, x1_t, x1s[rows, :], dst_t, dst[rows, :], scr)
    kb.barrier()
    return nc, kb


_PROG = {}


def kernel(**inputs):
    n_cores = 8
    n_seq = 32 // n_cores
    if "p" not in _PROG:
        _PROG["p"] = build_program(n_seq=n_seq)
    nc, _ = _PROG["p"]
    hc = host_consts(inputs)
    shared = {}
    for name, shape in IN_SPECS:
        if name == "x":
            continue
        shared[name] = hc[name] if name in hc else np.ascontiguousarray(np.asarray(inputs[name], dtype=np.float32))
    x = np.ascontiguousarray(np.asarray(inputs["x"], dtype=np.float32)).reshape(n_cores, n_seq * SEQ, D)
    in_maps = [dict(shared, x=x[i]) for i in range(n_cores)]
    res = run_bass_kernel_spmd(nc, in_maps, core_ids=list(range(n_cores)))
    outs = [np.asarray(r["out"], dtype=np.float32).reshape(n_seq, SEQ, D) for r in res.results]
    return np.concatenate(outs, axis=0)
```

```python
import numpy as np
import concourse.bass as bass
import concourse.mybir as mybir
from concourse.bass_utils import run_bass_kernel_spmd

F32 = mybir.dt.float32
BF16 = mybir.dt.bfloat16
I32 = mybir.dt.int32
AF = mybir.ActivationFunctionType
ALU = mybir.AluOpType
AX = mybir.AxisListType

D = 1024
SEQ = 2048
NT = SEQ // 128
DEPTH = 2
D_IN = 7176
NEG = -30000.0
C_QA, C_KA, C_VA, C_QKVB, C_BETA, C_DEC, C_GBO, C_UC, C_GATE = 0, 512, 1024, 1536, 3072, 3076, 3080, 3592, 4104
DN_ALPHA = (2 * DEPTH) ** 0.25
LN_EPS = 1e-5
RMS_EPS = 1e-6


class Sem:
    def __init__(self, h):
        self.h = h
        self.val = 0


class T:
    def __init__(self, h, name=""):
        self.h = h
        self.name = name
        self.w = {}
        self.r = {}
        self.excl = False

    def __getitem__(self, k):
        return self.h[k]


class KB:
    NDS = 12

    def __init__(self, nc):
        self.nc = nc
        self.eng = {"pe": nc.tensor, "act": nc.scalar, "dve": nc.vector, "pool": nc.gpsimd, "sp": nc.sync}
        self.esem = {e: Sem(nc.alloc_semaphore("s_" + e)) for e in self.eng}
        self.seen = {e: {} for e in self.eng}
        self.dsem = {q: [Sem(nc.alloc_semaphore("d_%s%d" % (q, i))) for i in range(self.NDS)]
                     for q in ("sp", "pool")}
        self.dctr = {q: 0 for q in self.dsem}
        self.n_ins = 0
        self._ps = None
        self._psi = 0

    @staticmethod
    def _deps(reads, writes):
        deps = {}
        for t in reads:
            for s, v in t.w.items():
                if deps.get(s, 0) < v:
                    deps[s] = v
            if t.excl:
                for s, v in t.r.items():
                    if deps.get(s, 0) < v:
                        deps[s] = v
        for t in writes:
            for s, v in t.w.items():
                if deps.get(s, 0) < v:
                    deps[s] = v
            for s, v in t.r.items():
                if deps.get(s, 0) < v:
                    deps[s] = v
        return deps

    def _wait(self, e, deps):
        seen = self.seen[e]
        for s, v in deps.items():
            if seen.get(s, 0) < v:
                self.eng[e].wait_ge(s.h, v)
                seen[s] = v

    def op(self, e, fn, reads=(), writes=()):
        deps = self._deps(reads, writes)
        s = self.esem[e]
        if e == "pe":
            deps.pop(s, None)
        self._wait(e, deps)
        ins = fn(self.eng[e])
        s.val += 1
        ins.then_inc(s.h, 1)
        for t in reads:
            t.r[s] = s.val
        for t in writes:
            t.w[s] = s.val
        self.n_ins += 1
        return ins

    def dma(self, q, out, in_, reads=(), writes=(), **kw):
        pool = self.dsem[q]
        s = pool[self.dctr[q] % len(pool)]
        self.dctr[q] += 1
        deps = self._deps(reads, writes)
        if deps.get(s, 0) < s.val:
            deps[s] = s.val
        self._wait(q, deps)
        ins = self.eng[q].dma_start(out=out, in_=in_, **kw)
        s.val += 16
        ins.then_inc(s.h, 16)
        for t in reads:
            t.r[s] = s.val
        for t in writes:
            t.w[s] = s.val
        self.n_ins += 1
        return ins

    def idma(self, out, out_off, in_, in_off, reads=(), writes=()):
        q = "pool"
        pool = self.dsem[q]
        s = pool[self.dctr[q] % len(pool)]
        self.dctr[q] += 1
        deps = self._deps(reads, writes)
        if deps.get(s, 0) < s.val:
            deps[s] = s.val
        self._wait(q, deps)
        ins = self.nc.gpsimd.indirect_dma_start(out=out, out_offset=out_off, in_=in_, in_offset=in_off)
        s.val += 16
        ins.then_inc(s.h, 16)
        for t in reads:
            t.r[s] = s.val
        for t in writes:
            t.w[s] = s.val
        self.n_ins += 1
        return ins

    def barrier(self):
        allsems = list(self.esem.values()) + [s for p in self.dsem.values() for s in p]
        for e in self.eng:
            self._wait(e, {s: s.val for s in allsems if s.val > 0})

    def sb(self, name, shape, dtype):
        return T(self.nc.alloc_sbuf_tensor(name, list(shape), dtype), name)

    def init_arena(self, nbytes):
        self.arena = self.nc.alloc_sbuf_tensor("arena", [128, nbytes // 2], BF16)
        self.arena_n = nbytes // 2
        self.arena_p = 0

    def al(self, name, shape, dtype):
        n = 1
        for d in shape[1:]:
            n *= d
        n2 = n * (2 if dtype in (F32, I32) else 1)
        n2 = (n2 + 15) // 16 * 16
        assert self.arena_p + n2 <= self.arena_n, ("arena overflow", name, self.arena_p, n2, self.arena_n)
        v = self.arena[0:shape[0], self.arena_p:self.arena_p + n2]
        self.arena_p += n2
        if dtype != BF16:
            v = v.bitcast(dtype)
        v = v[:, 0:n]
        if len(shape) == 3:
            v = v.rearrange("p (a b) -> p a b", a=shape[1])
        elif len(shape) == 4:
            v = v.rearrange("p (a b c) -> p a b c", a=shape[1], b=shape[2])
        return T(v, name)

    def mark(self):
        return self.arena_p

    def release(self, m):
        self.barrier()
        self.arena_p = m

    def init_psum(self):
        self._ps = [T(self.nc.alloc_psum_tensor("ps%d" % i, [128, 512], F32), "ps%d" % i) for i in range(8)]
        for t in self._ps:
            t.excl = True

    def psum(self):
        t = self._ps[self._psi % 8]
        self._psi += 1
        return t

    def mm(self, out_t, out, lhsT_t, lhsT, rhs_t, rhs, start=True, stop=True):
        return self.op("pe", lambda e: e.matmul(out, lhsT, rhs, start=start, stop=stop),
                       reads=[lhsT_t, rhs_t], writes=[out_t])

    def tr(self, out_t, out, in_t, in_, id_t, ident):
        return self.op("pe", lambda e: e.transpose(out, in_, ident), reads=[in_t, id_t], writes=[out_t])


class Ctx:
    pass


def dram_in(nc, name, shape, dtype=F32):
    return nc.dram_tensor(name, list(shape), dtype, kind="ExternalInput").ap()


def load_w(kb, c, dst_t, dst, src, kc, cols):
    per = max(1, WST // cols)
    k0 = 0
    while k0 < kc:
        kk = min(per, kc - k0)
        st = c.wst[c.wst_i % len(c.wst)]
        c.wst_i += 1
        sv = st[:, 0:kk * cols].rearrange("p (k c) -> p k c", k=kk)
        kb.dma("sp", sv, src[k0 * 128:(k0 + kk) * 128, :].rearrange("(k p) c -> p k c", p=128), writes=[st])
        d = dst[:, k0:k0 + kk, :]
        kb.op("pool", lambda e: e.tensor_copy(d, sv), reads=[st], writes=[dst_t])
        k0 += kk


def bc(ap, shape):
    return ap.to_broadcast(list(shape))


def build_xT(kb, c, x_t, x_dram, xT):
    m0 = kb.mark()
    xin = [kb.al("xin%d" % i, [128, 1024], F32) for i in range(2)]
    for n in range(NT):
        xi = xin[n % 2]
        kb.dma("sp", xi[:, :], x_dram[n * 128:(n + 1) * 128, :], reads=[x_t], writes=[xi])
        for half in range(2):
            bank = kb.psum()
            for j in range(4):
                k = half * 4 + j
                kb.tr(bank, bank[:, j * 128:(j + 1) * 128], xi, xi[:, k * 128:(k + 1) * 128], c.identf, c.identf[:, :])
            dst = xT[:, half * 4:half * 4 + 4, n * 128:(n + 1) * 128]
            src = bank[:, :].rearrange("p (k t) -> p k t", k=4)
            if half == 0:
                kb.op("act", lambda e: e.copy(dst, src), reads=[bank], writes=[xT])
            else:
                kb.op("dve", lambda e: e.tensor_copy(dst, src), reads=[bank], writes=[xT])
    kb.release(m0)


def attention_stage(kb, c, L, dr, w_in_l, xT, oaT):
    m0 = kb.mark()
    qT = kb.al("qT", [128, 4, SEQ], BF16)
    kT = kb.al("kT", [128, 4, SEQ], BF16)
    vpad = kb.al("vpad", [128, NT, 8, 128], BF16)
    PT = [kb.al("PT%d" % i, [128, 10 * 128], BF16) for i in range(2)]
    rden = [kb.al("rden%d" % i, [128, 128], F32) for i in range(2)]
    btab = kb.al("btab", [128, 8, 5, 128], BF16)
    bst = [kb.al("bst%d" % i, [128, 640], F32) for i in range(2)]
    wb = c.wb
    for h in range(8):
        b_ = bst[h % 2]
        kb.dma("sp", b_[:, :], dr["a_bias"][L, :, h, :], writes=[b_])
        bt = btab[:, h, :, :].rearrange("p j q -> p (j q)")
        kb.op("dve", lambda e: e.tensor_tensor(bt, b_[:, :], c.amask[:, :], ALU.add), reads=[b_, c.amask], writes=[btab])
    kb.op("pool", lambda e: e.memset(vpad[:, :, :, :], 0.0), writes=[vpad])
    for wi, (c0, dstT, scale) in enumerate(((C_QA, qT, 0.125), (C_KA, kT, 1.0))):
        w = wb[wi % 2]
        load_w(kb, c, w, w[:, :, :], w_in_l[:, c0:c0 + 512], 8, 512)
        for cc in range(4):
            for tb in range(4):
                bank = kb.psum()
                for k in range(8):
                    kb.mm(bank, bank[:, :], w, w[:, k, cc * 128:(cc + 1) * 128], xT, xT[:, k, tb * 512:(tb + 1) * 512],
                          start=(k == 0), stop=(k == 7))
                dst = dstT[:, cc, tb * 512:(tb + 1) * 512]
                kb.op("act", lambda e: e.activation(dst, bank[:, :], AF.Copy, scale=scale), reads=[bank], writes=[dstT])
    w = wb[0]
    load_w(kb, c, w, w[:, :, :], w_in_l[:, C_VA:C_VA + 512], 8, 512)
    for n in range(NT):
        bank = kb.psum()
        for k in range(8):
            kb.mm(bank, bank[:, :], xT, xT[:, k, n * 128:(n + 1) * 128], w, w[:, k, :], start=(k == 0), stop=(k == 7))
        bv = bank[:, :].rearrange("p (hp two d) -> p hp two d", two=2, d=64)
        vv = vpad[:, n, :, :].rearrange("p (hp two) d -> p hp two d", two=2)
        kb.op("dve", lambda e: e.tensor_copy(vv[:, :, 0, 0:64], bv[:, :, 0, :]), reads=[bank], writes=[vpad])
        kb.op("act", lambda e: e.copy(vv[:, :, 1, 64:128], bv[:, :, 1, :]), reads=[bank], writes=[vpad])
    it = 0
    for i in range(NT):
        js = [j for j in range(i - 4, i + 1) if j >= 0]
        for p in range(4):
            tiles = [(hh, j) for hh in (0, 1) for j in js]
            pt = PT[it % 2]
            rd = rden[it % 2]
            it += 1
            for g in range(0, len(tiles), 4):
                grp = tiles[g:g + 4]
                bank = kb.psum()
                for idx, (hh, j) in enumerate(grp):
                    h = 2 * p + hh
                    jj = j - (i - 4)
                    o = bank[:, idx * 128:(idx + 1) * 128]
                    kb.mm(bank, o, kT, kT[hh * 64:(hh + 1) * 64, p, j * 128:(j + 1) * 128],
                          qT, qT[hh * 64:(hh + 1) * 64, p, i * 128:(i + 1) * 128], start=True, stop=False)
                    kb.mm(bank, o, c.identb, c.identb[:, :], btab, btab[:, h, jj, :], start=False, stop=True)
                n_ = len(grp) * 128
                dst = pt[:, g * 128:g * 128 + n_]
                src = bank[:, 0:n_]
                kb.op("act", lambda e: e.activation(dst, src, AF.Exp), reads=[bank], writes=[pt])
            bank = kb.psum()
            for idx, (hh, j) in enumerate(tiles):
                kb.mm(bank, bank[:, 0:128], vpad, vpad[:, j, 2 * p + hh, :], pt, pt[:, idx * 128:(idx + 1) * 128],
                      start=(idx == 0), stop=(idx == len(tiles) - 1))
            for idx, (hh, j) in enumerate(tiles):
                kb.mm(bank, bank[:, 128:256], c.onespad, c.onespad[:, hh, :], pt, pt[:, idx * 128:(idx + 1) * 128],
                      start=(idx == 0), stop=(idx == len(tiles) - 1))
            kb.op("dve", lambda e: e.reciprocal(rd[:, :], bank[:, 128:256]), reads=[bank], writes=[rd])
            dst = oaT[:, p, i * 128:(i + 1) * 128]
            kb.op("dve", lambda e: e.tensor_tensor(dst, bank[:, 0:128], rd[:, :], ALU.mult), reads=[bank, rd], writes=[oaT])
    kb.release(m0)


class _Stop(Exception):
    pass


def _chk(k):
    import os
    if float(os.environ.get("GDN_STOP", "0")) == k:
        raise _Stop()


def gdn_stage(kb, c, L, dr, w_in_l, xT, obT):
    m0 = kb.mark()
    try:
        _gdn_stage(kb, c, L, dr, w_in_l, xT, obT)
    except _Stop:
        pass
    kb.release(m0)


def _gdn_stage(kb, c, L, dr, w_in_l, xT, obT):
    al = kb.al
    pre = al("g_pre", [128, 3 + SEQ], F32)
    acc = al("g_acc", [128, 512], F32)
    tmp = al("g_tmp", [128, 512], F32)
    qb = al("g_qb", [128, SEQ], BF16)
    kbf = al("g_kb", [128, SEQ], BF16)
    vb = al("g_vb", [128, SEQ], BF16)
    gsl = al("g_gsl", [128, NT, 128], BF16)
    u = al("g_u", [128, NT, 128], BF16)
    wT = al("g_wT", [128, SEQ], BF16)
    kd = al("g_kd", [128, NT, 128], BF16)
    qkT = al("g_qkT", [128, NT, 128], BF16)
    obr = al("g_obr", [128, NT, 128], BF16)
    wsm = [al("g_wsm%d" % i, [128, 8, 128], BF16) for i in range(2)]
    w8 = al("g_w8", [128, 8, 8], BF16)
    cw = al("g_cw", [128, 12, 4], F32)
    vec4 = al("g_vec4", [128, 8], F32)
    normg = al("g_normg", [128, 128], F32)
    bg = al("g_bg", [128, 128], F32)
    sm = {n_: al("g_" + n_, [128, 64], F32) for n_ in
          ("beta", "g", "gcum", "gtot", "egc", "ekd", "egl", "beg", "nbeta", "z", "az", "sp")}
    aexp = al("g_aexp", [128, 4], F32)
    ssum = al("g_ssum", [128, NT], F32)
    rstd = al("g_rstd", [128, NT], F32)
    rg = [al("g_rg%d" % i, [128, 4, 128], F32) for i in range(2)]
    Dm3 = [al("g_Dm%d" % i, [128, 4, 128], F32) for i in range(2)]
    Dm = [T(d_[:, :, :].rearrange("p j d -> p (j d)"), d_.name) for d_ in Dm3]
    for a_, b__ in zip(Dm, Dm3):
        a_.w, a_.r = b__.w, b__.r
    DT = [al("g_DT%d" % i, [128, 512], F32) for i in range(2)]
    P = [al("g_P%d" % i, [128, 4, 128], BF16) for i in range(2)]
    PTt = [al("g_PT%d" % i, [128, 4, 128], BF16) for i in range(2)]
    X = [al("g_X%d" % i, [128, 4, 256], BF16) for i in range(2)]
    wtok = rg
    MoT = [al("g_MoT%d" % i, [128, 4, 128], BF16) for i in range(2)]
    TdT = [al("g_TdT%d" % i, [128, 4, 128], BF16) for i in range(2)]
    Bx = [al("g_Bx%d" % i, [128, 4, 256], BF16) for i in range(2)]
    S = al("g_S", [128, 128], F32)
    Sb = al("g_Sb", [128, 128], BF16)
    vn = [al("g_vn%d" % i, [128, 128], BF16) for i in range(2)]
    t1 = [al("g_t1%d" % i, [128, 128], F32) for i in range(2)]
    onf = Dm3

    def v3(t_):
        return t_[:, :].rearrange("p (n h) -> p n h", h=4)

    kb.dma("sp", cw[:, :, :], dr["p_cw"][L], writes=[cw])
    kb.dma("sp", vec4[:, :], dr["p_vec4"][L], writes=[vec4])
    kb.dma("sp", normg[:, :], dr["p_normg"][L], writes=[normg])
    kb.op("pool", lambda e: e.memset(pre[:, 0:3], 0.0), writes=[pre])

    _chk(0.1)
    load_w(kb, c, w8, w8[:, :, :], w_in_l[:, C_BETA:C_BETA + 8], 8, 8)
    _chk(0.2)
    bank = kb.psum()
    for n in range(NT):
        for k in range(8):
            kb.mm(bank, bank[:, n * 8:(n + 1) * 8], xT, xT[:, k, n * 128:(n + 1) * 128], w8, w8[:, k, :],
                  start=(k == 0), stop=(k == 7))
    kb.op("act", lambda e: e.copy(bg[:, :], bank[:, 0:128]), reads=[bank], writes=[bg])
    _chk(0.3)
    bg3 = bg[:, :].rearrange("p (n c) -> p n c", c=8)
    beta, g, gcum, gtot, egc, ekd, egl, beg, nbeta = (sm[k_] for k_ in
                                                      ("beta", "g", "gcum", "gtot", "egc", "ekd", "egl", "beg", "nbeta"))
    z, az, sp = sm["z"], sm["az"], sm["sp"]
    kb.op("act", lambda e: e.activation(v3(beta), bg3[:, :, 0:4], AF.Sigmoid), reads=[bg], writes=[beta])
    kb.op("dve", lambda e: e.tensor_tensor(v3(z), bg3[:, :, 4:8], bc(vec4[:, 4:8].unsqueeze(1), [128, NT, 4]), ALU.add),
          reads=[bg, vec4], writes=[z])
    kb.op("dve", lambda e: e.scalar_tensor_tensor(az[:, :], z[:, :], -1.0, z[:, :], ALU.mult, ALU.max), reads=[z], writes=[az])
    _chk(0.4)
    kb.op("act", lambda e: e.activation(az[:, :], az[:, :], AF.Exp, scale=-1.0), reads=[az], writes=[az])
    kb.op("act", lambda e: e.activation(az[:, :], az[:, :], AF.Ln, bias=c.one1[:, 0:1]), reads=[az, c.one1], writes=[az])
    kb.op("dve", lambda e: e.scalar_tensor_tensor(sp[:, :], z[:, :], 0.0, az[:, :], ALU.max, ALU.add),
          reads=[z, az], writes=[sp])
    kb.op("act", lambda e: e.activation(aexp[:, :], vec4[:, 0:4], AF.Exp), reads=[vec4], writes=[aexp])
    kb.op("dve", lambda e: e.scalar_tensor_tensor(v3(g), v3(sp), -1.0, bc(aexp[:, :].unsqueeze(1), [128, NT, 4]),
                                                  ALU.mult, ALU.mult), reads=[sp, aexp], writes=[g])
    _chk(0.5)
    bank = kb.psum()
    kb.mm(bank, bank[:, 0:64], c.U, c.U[:, :], g, g[:, :])
    _chk(0.52)
    kb.mm(bank, bank[:, 64:128], c.onesf, c.onesf[:, :], g, g[:, :])
    _chk(0.54)
    kb.op("act", lambda e: e.copy(gcum[:, :], bank[:, 0:64]), reads=[bank], writes=[gcum])
    _chk(0.56)
    kb.op("dve", lambda e: e.tensor_copy(gtot[:, :], bank[:, 64:128]), reads=[bank], writes=[gtot])
    _chk(0.6)
    kb.op("act", lambda e: e.activation(egc[:, :], gcum[:, :], AF.Exp), reads=[gcum], writes=[egc])
    kb.op("dve", lambda e: e.tensor_tensor(ekd[:, :], gtot[:, :], gcum[:, :], ALU.subtract), reads=[gtot, gcum], writes=[ekd])
    kb.op("act", lambda e: e.activation(ekd[:, :], ekd[:, :], AF.Exp), reads=[ekd], writes=[ekd])
    kb.op("act", lambda e: e.activation(egl[:, :], gtot[:, :], AF.Exp), reads=[gtot], writes=[egl])
    kb.op("dve", lambda e: e.tensor_tensor(beg[:, :], beta[:, :], egc[:, :], ALU.mult), reads=[beta, egc], writes=[beg])
    kb.op("dve", lambda e: e.tensor_scalar(nbeta[:, :], beta[:, :], -1.0, None, ALU.mult), reads=[beta], writes=[nbeta])

    _chk(1)
    wi = 0
    for h in range(4):
        w = wsm[wi % 2]
        wi += 1
        load_w(kb, c, w, w[:, :, :], w_in_l[:, C_GBO + h * 128:C_GBO + (h + 1) * 128], 8, 128)
        for n0 in range(0, NT, 4):
            bank = kb.psum()
            for j in range(4):
                n = n0 + j
                for k in range(8):
                    kb.mm(bank, bank[:, j * 128:(j + 1) * 128], xT, xT[:, k, n * 128:(n + 1) * 128], w, w[:, k, :],
                          start=(k == 0), stop=(k == 7))
            dst = gsl[:, n0:n0 + 4, :]
            src = bank[:, :].rearrange("p (j d) -> p j d", j=4)
            kb.op("act", lambda e: e.activation(dst, src, AF.Silu), reads=[bank], writes=[gsl])
        _chk(2)
        for t, dstb in ((0, qb), (1, kbf), (2, vb)):
            w = wsm[wi % 2]
            wi += 1
            col = C_QKVB + t * 512 + h * 128
            load_w(kb, c, w, w[:, :, :], w_in_l[:, col:col + 128], 8, 128)
            for tb in range(4):
                bank = kb.psum()
                for k in range(8):
                    kb.mm(bank, bank[:, :], w, w[:, k, :], xT, xT[:, k, tb * 512:(tb + 1) * 512],
                          start=(k == 0), stop=(k == 7))
                dst = pre[:, 3 + tb * 512:3 + (tb + 1) * 512]
                kb.op("act", lambda e: e.copy(dst, bank[:, :]), reads=[bank], writes=[pre])
            cwv = cw[:, t * 4 + h, :]
            for hb in range(4):
                o0 = hb * 512
                kb.op("dve", lambda e: e.tensor_scalar(acc[:, :], pre[:, o0:o0 + 512], cwv[:, 0:1], None, ALU.mult),
                      reads=[pre, cw], writes=[acc])
                for j in range(1, 4):
                    kb.op("dve", lambda e: e.scalar_tensor_tensor(acc[:, :], pre[:, o0 + j:o0 + j + 512], cwv[:, j:j + 1],
                                                                  acc[:, :], ALU.mult, ALU.add),
                          reads=[pre, cw, acc], writes=[acc])
                kb.op("act", lambda e: e.activation(tmp[:, :], acc[:, :], AF.Silu), reads=[acc], writes=[tmp])
                dst = dstb[:, o0:o0 + 512]
                if t < 2:
                    kb.op("pool", lambda e: e.tensor_tensor(acc[:, :], tmp[:, :], tmp[:, :], ALU.mult), reads=[tmp], writes=[acc])
                    sc = (128.0 ** -0.5) if t == 0 else 1.0
                    bank = kb.psum()
                    kb.mm(bank, bank[:, :], c.onesf, c.onesf[:, :], acc, acc[:, :])
                    kb.op("act", lambda e: e.activation(acc[:, :], bank[:, :], AF.Sqrt, bias=c.eps6[:, 0:1]),
                          reads=[bank, c.eps6], writes=[acc])
                    kb.op("dve", lambda e: e.reciprocal(acc[:, :], acc[:, :]), reads=[acc], writes=[acc])
                    kb.op("dve", lambda e: e.scalar_tensor_tensor(dst, tmp[:, :], sc, acc[:, :], ALU.mult, ALU.mult),
                          reads=[tmp, acc], writes=[dstb])
                else:
                    kb.op("pool", lambda e: e.tensor_copy(dst, tmp[:, :]), reads=[tmp], writes=[dstb])
        _chk(3)
        for pair in range(2):
            tgs = [2 * pair, 2 * pair + 1]
            for s, tg in enumerate(tgs):
                n0 = tg * 4
                kb.op("dve", lambda e: e.tensor_tensor(rg[s][:, :, :], bc(c.V[:, :].unsqueeze(1), [128, 4, 128]),
                                                       bc(v3(g)[:, n0:n0 + 4, h:h + 1], [128, 4, 128]), ALU.mult),
                      reads=[c.V, g], writes=[rg[s]])
                bG = kb.psum()
                bGT = kb.psum()
                for j in range(4):
                    o = bG[:, j * 128:(j + 1) * 128]
                    kb.mm(bG, o, c.U, c.U[:, :], rg[s], rg[s][:, j, :], start=True, stop=False)
                    kb.mm(bG, o, c.identf, c.identf[:, :], c.mSL, c.mSL[:, :], start=False, stop=True)
                for j in range(4):
                    o = bGT[:, j * 128:(j + 1) * 128]
                    kb.mm(bGT, o, rg[s], rg[s][:, j, :], c.U, c.U[:, :], start=True, stop=False)
                    kb.mm(bGT, o, c.identf, c.identf[:, :], c.mTU, c.mTU[:, :], start=False, stop=True)
                kb.op("act", lambda e: e.activation(Dm[s][:, :], bG[:, :], AF.Exp), reads=[bG], writes=[Dm[s]])
                kb.op("act", lambda e: e.activation(DT[s][:, :], bGT[:, :], AF.Exp), reads=[bGT], writes=[DT[s]])
                bKK = kb.psum()
                bQK = kb.psum()
                for j in range(4):
                    ts_ = slice((n0 + j) * 128, (n0 + j + 1) * 128)
                    kb.mm(bKK, bKK[:, j * 128:(j + 1) * 128], kbf, kbf[:, ts_], kbf, kbf[:, ts_])
                    kb.mm(bQK, bQK[:, j * 128:(j + 1) * 128], kbf, kbf[:, ts_], qb, qb[:, ts_])
                kb.op("dve", lambda e: e.tensor_tensor(Dm[s][:, :], bKK[:, :], Dm[s][:, :], ALU.mult),
                      reads=[bKK, Dm[s]], writes=[Dm[s]])
                kb.op("dve", lambda e: e.tensor_tensor(P[s][:, :, :], Dm[s][:, :].rearrange("p (j d) -> p j d", j=4),
                                                        bc(v3(nbeta)[:, n0:n0 + 4, h:h + 1], [128, 4, 128]), ALU.mult),
                      reads=[Dm[s], nbeta], writes=[P[s]])
                kb.op("dve", lambda e: e.tensor_tensor(qkT[:, n0:n0 + 4, :], bQK[:, :].rearrange("p (j d) -> p j d", j=4),
                                                       DT[s][:, :].rearrange("p (j d) -> p j d", j=4), ALU.mult),
                      reads=[bQK, DT[s]], writes=[qkT])
                bT = kb.psum()
                bTv = bT[:, :].bitcast(BF16)
                for j in range(4):
                    kb.tr(bT, bTv[:, j * 128:(j + 1) * 128], P[s], P[s][:, j, :], c.identb, c.identb[:, :])
                mtv = bTv[:, 0:512].rearrange("p (j d) -> p j d", j=4)
                bd_b = bc(c.bd[:, :].unsqueeze(1), [128, 4, 128])
                obd_b = bc(c.obd[:, :].unsqueeze(1), [128, 4, 128])
                kb.op("dve", lambda e: e.tensor_tensor(PTt[s][:, :, :], mtv, bd_b, ALU.mult), reads=[bT, c.bd], writes=[PTt[s]])
                kb.op("dve", lambda e: e.tensor_tensor(MoT[s][:, :, :], mtv, obd_b, ALU.mult), reads=[bT, c.obd], writes=[MoT[s]])
                kb.op("dve", lambda e: e.tensor_tensor(P[s][:, :, :], P[s][:, :, :], bd_b, ALU.mult), reads=[P[s], c.bd], writes=[P[s]])
                kb.op("dve", lambda e: e.tensor_tensor(TdT[s][:, :, :], PTt[s][:, :, :], bc(c.identf[:, :].unsqueeze(1), [128, 4, 128]), ALU.add),
                      reads=[PTt[s], c.identf], writes=[TdT[s]])
                bX = kb.psum()
                bXv = bX[:, :].bitcast(BF16)
                for j in range(4):
                    ts_ = slice((n0 + j) * 128, (n0 + j + 1) * 128)
                    kb.tr(bX, bXv[:, j * 128:(j + 1) * 128], vb, vb[:, ts_], c.identb, c.identb[:, :])
                    kb.tr(bX, bXv[:, 512 + j * 128:512 + (j + 1) * 128], kbf, kbf[:, ts_], c.identb, c.identb[:, :])
                vps = bXv[:, 0:512].rearrange("p (j d) -> p j d", j=4)
                kps = bXv[:, 512:1024].rearrange("p (j d) -> p j d", j=4)
                kb.op("dve", lambda e: e.tensor_tensor(Bx[s][:, :, 0:128], vps, bc(v3(beta)[:, n0:n0 + 4, h:h + 1], [128, 4, 128]),
                                                       ALU.mult), reads=[bX, beta], writes=[Bx[s]])
                kb.op("dve", lambda e: e.tensor_tensor(Bx[s][:, :, 128:256], kps, bc(v3(beg)[:, n0:n0 + 4, h:h + 1], [128, 4, 128]),
                                                       ALU.mult), reads=[bX, beg], writes=[Bx[s]])
                kb.op("dve", lambda e: e.tensor_tensor(kd[:, n0:n0 + 4, :], kps, bc(v3(ekd)[:, n0:n0 + 4, h:h + 1], [128, 4, 128]),
                                                       ALU.mult), reads=[bX, ekd], writes=[kd])
            _chk(4)
            for lev in range(4):
                for s, tg in enumerate(tgs):
                    bP = kb.psum()
                    bPT = kb.psum()
                    for j in range(4):
                        kb.mm(bP, bP[:, j * 128:(j + 1) * 128], PTt[s], PTt[s][:, j, :], P[s], P[s][:, j, :])
                    for j in range(4):
                        kb.mm(bPT, bPT[:, j * 128:(j + 1) * 128], P[s], P[s][:, j, :], PTt[s], PTt[s][:, j, :])
                    kb.op("act", lambda e: e.copy(P[s][:, :, :], bP[:, :].rearrange("p (j d) -> p j d", j=4)),
                          reads=[bP], writes=[P[s]])
                    kb.op("act", lambda e: e.copy(PTt[s][:, :, :], bPT[:, :].rearrange("p (j d) -> p j d", j=4)),
                          reads=[bPT], writes=[PTt[s]])
                    bU = kb.psum()
                    for j in range(4):
                        kb.mm(bU, bU[:, j * 128:(j + 1) * 128], P[s], P[s][:, j, :], TdT[s], TdT[s][:, j, :])
                    kb.op("dve", lambda e: e.tensor_tensor(TdT[s][:, :, :], TdT[s][:, :, :], bU[:, :].rearrange("p (j d) -> p j d", j=4), ALU.add),
                          reads=[TdT[s], bU], writes=[TdT[s]])
            for it in range(4):
                for s, tg in enumerate(tgs):
                    n0 = tg * 4
                    if it > 0:
                        bY = [kb.psum(), kb.psum()]
                        for j in range(4):
                            kb.mm(bY[j // 2], bY[j // 2][:, (j % 2) * 256:(j % 2 + 1) * 256], MoT[s], MoT[s][:, j, :], X[s], X[s][:, j, :])
                        for b_ in range(2):
                            yv = bY[b_][:, :].rearrange("p (j d) -> p j d", j=2)
                            kb.op("dve", lambda e: e.tensor_tensor(X[s][:, 2 * b_:2 * b_ + 2, :], Bx[s][:, 2 * b_:2 * b_ + 2, :], yv, ALU.add),
                                  reads=[Bx[s], bY[b_]], writes=[X[s]])
                    src_t = Bx[s] if it == 0 else X[s]
                    bA = [kb.psum(), kb.psum()]
                    for j in range(4):
                        kb.mm(bA[j // 2], bA[j // 2][:, (j % 2) * 256:(j % 2 + 1) * 256], TdT[s], TdT[s][:, j, :], src_t, src_t[:, j, :])
                    for b_ in range(2):
                        av = bA[b_][:, :].rearrange("p (j d) -> p j d", j=2)
                        if it < 3:
                            kb.op("act", lambda e: e.copy(X[s][:, 2 * b_:2 * b_ + 2, :], av), reads=[bA[b_]], writes=[X[s]])
                        else:
                            kb.op("act", lambda e: e.copy(u[:, n0 + 2 * b_:n0 + 2 * b_ + 2, :], av[:, :, 0:128]), reads=[bA[b_]], writes=[u])
                            kb.op("dve", lambda e: e.tensor_copy(wtok[s][:, 2 * b_:2 * b_ + 2, :], av[:, :, 128:256]), reads=[bA[b_]], writes=[wtok[s]])
            _chk(5)
            for s, tg in enumerate(tgs):
                n0 = tg * 4
                bW = kb.psum()
                for j in range(4):
                    kb.tr(bW, bW[:, j * 128:(j + 1) * 128], wtok[s], wtok[s][:, j, :], c.identf, c.identf[:, :])
                kb.op("act", lambda e: e.copy(wT[:, n0 * 128:(n0 + 4) * 128], bW[:, :]), reads=[bW], writes=[wT])
        _chk(6)
        kb.op("pool", lambda e: e.memset(S[:, :], 0.0), writes=[S])
        kb.op("pool", lambda e: e.memset(Sb[:, :], 0.0), writes=[Sb])
        for n in range(NT):
            ts_ = slice(n * 128, (n + 1) * 128)
            col = n * 4 + h
            b1 = kb.psum()
            kb.mm(b1, b1[:, 0:128], wT, wT[:, ts_], Sb, Sb[:, :])
            v_ = vn[n % 2]
            kb.op("dve", lambda e: e.tensor_tensor(v_[:, :], u[:, n, :], b1[:, 0:128], ALU.subtract), reads=[u, b1], writes=[v_])
            b2 = kb.psum()
            kb.mm(b2, b2[:, 0:128], qb, qb[:, ts_], Sb, Sb[:, :])
            kb.mm(b2, b2[:, 128:256], qkT, qkT[:, n, :], v_, v_[:, :])
            t_ = t1[n % 2]
            kb.op("dve", lambda e: e.tensor_scalar(t_[:, :], b2[:, 0:128], egc[:, col:col + 1], None, ALU.mult),
                  reads=[b2, egc], writes=[t_])
            kb.op("dve", lambda e: e.tensor_tensor(obr[:, n, :], t_[:, :], b2[:, 128:256], ALU.add), reads=[t_, b2], writes=[obr])
            if n < NT - 1:
                b3 = kb.psum()
                kb.mm(b3, b3[:, 0:128], kd, kd[:, n, :], v_, v_[:, :])
                kb.op("dve", lambda e: e.scalar_tensor_tensor(S[:, :], S[:, :], egl[:, col:col + 1], b3[:, 0:128], ALU.mult, ALU.add),
                      reads=[S, egl, b3], writes=[S])
                kb.op("act", lambda e: e.copy(Sb[:, :], S[:, :]), reads=[S], writes=[Sb])
        _chk(7)
        for g4 in range(4):
            n0 = g4 * 4
            o_ = onf[g4 % 2]
            kb.op("dve", lambda e: e.tensor_tensor(o_[:, :, :], obr[:, n0:n0 + 4, :], obr[:, n0:n0 + 4, :], ALU.mult),
                  reads=[obr], writes=[o_])
            kb.op("dve", lambda e: e.tensor_reduce(ssum[:, n0:n0 + 4], o_[:, :, :], AX.X, ALU.add), reads=[o_], writes=[ssum])
        kb.op("act", lambda e: e.activation(rstd[:, :], ssum[:, :], AF.Sqrt, bias=c.eps6[:, 0:1], scale=1.0 / 128.0),
              reads=[ssum, c.eps6], writes=[rstd])
        kb.op("dve", lambda e: e.reciprocal(rstd[:, :], rstd[:, :]), reads=[rstd], writes=[rstd])
        for g4 in range(4):
            n0 = g4 * 4
            o_ = onf[g4 % 2]
            kb.op("dve", lambda e: e.tensor_tensor(o_[:, :, :], obr[:, n0:n0 + 4, :], bc(rstd[:, n0:n0 + 4].unsqueeze(2), [128, 4, 128]),
                                                   ALU.mult), reads=[obr, rstd], writes=[o_])
            kb.op("dve", lambda e: e.tensor_tensor(o_[:, :, :], o_[:, :, :], bc(normg[:, :].unsqueeze(1), [128, 4, 128]), ALU.mult),
                  reads=[o_, normg], writes=[o_])
            kb.op("dve", lambda e: e.tensor_tensor(o_[:, :, :], o_[:, :, :], gsl[:, n0:n0 + 4, :], ALU.mult),
                  reads=[o_, gsl], writes=[o_])
            bank = kb.psum()
            for j in range(4):
                kb.tr(bank, bank[:, j * 128:(j + 1) * 128], o_, o_[:, j, :], c.identf, c.identf[:, :])
            kb.op("act", lambda e: e.copy(obT[:, h, n0 * 128:(n0 + 4) * 128], bank[:, :]), reads=[bank], writes=[obT])


def pool_stage(kb, c, L, dr, w_in_l, xT, ocT):
    m0 = kb.mark()
    al = kb.al
    PAD = 16
    up = al("p_up", [128, PAD + SEQ], F32)
    sa = al("p_sa", [128, PAD + SEQ], F32)
    sbb = al("p_sb", [128, PAD + SEQ], F32)
    wu = [al("p_wu%d" % i, [128, 8, 128], BF16) for i in range(2)]
    pw = [al("p_pw%d" % i, [128, 1, 128], BF16) for i in range(2)]
    pl = al("p_pl", [128, SEQ], BF16)
    psc = al("p_psc", [128, 4], F32)
    invc = al("p_invc", [128, 4, 16], F32)
    t16 = al("p_t16", [128, 16], F32)
    kb.dma("sp", psc[:, :], dr["p_pscale"][L], writes=[psc])
    kb.dma("sp", invc[:, :, :], dr["c_invc"], writes=[invc])
    for b_ in (up, sa, sbb):
        kb.op("pool", lambda e: e.memset(b_[:, 0:PAD], 0.0), writes=[b_])
    for gi in range(4):
        win = 2 ** (gi + 1)
        w = wu[gi % 2]
        load_w(kb, c, w, w[:, :, :], w_in_l[:, C_UC + gi * 128:C_UC + (gi + 1) * 128], 8, 128)
        for tb in range(4):
            bank = kb.psum()
            for k in range(8):
                kb.mm(bank, bank[:, :], w, w[:, k, :], xT, xT[:, k, tb * 512:(tb + 1) * 512], start=(k == 0), stop=(k == 7))
            dst = up[:, PAD + tb * 512:PAD + (tb + 1) * 512]
            kb.op("act", lambda e: e.copy(dst, bank[:, :]), reads=[bank], writes=[up])
        src = up
        shift = 1
        bufs = [sa, sbb]
        for st in range(gi + 1):
            dst_t = bufs[st % 2]
            eng = "dve" if st % 2 == 0 else "pool"
            a_ = src[:, PAD:PAD + SEQ]
            b_ = src[:, PAD - shift:PAD - shift + SEQ]
            d_ = dst_t[:, PAD:PAD + SEQ]
            kb.op(eng, lambda e: e.tensor_tensor(d_, a_, b_, ALU.add), reads=[src], writes=[dst_t])
            src = dst_t
            shift *= 2
        kb.op("dve", lambda e: e.scalar_tensor_tensor(pl[:, :], src[:, PAD:PAD + SEQ], 1.0 / win, up[:, PAD:PAD + SEQ],
                                                      ALU.mult, ALU.subtract), reads=[src, up], writes=[pl])
        kb.op("dve", lambda e: e.tensor_tensor(t16[:, :], src[:, PAD:PAD + 16], invc[:, gi, :], ALU.mult),
              reads=[src, invc], writes=[t16])
        kb.op("dve", lambda e: e.tensor_tensor(pl[:, 0:16], t16[:, :], up[:, PAD:PAD + 16], ALU.subtract),
              reads=[t16, up, pl], writes=[pl])
        w2 = pw[gi % 2]
        load_w(kb, c, w2, w2[:, :, :], dr["pool_w"][L, gi], 1, 128)
        for tb in range(4):
            bank = kb.psum()
            kb.mm(bank, bank[:, :], w2, w2[:, 0, :], pl, pl[:, tb * 512:(tb + 1) * 512])
            dst = ocT[:, gi, tb * 512:(tb + 1) * 512]
            kb.op("act", lambda e: e.activation(dst, bank[:, :], AF.Identity, scale=psc[:, gi:gi + 1]), reads=[bank, psc], writes=[ocT])
    kb.release(m0)


def layer_norm(kb, c, r_t, r_, g_t, b_t, ws):
    kb.op("dve", lambda e: e.bn_stats(ws.stats[:, 0, :], r_[:, 0:512]), reads=[r_t], writes=[ws.stats])
    kb.op("dve", lambda e: e.bn_stats(ws.stats[:, 1, :], r_[:, 512:1024]), reads=[r_t], writes=[ws.stats])
    kb.op("dve", lambda e: e.bn_aggr(ws.mv[:, :], ws.stats[:, :, :].rearrange("p a b -> p (a b)")), reads=[ws.stats], writes=[ws.mv])
    kb.op("act", lambda e: e.activation(ws.sd[:, :], ws.mv[:, 1:2], AF.Sqrt, bias=c.eps5[:, 0:1]), reads=[ws.mv, c.eps5], writes=[ws.sd])
    kb.op("dve", lambda e: e.reciprocal(ws.sd[:, :], ws.sd[:, :]), reads=[ws.sd], writes=[ws.sd])
    kb.op("dve", lambda e: e.tensor_scalar(r_, r_, ws.mv[:, 0:1], ws.sd[:, 0:1], ALU.subtract, ALU.mult),
          reads=[r_t, ws.mv, ws.sd], writes=[r_t])
    kb.op("pool", lambda e: e.tensor_tensor(r_, r_, g_t[:, :], ALU.mult), reads=[r_t, g_t], writes=[r_t])
    kb.op("pool", lambda e: e.tensor_tensor(r_, r_, b_t[:, :], ALU.add), reads=[r_t, b_t], writes=[r_t])


def ln_ws(kb, pfx):
    ws = Ctx()
    ws.stats = kb.al(pfx + "_st", [128, 2, 6], F32)
    ws.mv = kb.al(pfx + "_mv", [128, 2], F32)
    ws.sd = kb.al(pfx + "_sd", [128, 1], F32)
    return ws


def routing(kb, c, bank, rb, wout_t, wout, ws, sp=None):
    lgle, mx, ohg, eg, se, tmp48, lsel, top8, sel, ex, den, w8 = (
        ws.lgle, ws.mx, ws.ohg, ws.eg, ws.se, ws.tmp48, ws.lsel, ws.top8, ws.sel, ws.ex, ws.den, ws.w8)
    op = kb.op
    op("dve", lambda e: e.tensor_tensor(lgle[:, :], bank[:, 0:36], rb[:, :], ALU.add), reads=[bank, rb], writes=[lgle])
    op("dve", lambda e: e.tensor_reduce(mx[:, 0:1], lgle[:, 0:4], AX.X, ALU.max), reads=[lgle], writes=[mx])
    op("dve", lambda e: e.tensor_scalar(ohg[:, :], lgle[:, 0:4], mx[:, 0:1], None, ALU.is_equal), reads=[lgle, mx], writes=[ohg])
    op("dve", lambda e: e.tensor_scalar(mx[:, 1:2], mx[:, 0:1], -1.0, None, ALU.mult), reads=[mx], writes=[mx])
    op("act", lambda e: e.activation(eg[:, :], lgle[:, 0:4], AF.Exp, bias=mx[:, 1:2]), reads=[lgle, mx], writes=[eg])
    op("dve", lambda e: e.tensor_reduce(se[:, 0:1], eg[:, :], AX.X, ALU.add), reads=[eg], writes=[se])
    op("dve", lambda e: e.reciprocal(se[:, 1:2], se[:, 0:1]), reads=[se], writes=[se])
    le3 = lgle[:, 4:36].rearrange("p (g e) -> p g e", g=4)
    op("dve", lambda e: e.tensor_tensor(tmp48[:, :, :], le3, bc(ohg[:, :].unsqueeze(2), [128, 4, 8]), ALU.mult),
       reads=[lgle, ohg], writes=[tmp48])
    op("dve", lambda e: e.tensor_reduce(lsel[:, :], tmp48[:, :, :].rearrange("p g e -> p e g"), AX.X, ALU.add),
       reads=[tmp48], writes=[lsel])
    op("dve", lambda e: e.max(top8[:, :], lsel[:, :]), reads=[lsel], writes=[top8])
    op("dve", lambda e: e.tensor_scalar(sel[:, :], lsel[:, :], top8[:, 1:2], None, ALU.is_ge), reads=[lsel, top8], writes=[sel])
    op("dve", lambda e: e.tensor_scalar(mx[:, 2:3], top8[:, 0:1], -1.0, None, ALU.mult), reads=[top8], writes=[mx])
    op("act", lambda e: e.activation(ex[:, :], lsel[:, :], AF.Exp, bias=mx[:, 2:3]), reads=[lsel, mx], writes=[ex])
    op("dve", lambda e: e.tensor_tensor(ex[:, :], ex[:, :], sel[:, :], ALU.mult), reads=[ex, sel], writes=[ex])
    op("dve", lambda e: e.tensor_reduce(den[:, 0:1], ex[:, :], AX.X, ALU.add), reads=[ex], writes=[den])
    op("dve", lambda e: e.reciprocal(den[:, 1:2], den[:, 0:1]), reads=[den], writes=[den])
    op("dve", lambda e: e.tensor_tensor(den[:, 2:3], den[:, 1:2], se[:, 1:2], ALU.mult), reads=[den, se], writes=[den])
    op("dve", lambda e: e.tensor_scalar(w8[:, :], ex[:, :], den[:, 2:3], None, ALU.mult), reads=[ex, den], writes=[w8])
    op("dve", lambda e: e.tensor_tensor(wout, bc(ohg[:, :].unsqueeze(2), [128, 4, 8]), bc(w8[:, :].unsqueeze(1), [128, 4, 8]), ALU.mult),
       reads=[ohg, w8], writes=[wout_t])
    if sp is not None:
        ohs_t, ohs, oha_t, oha, wab_t, wab = sp
        selA = ws.selA
        op("dve", lambda e: e.tensor_scalar(selA[:, :], lsel[:, :], top8[:, 0:1], None, ALU.is_equal), reads=[lsel, top8], writes=[selA])
        op("dve", lambda e: e.tensor_tensor(ohs, bc(ohg[:, :].unsqueeze(2), [128, 4, 8]), bc(sel[:, :].unsqueeze(1), [128, 4, 8]), ALU.mult),
           reads=[ohg, sel], writes=[ohs_t])
        op("dve", lambda e: e.tensor_tensor(oha, bc(ohg[:, :].unsqueeze(2), [128, 4, 8]), bc(selA[:, :].unsqueeze(1), [128, 4, 8]), ALU.mult),
           reads=[ohg, selA], writes=[oha_t])
        op("dve", lambda e: e.tensor_tensor(selA[:, :], selA[:, :], w8[:, :], ALU.mult), reads=[selA, w8], writes=[selA])
        op("dve", lambda e: e.tensor_reduce(wab[:, 0:1], selA[:, :], AX.X, ALU.add), reads=[selA], writes=[wab_t])
        op("dve", lambda e: e.tensor_reduce(den[:, 3:4], w8[:, :], AX.X, ALU.add), reads=[w8], writes=[den])
        op("dve", lambda e: e.tensor_tensor(wab[:, 1:2], den[:, 3:4], wab[:, 0:1], ALU.subtract), reads=[den, wab_t], writes=[wab_t])


def merge_stage(kb, c, L, dr, w_in_l, xT, oT, x_t, x_dram, x1_t, x1_dram, wts, spr=None):
    m0 = kb.mark()
    al = kb.al
    yT = al("m_yT", [128, 8, SEQ], BF16)
    if wts is None:
        wts = al("m_wts", [128, NT, 32], F32)
    wg = [al("m_wg%d" % i, [128, 8, 128], BF16) for i in range(3)]
    wbr = [al("m_wbr%d" % i, [128, 4, 128], BF16) for i in range(3)]
    gs = [al("m_gs%d" % i, [128, 512], F32) for i in range(2)]
    ya = [al("m_ya%d" % i, [128, 512], F32) for i in range(2)]
    tm = [al("m_tm%d" % i, [128, 512], F32) for i in range(2)]
    gateb = al("m_gateb", [128, 24], F32)
    kb.dma("sp", gateb[:, :], dr["p_gateb"][L], writes=[gateb])
    brw = (dr["w_branch_a"][L], dr["w_branch_b"][L], dr["w_branch_c"][L])
    it = 0
    for dc in range(8):
        for br in range(3):
            col = C_GATE + br * 1024 + dc * 128
            load_w(kb, c, wg[br], wg[br][:, :, :], w_in_l[:, col:col + 128], 8, 128)
            load_w(kb, c, wbr[br], wbr[br][:, :, :], brw[br][:, dc * 128:(dc + 1) * 128], 4, 128)
        for tb in range(4):
            tsl = slice(tb * 512, (tb + 1) * 512)
            y_ = ya[tb % 2]
            for br in range(3):
                bg_ = kb.psum()
                for k in range(8):
                    kb.mm(bg_, bg_[:, :], wg[br], wg[br][:, k, :], xT, xT[:, k, tsl], start=(k == 0), stop=(k == 7))
                g_ = gs[it % 2]
                t_ = tm[it % 2]
                it += 1
                kb.op("act", lambda e: e.activation(g_[:, :], bg_[:, :], AF.Sigmoid, bias=gateb[:, br * 8 + dc:br * 8 + dc + 1]),
                      reads=[bg_, gateb], writes=[g_])
                bb_ = kb.psum()
                for k in range(4):
                    kb.mm(bb_, bb_[:, :], wbr[br], wbr[br][:, k, :], oT[br], oT[br][:, k, tsl], start=(k == 0), stop=(k == 3))
                if br == 0:
                    kb.op("dve", lambda e: e.tensor_tensor(y_[:, :], g_[:, :], bb_[:, :], ALU.mult), reads=[g_, bb_], writes=[y_])
                elif br == 1:
                    kb.op("dve", lambda e: e.tensor_tensor(t_[:, :], g_[:, :], bb_[:, :], ALU.mult), reads=[g_, bb_], writes=[t_])
                    kb.op("pool", lambda e: e.tensor_tensor(y_[:, :], y_[:, :], t_[:, :], ALU.add), reads=[y_, t_], writes=[y_])
                else:
                    kb.op("dve", lambda e: e.tensor_tensor(t_[:, :], g_[:, :], bb_[:, :], ALU.mult), reads=[g_, bb_], writes=[t_])
                    kb.op("pool", lambda e: e.tensor_tensor(yT[:, dc, tsl], y_[:, :], t_[:, :], ALU.add), reads=[y_, t_], writes=[yT])
    wo = c.wb
    load_w(kb, c, wo[0], wo[0][:, :, :], dr["w_out"][L][:, 0:512], 8, 512)
    load_w(kb, c, wo[1], wo[1][:, :, :], dr["w_out"][L][:, 512:1024], 8, 512)
    lng = al("m_lng", [128, 1024], F32)
    lnb = al("m_lnb", [128, 1024], F32)
    kb.dma("sp", lng[:, :], dr["p_ln"][L, 0], writes=[lng])
    kb.dma("sp", lnb[:, :], dr["p_ln"][L, 1], writes=[lnb])
    rw = al("m_rw", [128, 8, 36], F32)
    rb = al("m_rb", [128, 36], F32)
    kb.dma("sp", rw[:, :, :], dr["p_rw"][L], writes=[rw])
    kb.dma("sp", rb[:, :], dr["p_rb"][L], writes=[rb])
    xin = [al("m_xin%d" % i, [128, 1024], F32) for i in range(2)]
    rr = [al("m_r%d" % i, [128, 1024], F32) for i in range(2)]
    x1Tf = al("m_x1Tf", [128, 8, 128], F32)
    lws = ln_ws(kb, "m_ln")
    rws = Ctx()
    rws.lgle = al("r_lgle", [128, 36], F32)
    rws.mx = al("r_mx", [128, 4], F32)
    rws.ohg = al("r_ohg", [128, 4], F32)
    rws.eg = al("r_eg", [128, 4], F32)
    rws.se = al("r_se", [128, 2], F32)
    rws.tmp48 = al("r_tmp48", [128, 4, 8], F32)
    rws.lsel = al("r_lsel", [128, 8], F32)
    rws.top8 = al("r_top8", [128, 8], F32)
    rws.sel = al("r_sel", [128, 8], F32)
    rws.ex = al("r_ex", [128, 8], F32)
    rws.den = al("r_den", [128, 4], F32)
    rws.w8 = al("r_w8", [128, 8], F32)
    rws.selA = al("r_selA", [128, 8], F32)
    for n in range(NT):
        xi = xin[n % 2]
        r_ = rr[n % 2]
        kb.dma("sp", xi[:, :], x_dram[n * 128:(n + 1) * 128, :], reads=[x_t], writes=[xi])
        for half in range(2):
            bank = kb.psum()
            for k in range(8):
                kb.mm(bank, bank[:, :], yT, yT[:, k, n * 128:(n + 1) * 128], wo[half], wo[half][:, k, :], start=(k == 0), stop=(k == 7))
            hs = slice(half * 512, (half + 1) * 512)
            kb.op("dve", lambda e: e.scalar_tensor_tensor(r_[:, hs], xi[:, hs], DN_ALPHA, bank[:, :], ALU.mult, ALU.add),
                  reads=[xi, bank], writes=[r_])
        layer_norm(kb, c, r_, r_[:, :], lng, lnb, lws)
        kb.dma("sp", x1_dram[n * 128:(n + 1) * 128, :], r_[:, :], reads=[r_], writes=[x1_t])
        for half in range(2):
            bank = kb.psum()
            for j in range(4):
                k = half * 4 + j
                kb.tr(bank, bank[:, j * 128:(j + 1) * 128], r_, r_[:, k * 128:(k + 1) * 128], c.identf, c.identf[:, :])
            src = bank[:, :].rearrange("p (k t) -> p k t", k=4)
            kb.op("act", lambda e: e.copy(xT[:, half * 4:half * 4 + 4, n * 128:(n + 1) * 128], src), reads=[bank], writes=[xT])
            kb.op("dve", lambda e: e.tensor_copy(x1Tf[:, half * 4:half * 4 + 4, :], src), reads=[bank], writes=[x1Tf])
        bank = kb.psum()
        for k in range(8):
            kb.mm(bank, bank[:, 0:36], x1Tf, x1Tf[:, k, :], rw, rw[:, k, :], start=(k == 0), stop=(k == 7))
        sp = None
        if spr is not None:
            ohs, oha, wab = spr
            sp = (ohs, ohs[:, n, :].rearrange("p (g e) -> p g e", g=4), oha, oha[:, n, :].rearrange("p (g e) -> p g e", g=4),
                  wab, wab[:, n, :])
        routing(kb, c, bank, rb, wts, wts[:, n, :].rearrange("p (g e) -> p g e", g=4), rws, sp)
    kb.release(m0)


def moe_stage(kb, c, L, dr, xT, wts, x1_t, x1_dram, o_t, out_dram, n_exp=32):
    m0 = kb.mark()
    al = kb.al
    acc = al("e_acc", [128, 8, 1024], F32)
    w1b = [al("e_w1b%d" % i, [128, 8, 512], BF16) for i in range(2)]
    w3b = [al("e_w3b%d" % i, [128, 8, 512], BF16) for i in range(2)]
    w2b = [al("e_w2b%d" % i, [128, 4, 1024], BF16) for i in range(2)]
    hT = [al("e_hT%d" % i, [128, 4, 512], BF16) for i in range(2)]
    sl = [al("e_sl%d" % i, [128, 512], F32) for i in range(2)]
    lng = al("e_lng", [128, 1024], F32)
    lnb = al("e_lnb", [128, 1024], F32)
    kb.dma("sp", lng[:, :], dr["p_ln"][L, 2], writes=[lng])
    kb.dma("sp", lnb[:, :], dr["p_ln"][L, 3], writes=[lnb])
    xin = [al("e_xin%d" % i, [128, 1024], F32) for i in range(2)]
    lws = ln_ws(kb, "e_ln")
    it = 0
    for hf in range(2):
        for ex in range(n_exp):
            wi = ex % 2
            load_w(kb, c, w1b[wi], w1b[wi][:, :, :], dr["moe_w1"][L, ex], 8, 512)
            load_w(kb, c, w3b[wi], w3b[wi][:, :, :], dr["moe_w3"][L, ex], 8, 512)
            load_w(kb, c, w2b[wi], w2b[wi][:, :, :], dr["moe_w2"][L, ex], 4, 1024)
            for tb2 in range(2):
                tok0 = hf * 1024 + tb2 * 512
                h_ = hT[it % 2]
                it += 1
                for fc in range(4):
                    b1 = kb.psum()
                    for k in range(8):
                        kb.mm(b1, b1[:, :], w1b[wi], w1b[wi][:, k, fc * 128:(fc + 1) * 128], xT, xT[:, k, tok0:tok0 + 512],
                              start=(k == 0), stop=(k == 7))
                    b3 = kb.psum()
                    for k in range(8):
                        kb.mm(b3, b3[:, :], w3b[wi], w3b[wi][:, k, fc * 128:(fc + 1) * 128], xT, xT[:, k, tok0:tok0 + 512],
                              start=(k == 0), stop=(k == 7))
                    s_ = sl[fc % 2]
                    kb.op("act", lambda e: e.activation(s_[:, :], b1[:, :], AF.Silu), reads=[b1], writes=[s_])
                    kb.op("dve", lambda e: e.tensor_tensor(h_[:, fc, :], s_[:, :], b3[:, :], ALU.mult), reads=[s_, b3], writes=[h_])
                for sub in range(4):
                    nl = tb2 * 4 + sub
                    n = hf * 8 + nl
                    for half in range(2):
                        b_ = kb.psum()
                        for fc in range(4):
                            kb.mm(b_, b_[:, :], h_, h_[:, fc, sub * 128:(sub + 1) * 128], w2b[wi], w2b[wi][:, fc, half * 512:(half + 1) * 512],
                                  start=(fc == 0), stop=(fc == 3))
                        a_ = acc[:, nl, half * 512:(half + 1) * 512]
                        wcol = wts[:, n, ex:ex + 1]
                        if ex == 0:
                            kb.op("dve", lambda e: e.tensor_scalar(a_, b_[:, :], wcol, None, ALU.mult), reads=[b_, wts], writes=[acc])
                        else:
                            kb.op("dve", lambda e: e.scalar_tensor_tensor(a_, b_[:, :], wcol, a_, ALU.mult, ALU.add),
                                  reads=[b_, wts, acc], writes=[acc])
        for nl in range(8):
            n = hf * 8 + nl
            xi = xin[nl % 2]
            kb.dma("sp", xi[:, :], x1_dram[n * 128:(n + 1) * 128, :], reads=[x1_t], writes=[xi])
            kb.op("dve", lambda e: e.scalar_tensor_tensor(xi[:, :], xi[:, :], DN_ALPHA, acc[:, nl, :], ALU.mult, ALU.add),
                  reads=[xi, acc], writes=[xi])
            layer_norm(kb, c, xi, xi[:, :], lng, lnb, lws)
            kb.dma("sp", out_dram[n * 128:(n + 1) * 128, :], xi[:, :], reads=[xi], writes=[o_t])
    kb.release(m0)


NBLK = 64


def moe_sparse_stage(kb, c, L, dr, spr, x1_t, x1_dram, o_t, out_dram, scr):
    IOA = bass.IndirectOffsetOnAxis
    ohs, oha, wab = spr
    xs_t, xs, yb_t, ybuf = scr
    m0 = kb.mark()
    al = kb.al
    dAi = al("s_dAi", [128, NT], I32)
    dBi = al("s_dBi", [128, NT], I32)
    idxw = al("s_idxw", [128, NBLK], I32)
    mA = kb.mark()
    rk = al("s_rk", [128, NT, 32], F32)
    cnt = al("s_cnt", [128, NT, 32], F32)
    off = al("s_off", [128, NT, 32], F32)
    tot = al("s_tot", [128, 32], F32)
    cmp1 = al("s_cmp1", [128, 32, 16], F32)
    nb = al("s_nb", [128, 32], F32)
    pe_ = al("s_pe", [128, 32], F32)
    ps_ = al("s_ps", [128, 32], F32)
    one32 = al("s_one32", [128, 32], F32)
    pos = al("s_pos", [128, NT, 32], F32)
    tmpp = al("s_tmpp", [128, NT, 32], F32)
    dA = al("s_dA", [128, NT], F32)
    dB = al("s_dB", [128, NT], F32)
    cmp2 = al("s_cmp2", [128, NBLK, 32], F32)
    be = al("s_be", [128, NBLK], F32)
    thr = al("s_thr", [128, 16 + NBLK + 1], F32)
    kb.dma("sp", thr[:, :], dr["c_thr"][:, :], writes=[thr])
    kb.op("pool", lambda e: e.memset(one32[:, :], 1.0), writes=[one32])
    ohf = ohs[:, :, :].rearrange("p n e -> p (n e)")
    bank = kb.psum()
    kb.mm(bank, bank[:, :], c.SU, c.SU[:, :], ohs, ohf)
    kb.op("act", lambda e: e.copy(rk[:, :, :].rearrange("p n e -> p (n e)"), bank[:, :]), reads=[bank], writes=[rk])
    bank = kb.psum()
    kb.mm(bank, bank[:, :], c.onesf, c.onesf[:, :], ohs, ohf)
    kb.op("dve", lambda e: e.tensor_copy(cnt[:, :, :].rearrange("p n e -> p (n e)"), bank[:, :]), reads=[bank], writes=[cnt])
    kb.op("pool", lambda e: e.memset(off[:, 0, :], 0.0), writes=[off])
    for n in range(1, NT):
        kb.op("dve", lambda e: e.tensor_tensor(off[:, n, :], off[:, n - 1, :], cnt[:, n - 1, :], ALU.add), reads=[off, cnt], writes=[off])
    kb.op("dve", lambda e: e.tensor_tensor(tot[:, :], off[:, NT - 1, :], cnt[:, NT - 1, :], ALU.add), reads=[off, cnt], writes=[tot])
    kb.op("dve", lambda e: e.tensor_tensor(cmp1[:, :, :], bc(tot[:, :].unsqueeze(2), [128, 32, 16]), bc(thr[:, 0:16].unsqueeze(1), [128, 32, 16]),
                                           ALU.is_gt), reads=[tot, thr], writes=[cmp1])
    kb.op("dve", lambda e: e.tensor_reduce(nb[:, :], cmp1[:, :, :], AX.X, ALU.add), reads=[cmp1], writes=[nb])
    kb.op("dve", lambda e: e.tensor_tensor_scan(pe_[:, :], one32[:, :], nb[:, :], 0.0, ALU.mult, ALU.add), reads=[one32, nb], writes=[pe_])
    kb.op("dve", lambda e: e.tensor_tensor(ps_[:, :], pe_[:, :], nb[:, :], ALU.subtract), reads=[pe_, nb], writes=[ps_])
    kb.op("dve", lambda e: e.tensor_scalar(ps_[:, :], ps_[:, :], 128.0, None, ALU.mult), reads=[ps_], writes=[ps_])
    kb.op("dve", lambda e: e.tensor_scalar(pe_[:, :], pe_[:, :], 128.0, None, ALU.mult), reads=[pe_], writes=[pe_])
    kb.op("dve", lambda e: e.tensor_tensor(pos[:, :, :], off[:, :, :], bc(ps_[:, :].unsqueeze(1), [128, NT, 32]), ALU.add),
          reads=[off, ps_], writes=[pos])
    kb.op("dve", lambda e: e.tensor_tensor(pos[:, :, :], pos[:, :, :], rk[:, :, :], ALU.add), reads=[pos, rk], writes=[pos])
    kb.op("dve", lambda e: e.tensor_tensor(tmpp[:, :, :], pos[:, :, :], oha[:, :, :], ALU.mult), reads=[pos, oha], writes=[tmpp])
    kb.op("dve", lambda e: e.tensor_reduce(dA[:, :], tmpp[:, :, :], AX.X, ALU.add), reads=[tmpp], writes=[dA])
    kb.op("dve", lambda e: e.tensor_tensor(tmpp[:, :, :], pos[:, :, :], ohs[:, :, :], ALU.mult), reads=[pos, ohs], writes=[tmpp])
    kb.op("dve", lambda e: e.tensor_reduce(dB[:, :], tmpp[:, :, :], AX.X, ALU.add), reads=[tmpp], writes=[dB])
    kb.op("dve", lambda e: e.tensor_tensor(dB[:, :], dB[:, :], dA[:, :], ALU.subtract), reads=[dB, dA], writes=[dB])
    kb.op("dve", lambda e: e.tensor_copy(dAi[:, :], dA[:, :]), reads=[dA], writes=[dAi])
    kb.op("dve", lambda e: e.tensor_copy(dBi[:, :], dB[:, :]), reads=[dB], writes=[dBi])
    kb.op("dve", lambda e: e.tensor_tensor(cmp2[:, :, :], bc(pe_[:, :].unsqueeze(1), [128, NBLK, 32]),
                                           bc(thr[:, 16:16 + NBLK].unsqueeze(2), [128, NBLK, 32]), ALU.is_le), reads=[pe_, thr], writes=[cmp2])
    kb.op("dve", lambda e: e.tensor_reduce(be[:, :], cmp2[:, :, :], AX.X, ALU.add), reads=[cmp2], writes=[be])
    kb.op("dve", lambda e: e.tensor_scalar(be[:, :], be[:, :], 31.0, 128.0, ALU.min, ALU.mult), reads=[be], writes=[be])
    kb.op("dve", lambda e: e.tensor_scalar(be[:, :], be[:, :], thr[:, 16 + NBLK:16 + NBLK + 1], None, ALU.add), reads=[be, thr], writes=[be])
    kb.op("dve", lambda e: e.tensor_scalar(be[:, :], be[:, :], float(L * 4096), None, ALU.add), reads=[be], writes=[be])
    kb.op("dve", lambda e: e.tensor_copy(idxw[:, :], be[:, :]), reads=[be], writes=[idxw])
    xin = [al("s_xin%d" % i, [128, 1024], F32) for i in range(2)]
    x1b = [al("s_x1b%d" % i, [128, 1024], BF16) for i in range(2)]
    for n in range(NT):
        xi, xb_ = xin[n % 2], x1b[n % 2]
        kb.dma("sp", xi[:, :], x1_dram[n * 128:(n + 1) * 128, :], reads=[x1_t], writes=[xi])
        kb.op("act", lambda e: e.copy(xb_[:, :], xi[:, :]), reads=[xi], writes=[xb_])
        kb.idma(xs[:, :], IOA(ap=dAi[:, n:n + 1], axis=0), xb_[:, :], None, reads=[xb_, dAi], writes=[xs_t])
        kb.idma(xs[:, :], IOA(ap=dBi[:, n:n + 1], axis=0), xb_[:, :], None, reads=[xb_, dBi], writes=[xs_t])
    kb.release(mA)
    stage = [al("s_stg%d" % i, [128, 4096], F32) for i in range(2)]
    si = 0
    w1b = [al("s_w1b%d" % i, [128, 8, 512], BF16) for i in range(2)]
    w3b = [al("s_w3b%d" % i, [128, 8, 512], BF16) for i in range(2)]
    w2b = [al("s_w2b%d" % i, [128, 4, 1024], BF16) for i in range(2)]
    xbk = [al("s_xbk%d" % i, [128, 1024], BF16) for i in range(2)]
    xsT = [al("s_xsT%d" % i, [128, 8, 128], BF16) for i in range(2)]
    sl = [al("s_sl%d" % i, [128, 512], F32) for i in range(2)]
    hT = [al("s_hT%d" % i, [128, 4, 128], BF16) for i in range(2)]
    ybk = [al("s_ybk%d" % i, [128, 1024], F32) for i in range(2)]
    for b in range(NBLK):
        i2 = b % 2
        for (wt_, name, ceng) in ((w1b[i2], "moe_w1h", "act"), (w3b[i2], "moe_w3h", "act"), (w2b[i2], "moe_w2h", "dve")):
            flat = wt_[:, :, :].rearrange("p a b -> p (a b)")
            stg = stage[si % 2]
            si += 1
            kb.idma(stg[:, :], None, dr[name].rearrange("l r c -> (l r) c"), IOA(ap=idxw[:, b:b + 1], axis=0),
                    reads=[idxw], writes=[stg])
            if ceng == "act":
                kb.op("act", lambda e: e.copy(flat, stg[:, :]), reads=[stg], writes=[wt_])
            else:
                kb.op("dve", lambda e: e.tensor_copy(flat, stg[:, :]), reads=[stg], writes=[wt_])
        xb_ = xbk[i2]
        kb.dma("sp", xb_[:, :], xs[b * 128:(b + 1) * 128, :], reads=[xs_t], writes=[xb_])
        bT = kb.psum()
        bTv = bT[:, :].bitcast(BF16)
        for k in range(8):
            kb.tr(bT, bTv[:, k * 128:(k + 1) * 128], xb_, xb_[:, k * 128:(k + 1) * 128], c.identb, c.identb[:, :])
        xt_ = xsT[i2]
        kb.op("act", lambda e: e.copy(xt_[:, :, :], bTv[:, :].rearrange("p (k t) -> p k t", k=8)), reads=[bT], writes=[xt_])
        b1 = kb.psum()
        b3 = kb.psum()
        for fc in range(4):
            for k in range(8):
                kb.mm(b1, b1[:, fc * 128:(fc + 1) * 128], w1b[i2], w1b[i2][:, k, fc * 128:(fc + 1) * 128], xt_, xt_[:, k, :],
                      start=(k == 0), stop=(k == 7))
        for fc in range(4):
            for k in range(8):
                kb.mm(b3, b3[:, fc * 128:(fc + 1) * 128], w3b[i2], w3b[i2][:, k, fc * 128:(fc + 1) * 128], xt_, xt_[:, k, :],
                      start=(k == 0), stop=(k == 7))
        s_ = sl[i2]
        h_ = hT[i2]
        kb.op("act", lambda e: e.activation(s_[:, :], b1[:, :], AF.Silu), reads=[b1], writes=[s_])
        kb.op("dve", lambda e: e.tensor_tensor(h_[:, :, :].rearrange("p a b -> p (a b)"), s_[:, :], b3[:, :], ALU.mult), reads=[s_, b3], writes=[h_])
        y_ = ybk[i2]
        for half in range(2):
            b_ = kb.psum()
            for fc in range(4):
                kb.mm(b_, b_[:, :], h_, h_[:, fc, :], w2b[i2], w2b[i2][:, fc, half * 512:(half + 1) * 512], start=(fc == 0), stop=(fc == 3))
            if half == 0:
                kb.op("act", lambda e: e.copy(y_[:, 0:512], b_[:, :]), reads=[b_], writes=[y_])
            else:
                kb.op("dve", lambda e: e.tensor_copy(y_[:, 512:1024], b_[:, :]), reads=[b_], writes=[y_])
        kb.dma("sp", ybuf[b * 128:(b + 1) * 128, :], y_[:, :], reads=[y_], writes=[yb_t])
    kb.release(mA)
    xin = [al("s_xin%d" % i, [128, 1024], F32) for i in range(2)]
    lng = al("s_lng", [128, 1024], F32)
    lnb = al("s_lnb", [128, 1024], F32)
    kb.dma("sp", lng[:, :], dr["p_ln"][L, 2], writes=[lng])
    kb.dma("sp", lnb[:, :], dr["p_ln"][L, 3], writes=[lnb])
    yA = [al("s_yA%d" % i, [128, 1024], F32) for i in range(2)]
    yB = [al("s_yB%d" % i, [128, 1024], F32) for i in range(2)]
    lws = ln_ws(kb, "s_ln")
    for n in range(NT):
        xi, ya_, yb_ = xin[n % 2], yA[n % 2], yB[n % 2]
        kb.dma("sp", xi[:, :], x1_dram[n * 128:(n + 1) * 128, :], reads=[x1_t], writes=[xi])
        kb.idma(ya_[:, :], None, ybuf[:, :], IOA(ap=dAi[:, n:n + 1], axis=0), reads=[yb_t, dAi], writes=[ya_])
        kb.idma(yb_[:, :], None, ybuf[:, :], IOA(ap=dBi[:, n:n + 1], axis=0), reads=[yb_t, dBi], writes=[yb_])
        kb.op("dve", lambda e: e.tensor_scalar(ya_[:, :], ya_[:, :], wab[:, n, 0:1], None, ALU.mult), reads=[ya_, wab], writes=[ya_])
        kb.op("dve", lambda e: e.scalar_tensor_tensor(ya_[:, :], yb_[:, :], wab[:, n, 1:2], ya_[:, :], ALU.mult, ALU.add),
              reads=[yb_, wab, ya_], writes=[ya_])
        kb.op("dve", lambda e: e.scalar_tensor_tensor(xi[:, :], xi[:, :], DN_ALPHA, ya_[:, :], ALU.mult, ALU.add),
              reads=[xi, ya_], writes=[xi])
        layer_norm(kb, c, xi, xi[:, :], lng, lnb, lws)
        kb.dma("sp", out_dram[n * 128:(n + 1) * 128, :], xi[:, :], reads=[xi], writes=[o_t])
    kb.release(m0)


def setup_consts(kb, c, dr):
    def ld(name, shape, src):
        t_ = kb.sb(name, shape, F32)
        kb.dma("sp", t_[:, :], src, writes=[t_])
        return t_
    c.identf = ld("identf", [128, 128], dr["c_ident"][:, :])
    c.U = ld("cU", [128, 128], dr["c_U"][:, :])
    c.V = ld("cV", [128, 128], dr["c_V"][:, :])
    c.onesf = ld("cones", [128, 128], dr["c_ones"][:, :])
    c.mSL = ld("cmSL", [128, 128], dr["c_mSL"][:, :])
    c.mTU = ld("cmTU", [128, 128], dr["c_mTU"][:, :])
    c.SU = ld("cSU", [128, 128], dr["c_SU"][:, :])
    c.bd = ld("cbd", [128, 128], dr["c_bd"][:, :])
    c.obd = ld("cobd", [128, 128], dr["c_obd"][:, :])
    c.amask = ld("amask", [128, 640], dr["c_amask"][:, :])
    c.identb = kb.sb("identb", [128, 128], BF16)
    kb.op("dve", lambda e: e.tensor_copy(c.identb[:, :], c.identf[:, :]), reads=[c.identf], writes=[c.identb])
    c.onespad = kb.sb("onespad", [128, 2, 128], BF16)
    kb.op("pool", lambda e: e.memset(c.onespad[:, :, :], 0.0), writes=[c.onespad])
    kb.op("pool", lambda e: e.memset(c.onespad[:, 0, 0:64], 1.0), writes=[c.onespad])
    kb.op("pool", lambda e: e.memset(c.onespad[:, 1, 64:128], 1.0), writes=[c.onespad])
    c.eps6 = kb.sb("eps6", [128, 1], F32)
    c.eps5 = kb.sb("eps5", [128, 1], F32)
    kb.op("pool", lambda e: e.memset(c.eps6[:, :], RMS_EPS), writes=[c.eps6])
    kb.op("pool", lambda e: e.memset(c.eps5[:, :], LN_EPS), writes=[c.eps5])
    c.one1 = kb.sb("one1", [128, 1], F32)
    kb.op("pool", lambda e: e.memset(c.one1[:, :], 1.0), writes=[c.one1])
    c.wst = [kb.sb("wst%d" % i, [128, WST], F32) for i in range(2)]
    c.wst_i = 0
    c.wb = [kb.sb("wb%d" % i, [128, 8, 512], BF16) for i in range(2)]


IN_SPECS = [
    ("x", None), ("w_in", [DEPTH, D, D_IN]), ("pool_w", [DEPTH, 4, 128, 128]),
    ("w_branch_a", [DEPTH, 512, D]), ("w_branch_b", [DEPTH, 512, D]), ("w_branch_c", [DEPTH, 512, D]),
    ("w_out", [DEPTH, D, D]), ("moe_w1h", [DEPTH, 4096, 4096]), ("moe_w3h", [DEPTH, 4096, 4096]),
    ("moe_w2h", [DEPTH, 4096, 4096]), ("c_SU", [128, 128]), ("c_thr", [128, 16 + 64 + 1]),
    ("a_bias", [DEPTH, 128, 8, 640]), ("c_ident", [128, 128]), ("c_amask", [128, 640]), ("c_U", [128, 128]),
    ("c_V", [128, 128]), ("c_ones", [128, 128]), ("c_mSL", [128, 128]), ("c_mTU", [128, 128]), ("c_bd", [128, 128]), ("c_obd", [128, 128]), ("c_invc", [128, 4, 16]),
    ("p_cw", [DEPTH, 128, 12, 4]), ("p_vec4", [DEPTH, 128, 8]), ("p_normg", [DEPTH, 128, 128]),
    ("p_pscale", [DEPTH, 128, 4]), ("p_gateb", [DEPTH, 128, 24]), ("p_ln", [DEPTH, 4, 128, D]),
    ("p_rw", [DEPTH, 128, 8, 36]), ("p_rb", [DEPTH, 128, 36]),
]


def host_consts(inputs):
    f = lambda k_: np.asarray(inputs[k_], dtype=np.float32)
    out = {}
    out["c_ident"] = np.eye(128, dtype=np.float32)
    t = np.arange(128)
    out["c_U"] = (t[:, None] <= t[None, :]).astype(np.float32)
    out["c_V"] = (t[:, None] > t[None, :]).astype(np.float32)
    out["c_ones"] = np.ones((128, 128), np.float32)
    out["c_mSL"] = np.where(t[:, None] > t[None, :], 0.0, NEG).astype(np.float32)
    out["c_mTU"] = np.where(t[:, None] <= t[None, :], 0.0, NEG).astype(np.float32)
    out["c_SU"] = (t[:, None] < t[None, :]).astype(np.float32)
    thr = np.zeros((128, 16 + 64 + 1), np.float32)
    thr[:, 0:16] = 128.0 * np.arange(16)
    thr[:, 16:80] = 128.0 * np.arange(64)
    thr[:, 80] = np.arange(128)
    out["c_thr"] = thr
    for nm in ("moe_w1", "moe_w3"):
        w = f(nm).reshape(DEPTH, 32, 8, 128, 512)
        out[nm + "h"] = np.ascontiguousarray(w.transpose(0, 1, 3, 2, 4)).reshape(DEPTH, 4096, 4096)
    w = f("moe_w2").reshape(DEPTH, 32, 4, 128, 1024)
    out["moe_w2h"] = np.ascontiguousarray(w.transpose(0, 1, 3, 2, 4)).reshape(DEPTH, 4096, 4096)
    out["c_bd"] = ((t[:, None] // 32) == (t[None, :] // 32)).astype(np.float32)
    out["c_obd"] = 1.0 - out["c_bd"]
    invc = np.zeros((128, 4, 16), np.float32)
    for gi in range(4):
        invc[:, gi, :] = 1.0 / np.minimum(np.arange(16) + 1.0, float(2 ** (gi + 1)))
    out["c_invc"] = invc
    k = np.arange(128)[:, None, None]
    jj = np.arange(5)[None, :, None]
    q = np.arange(128)[None, None, :]
    dist = q - ((jj - 4) * 128 + k)
    idx = np.clip(dist, -63, 256) + 63
    qc = q // 64
    kch = (jj - 4) * 2 + k // 64
    valid = (kch <= qc) & (kch >= qc - 8)
    out["c_amask"] = np.where(valid, 0.0, NEG).astype(np.float32).reshape(128, 640)
    rb = f("attn_rel_bias")
    g = rb[:, :, idx]
    out["a_bias"] = np.ascontiguousarray(g.transpose(0, 2, 1, 3, 4)).reshape(DEPTH, 128, 8, 640)
    cw = f("gdn_conv_w")
    out["p_cw"] = np.ascontiguousarray(cw.reshape(DEPTH, 4, 12, 128).transpose(0, 3, 2, 1))
    v4 = np.concatenate([f("gdn_a_log"), f("gdn_dt_bias")], axis=1)
    out["p_vec4"] = np.ascontiguousarray(np.broadcast_to(v4[:, None, :], (DEPTH, 128, 8)))
    out["p_normg"] = np.ascontiguousarray(np.broadcast_to(f("gdn_norm_g")[:, None, :], (DEPTH, 128, 128)))
    out["p_pscale"] = np.ascontiguousarray(f("pool_scale").reshape(DEPTH, 4, 128).transpose(0, 2, 1))
    out["p_gateb"] = np.ascontiguousarray(f("gate_b").reshape(DEPTH, 3, 8, 128).transpose(0, 3, 1, 2)).reshape(DEPTH, 128, 24)
    ln = np.stack([f("ln1_g"), f("ln1_b"), f("ln2_g"), f("ln2_b")], axis=1)
    out["p_ln"] = np.ascontiguousarray(np.broadcast_to(ln[:, :, None, :], (DEPTH, 4, 128, D)))
    rw = np.concatenate([f("router_group_w"), f("router_expert_w")], axis=2)
    out["p_rw"] = np.ascontiguousarray(rw.reshape(DEPTH, 8, 128, 36).transpose(0, 2, 1, 3))
    rbias = np.concatenate([f("router_group_b"), f("router_expert_b")], axis=1)
    out["p_rb"] = np.ascontiguousarray(np.broadcast_to(rbias[:, None, :], (DEPTH, 128, 36)))
    return out


ARENA_BYTES = 139 * 1024
WST = 1024


def build_program(n_seq=4, n_layers=DEPTH, stages=("attn", "gdn", "pool", "merge", "moe"), debug=False, n_exp=32, layer0=0):
    nc = bass.Bass("TRN2", target_bir_lowering=False)
    dr = {}
    for name, shape in IN_SPECS:
        shape = shape if shape is not None else [n_seq * SEQ, D]
        dr[name] = dram_in(nc, name, shape)
    out = nc.dram_tensor("out", [n_seq * SEQ, D], F32, kind="ExternalOutput").ap()
    kind = "ExternalOutput" if debug else "Internal"
    x1s = nc.dram_tensor("x1s", [n_seq * SEQ, D], F32, kind=kind).ap()
    x2s = nc.dram_tensor("x2s", [n_seq * SEQ, D], F32, kind=kind).ap()
    kb = KB(nc)
    kb.init_psum()
    c = Ctx()
    setup_consts(kb, c, dr)
    xT = kb.sb("xT", [128, 8, SEQ], BF16)
    ohs = kb.sb("ohs", [128, NT, 32], F32)
    oha = kb.sb("oha", [128, NT, 32], F32)
    wab = kb.sb("wab", [128, NT, 2], F32)
    spr = (ohs, oha, wab)
    xs = nc.dram_tensor("xs_scr", [NBLK * 128, D], BF16, kind="Internal").ap()
    ybuf = nc.dram_tensor("yb_scr", [NBLK * 128, D], F32, kind="Internal").ap()
    scr = (T(None, "xs"), xs, T(None, "yb"), ybuf)
    kb.init_arena(ARENA_BYTES)
    dbg = {}
    if debug:
        for nm in ("oaT", "obT", "ocT"):
            dbg[nm] = nc.dram_tensor("dbg_" + nm, [4, 128, SEQ], BF16, kind="ExternalOutput").ap()
        dbg["wts"] = nc.dram_tensor("dbg_wts", [128, NT, 32], F32, kind="ExternalOutput").ap()
    dbg_t = T(None, "dbg")
    x_t, x1_t, x2_t, o_t = T(None, "x"), T(None, "x1s"), T(None, "x2s"), T(None, "out")
    for L in range(layer0, n_layers):
        src, src_t = (dr["x"], x_t) if L == layer0 else (x2s, x2_t)
        dst, dst_t = (out, o_t) if L == n_layers - 1 else (x2s, x2_t)
        for s in range(n_seq):
            rows = slice(s * SEQ, (s + 1) * SEQ)
            build_xT(kb, c, src_t, src[rows, :], xT)
            mo = kb.mark()
            oT = [kb.al(nm, [128, 4, SEQ], BF16) for nm in ("oaT", "obT", "ocT")]
            if "attn" in stages:
                attention_stage(kb, c, L, dr, dr["w_in"][L], xT, oT[0])
            if "gdn" in stages:
                gdn_stage(kb, c, L, dr, dr["w_in"][L], xT, oT[1])
            if "pool" in stages:
                pool_stage(kb, c, L, dr, dr["w_in"][L], xT, oT[2])
            if debug and L == layer0 and s == 0:
                for i, nm in enumerate(("oaT", "obT", "ocT")):
                    kb.dma("sp", dbg[nm].rearrange("c p t -> p c t"), oT[i][:, :, :], reads=[oT[i]], writes=[dbg_t])
            if "merge" in stages:
                merge_stage(kb, c, L, dr, dr["w_in"][L], xT, oT, src_t, src[rows, :], x1_t, x1s[rows, :], None, spr)
            kb.release(mo)
            if "moe" in stages:
                moe_sparse_stage(kb, c, L, dr, spr, x1_t, x1s[rows, :], dst_t, dst[rows, :], scr)
    kb.barrier()
    return nc, kb


_PROG = {}


def kernel(**inputs):
    n_cores = 8
    n_seq = 32 // n_cores
    if "p" not in _PROG:
        _PROG["p"] = build_program(n_seq=n_seq)
    nc, _ = _PROG["p"]
    hc = host_consts(inputs)
    shared = {}
    for name, shape in IN_SPECS:
        if name == "x":
            continue
        shared[name] = hc[name] if name in hc else np.ascontiguousarray(np.asarray(inputs[name], dtype=np.float32))
    x = np.ascontiguousarray(np.asarray(inputs["x"], dtype=np.float32)).reshape(n_cores, n_seq * SEQ, D)
    in_maps = [dict(shared, x=x[i]) for i in range(n_cores)]
    res = run_bass_kernel_spmd(nc, in_maps, core_ids=list(range(n_cores)))
    outs = [np.asarray(r["out"], dtype=np.float32).reshape(n_seq, SEQ, D) for r in res.results]
    return np.concatenate(outs, axis=0)
```
